# Optimizing a Trainium2 kernel written in Bass

```python
import math
import jax
import jax.numpy as jnp
from jax import lax
import numpy as np

D_MODEL = 1024
BATCH = 8
SEQ = 4096
DEPTH = 4

FOX_HEADS = 8
FOX_HEAD_DIM = D_MODEL // 16
FOX_WIDTH = FOX_HEADS * FOX_HEAD_DIM
FOX_Q_BLOCK = 128
POOL_WINDOWS = (2, 4, 8, 16)
POOL_GROUPS = len(POOL_WINDOWS)
POOL_WIDTH = D_MODEL // 2
POOL_GROUP_DIM = POOL_WIDTH // POOL_GROUPS
EVEN_IN = 3 * FOX_WIDTH + FOX_HEADS + POOL_WIDTH
MOBA_HEADS = 16
MOBA_HEAD_DIM = D_MODEL // MOBA_HEADS
MOBA_WIDTH = MOBA_HEADS * MOBA_HEAD_DIM
MOBA_BLOCK = 256
MOBA_TOPK = 3
MOBA_Q_CHUNK = 16
D_FF = D_MODEL * 7 // 2
N_EXPERTS = 8
TOP_K = 2
LN_EPS = 1e-5
DEEPNORM_ALPHA = (2 * DEPTH) ** 0.25
DEEPNORM_BETA = (8 * DEPTH) ** -0.25
N_EVEN = (DEPTH + 1) // 2
N_ODD = DEPTH // 2

kernel_name = 'fox_pool_moba_moe_deepnorm_trunk'


def layer_norm(x, g, b):
    xf = x.astype(jnp.float32)
    mu = xf.mean(-1, keepdims=True)
    var = jnp.square(xf - mu).mean(-1, keepdims=True)
    return ((xf - mu) * lax.rsqrt(var + LN_EPS) * g + b).astype(x.dtype)


def split_heads(t, n_heads):
    b, s, _ = t.shape
    return t.reshape(b, s, n_heads, -1).transpose(0, 2, 1, 3)


def merge_heads(t):
    b, h, s, d = t.shape
    return t.transpose(0, 2, 1, 3).reshape(b, s, h * d)


def fox_attention(q, k, v, logf):
    b, h, s, d = q.shape
    nq = s // FOX_Q_BLOCK
    scale = d ** -0.5
    cum = jnp.cumsum(logf, axis=-1)
    qb = q.reshape(b, h, nq, FOX_Q_BLOCK, d).transpose(2, 0, 1, 3, 4)
    cb = cum.reshape(b, h, nq, FOX_Q_BLOCK).transpose(2, 0, 1, 3)
    kpos = jnp.arange(s)

    def block(args):
        q_blk, c_blk, i = args
        logits = jnp.einsum('bhqd,bhkd->bhqk', q_blk, k).astype(jnp.float32) * scale
        logits = logits + c_blk[..., None] - cum[:, :, None, :]
        qpos = i * FOX_Q_BLOCK + jnp.arange(FOX_Q_BLOCK)
        logits = jnp.where(kpos[None, :] <= qpos[:, None], logits, -jnp.inf)
        p = jax.nn.softmax(logits, axis=-1)
        return jnp.einsum('bhqk,bhkd->bhqd', p.astype(v.dtype), v)

    out = lax.map(block, (qb, cb, jnp.arange(nq)))
    return out.transpose(1, 2, 0, 3, 4).reshape(b, h, s, d)


def multiscale_pool(u, pool_w, pool_scale):
    b, s, _ = u.shape
    ug = u.reshape(b, s, POOL_GROUPS, POOL_GROUP_DIM).astype(jnp.float32)
    csum = jnp.cumsum(ug, axis=1)
    t = jnp.arange(s)
    pooled = []
    for g, w in enumerate(POOL_WINDOWS):
        cpad = jnp.pad(csum[:, :, g], ((0, 0), (w, 0), (0, 0)))
        wsum = cpad[:, w:] - cpad[:, :s]
        cnt = jnp.minimum(t + 1, w).astype(jnp.float32)
        pooled.append(wsum / cnt[None, :, None])
    mixed = (jnp.stack(pooled, axis=2) - ug).astype(u.dtype)
    y = jnp.einsum('bsgc,gcd->bsgd', mixed, pool_w)
    return y.reshape(b, s, POOL_WIDTH) * pool_scale


def moba_attention(q, k, v):
    b, h, s, d = q.shape
    scale = d ** -0.5
    nb = -(-s // MOBA_BLOCK)
    pad = nb * MOBA_BLOCK - s
    kp = jnp.pad(k, ((0, 0), (0, 0), (0, pad), (0, 0)))
    vp = jnp.pad(v, ((0, 0), (0, 0), (0, pad), (0, 0)))
    kb = kp.reshape(b, h, nb, MOBA_BLOCK, d)
    vb = vp.reshape(b, h, nb, MOBA_BLOCK, d)
    kmean = kb.astype(jnp.float32).mean(axis=3)
    gate = jnp.einsum('bhsd,bhnd->bhsn', q.astype(jnp.float32), kmean)
    qblk = jnp.arange(s) // MOBA_BLOCK
    past = jnp.arange(nb)[None, :] < qblk[:, None]
    gate = jnp.where(past, gate, -jnp.inf)
    n_sel = min(MOBA_TOPK, nb)
    _, sel = lax.top_k(gate, n_sel)
    nc = s // MOBA_Q_CHUNK
    qc = q.reshape(b, h, nc, MOBA_Q_CHUNK, d).transpose(2, 0, 1, 3, 4)
    selc = sel.reshape(b, h, nc, MOBA_Q_CHUNK, n_sel).transpose(2, 0, 1, 3, 4)
    b_i = jnp.arange(b)[:, None, None, None]
    h_i = jnp.arange(h)[None, :, None, None]

    def chunk(args):
        q_c, sel_c, i = args
        start = i * MOBA_Q_CHUNK
        blk = start // MOBA_BLOCK
        k_sel = kb[b_i, h_i, sel_c]
        v_sel = vb[b_i, h_i, sel_c]
        s_sel = jnp.einsum('bhqd,bhqnkd->bhqnk', q_c, k_sel).astype(jnp.float32) * scale
        s_sel = jnp.where((sel_c < blk)[..., None], s_sel, -jnp.inf)
        s_sel = s_sel.reshape(b, h, MOBA_Q_CHUNK, n_sel * MOBA_BLOCK)
        k_own = lax.dynamic_slice_in_dim(kp, blk * MOBA_BLOCK, MOBA_BLOCK, axis=2)
        v_own = lax.dynamic_slice_in_dim(vp, blk * MOBA_BLOCK, MOBA_BLOCK, axis=2)
        s_own = jnp.einsum('bhqd,bhkd->bhqk', q_c, k_own).astype(jnp.float32) * scale
        kpos = blk * MOBA_BLOCK + jnp.arange(MOBA_BLOCK)
        qpos = start + jnp.arange(MOBA_Q_CHUNK)
        s_own = jnp.where(kpos[None, :] <= qpos[:, None], s_own, -jnp.inf)
        p = jax.nn.softmax(jnp.concatenate([s_sel, s_own], axis=-1), axis=-1).astype(v.dtype)
        p_sel = p[..., :n_sel * MOBA_BLOCK].reshape(b, h, MOBA_Q_CHUNK, n_sel, MOBA_BLOCK)
        p_own = p[..., n_sel * MOBA_BLOCK:]
        return (jnp.einsum('bhqnk,bhqnkd->bhqd', p_sel, v_sel)
                + jnp.einsum('bhqk,bhkd->bhqd', p_own, v_own))

    out = lax.map(chunk, (qc, selc, jnp.arange(nc)))
    return out.transpose(1, 2, 0, 3, 4).reshape(b, h, s, d)


def even_mixer(x, w_in, b_forget, pool_w, pool_scale, w_out):
    hcat = jnp.einsum('bsd,de->bse', x, w_in)
    q, k, v, f, u = jnp.split(hcat, [FOX_WIDTH, 2 * FOX_WIDTH, 3 * FOX_WIDTH, 3 * FOX_WIDTH + FOX_HEADS], axis=-1)
    logf = jax.nn.log_sigmoid((f + b_forget).astype(jnp.float32)).transpose(0, 2, 1)
    a = merge_heads(fox_attention(split_heads(q, FOX_HEADS), split_heads(k, FOX_HEADS),
                                  split_heads(v, FOX_HEADS), logf))
    p = multiscale_pool(u, pool_w, pool_scale)
    return jnp.einsum('bse,ed->bsd', jnp.concatenate([a, p], axis=-1), w_out)


def odd_mixer(x, w_in, w_out):
    hcat = jnp.einsum('bsd,de->bse', x, w_in)
    q, k, v = jnp.split(hcat, 3, axis=-1)
    o = moba_attention(split_heads(q, MOBA_HEADS), split_heads(k, MOBA_HEADS), split_heads(v, MOBA_HEADS))
    return jnp.einsum('bse,ed->bsd', merge_heads(o), w_out)


def swiglu(x, w_gate, w_up, w_down):
    hid = jax.nn.silu(jnp.einsum('bsd,df->bsf', x, w_gate)) * jnp.einsum('bsd,df->bsf', x, w_up)
    return jnp.einsum('bsf,fd->bsd', hid, w_down)


def moe_ffn(x, w_router, w_gate, w_up, w_down):
    logits = jnp.einsum('bsd,de->bse', x, w_router).astype(jnp.float32)
    vals, idx = lax.top_k(logits, TOP_K)
    gates = jax.nn.softmax(vals, axis=-1)
    combine = jnp.sum(gates[..., None] * jax.nn.one_hot(idx, N_EXPERTS, dtype=jnp.float32), axis=-2)
    y = jnp.zeros_like(x)
    for e in range(N_EXPERTS):
        y = y + combine[..., e:e + 1].astype(x.dtype) * swiglu(x, w_gate[e], w_up[e], w_down[e])
    return y


def setup_inputs(seed: int = 0) -> dict:
    key = jax.random.key(seed)
    ks = jax.random.split(key, 20)
    nrm = jax.random.normal
    f32 = jnp.float32
    sd = D_MODEL ** -0.5
    sf = D_FF ** -0.5
    return {
        'x': nrm(ks[0], (BATCH, SEQ, D_MODEL), f32),
        'ln1_g': 1.0 + 0.02 * nrm(ks[1], (DEPTH, D_MODEL), f32),
        'ln1_b': 0.02 * nrm(ks[2], (DEPTH, D_MODEL), f32),
        'ln2_g': 1.0 + 0.02 * nrm(ks[3], (DEPTH, D_MODEL), f32),
        'ln2_b': 0.02 * nrm(ks[4], (DEPTH, D_MODEL), f32),
        'ev_w_in': sd * nrm(ks[5], (N_EVEN, D_MODEL, EVEN_IN), f32),
        'ev_b_forget': jnp.linspace(1.0, 4.0, FOX_HEADS, dtype=f32)[None, :] + 0.1 * nrm(ks[6], (N_EVEN, FOX_HEADS), f32),
        'ev_pool_w': POOL_GROUP_DIM ** -0.5 * nrm(ks[7], (N_EVEN, POOL_GROUPS, POOL_GROUP_DIM, POOL_GROUP_DIM), f32),
        'ev_pool_scale': 1.0 + 0.1 * nrm(ks[8], (N_EVEN, POOL_WIDTH), f32),
        'ev_w_out': DEEPNORM_BETA * sd * nrm(ks[9], (N_EVEN, FOX_WIDTH + POOL_WIDTH, D_MODEL), f32),
        'ev_ffn_gate': sd * nrm(ks[10], (N_EVEN, D_MODEL, D_FF), f32),
        'ev_ffn_up': sd * nrm(ks[11], (N_EVEN, D_MODEL, D_FF), f32),
        'ev_ffn_down': DEEPNORM_BETA * sf * nrm(ks[12], (N_EVEN, D_FF, D_MODEL), f32),
        'od_w_in': sd * nrm(ks[13], (N_ODD, D_MODEL, 3 * MOBA_WIDTH), f32),
        'od_w_out': DEEPNORM_BETA * MOBA_WIDTH ** -0.5 * nrm(ks[14], (N_ODD, MOBA_WIDTH, D_MODEL), f32),
        'od_router': sd * nrm(ks[15], (N_ODD, D_MODEL, N_EXPERTS), f32),
        'od_exp_gate': sd * nrm(ks[16], (N_ODD, N_EXPERTS, D_MODEL, D_FF), f32),
        'od_exp_up': sd * nrm(ks[17], (N_ODD, N_EXPERTS, D_MODEL, D_FF), f32),
        'od_exp_down': DEEPNORM_BETA * sf * nrm(ks[18], (N_ODD, N_EXPERTS, D_FF, D_MODEL), f32),
    }


def reference(x, ln1_g, ln1_b, ln2_g, ln2_b, ev_w_in, ev_b_forget, ev_pool_w, ev_pool_scale, ev_w_out,
              ev_ffn_gate, ev_ffn_up, ev_ffn_down, od_w_in, od_w_out, od_router, od_exp_gate, od_exp_up,
              od_exp_down):
    for layer in range(DEPTH):
        i = layer // 2
        if layer % 2 == 0:
            mix = even_mixer(x, ev_w_in[i], ev_b_forget[i], ev_pool_w[i], ev_pool_scale[i], ev_w_out[i])
        else:
            mix = odd_mixer(x, od_w_in[i], od_w_out[i])
        x = layer_norm(DEEPNORM_ALPHA * x + mix, ln1_g[layer], ln1_b[layer])
        if layer % 2 == 0:
            ffn = swiglu(x, ev_ffn_gate[i], ev_ffn_up[i], ev_ffn_down[i])
        else:
            ffn = moe_ffn(x, od_router[i], od_exp_gate[i], od_exp_up[i], od_exp_down[i])
        x = layer_norm(DEEPNORM_ALPHA * x + ffn, ln2_g[layer], ln2_b[layer])
    return x
```

```python
import numpy as np
import ml_dtypes
import concourse.bass as bass
import concourse.mybir as mybir
from concourse.bass_utils import run_bass_kernel_spmd

F32 = mybir.dt.float32
BF16 = mybir.dt.bfloat16
I32 = mybir.dt.int32
SPARSE = True
AF = mybir.ActivationFunctionType
ALU = mybir.AluOpType

D = 1024
DFF = 3584
NE = 8
ALPHA = float(8 ** 0.25)
EPS = 1e-5
POOL_W = (2, 4, 8, 16)
NEG = -30000.0
ARENA_BYTES = 206 * 1024


class Buf:
    __slots__ = ("name", "w", "r", "sem")

    def __init__(self, name, sem=None):
        self.name = name
        self.w = None
        self.r = {}
        self.sem = sem


class Eng:
    def __init__(self, name, sem):
        self.name = name
        self.sem = sem
        self.n = 0
        self.seen = {}
        self.prog = []


class Kern:
    def __init__(self, nc, sems):
        self.nc = nc
        self.sems = sems
        self.semcnt = [0] * len(sems)
        self.free = list(range(5, len(sems) - 12))
        self.free_sw = list(range(len(sems) - 12, len(sems)))
        self.phase_sems_sw = []
        self.E = {n: Eng(n, i) for i, n in enumerate(["pe", "act", "dve", "pool", "sp"])}
        self.phase_sems = []

    def buf(self, name, dma=False):
        s = None
        if dma == "sw":
            s = self.free_sw.pop()
            self.phase_sems_sw.append(s)
        elif dma:
            s = self.free.pop()
            self.phase_sems.append(s)
        return Buf(name, s)

    def end_phase(self):
        self.barrier()
        self.free.extend(self.phase_sems)
        self.phase_sems = []
        self.free_sw.extend(self.phase_sems_sw)
        self.phase_sems_sw = []

    def _waits(self, eng, R, W):
        deps = {}

        def add(s, v):
            if deps.get(s, 0) < v:
                deps[s] = v

        for b in R:
            if b.w is not None:
                add(*b.w)
        for b in W:
            if b.w is not None:
                add(*b.w)
            for s, v in b.r.items():
                add(s, v)
        waits = []
        for s, v in deps.items():
            if s == eng.sem and eng.name == "pe":
                continue
            if eng.seen.get(s, 0) < v:
                eng.seen[s] = v
                waits.append((s, v))
        return waits

    def op(self, en, fn, R=(), W=()):
        eng = self.E[en]
        waits = self._waits(eng, R, W)
        eng.n += 1
        tok = (eng.sem, eng.n)
        eng.prog.append((waits, fn, eng.sem, 1))
        for b in R:
            if b.r.get(tok[0], 0) < tok[1]:
                b.r[tok[0]] = tok[1]
        for b in W:
            b.w = tok
            b.r = {}

    def dma(self, qn, out, in_, R=(), W=(), dst=None):
        eng = self.E[qn]
        waits = self._waits(eng, R, W)
        s = dst.sem
        self.semcnt[s] += 16
        tok = (s, self.semcnt[s])
        eng.prog.append((waits, (lambda h, o=out, i=in_: h.dma_start(out=o, in_=i)), s, 16))
        for b in R:
            if b.r.get(s, 0) < tok[1]:
                b.r[s] = tok[1]
        for b in W:
            b.w = tok
            b.r = {}

    def idma(self, qn, fn, R=(), W=(), dst=None):
        eng = self.E[qn]
        waits = self._waits(eng, R, W)
        s = dst.sem
        self.semcnt[s] += 16
        tok = (s, self.semcnt[s])
        eng.prog.append((waits, fn, s, 16))
        for b in R:
            if b.r.get(s, 0) < tok[1]:
                b.r[s] = tok[1]
        for b in W:
            b.w = tok
            b.r = {}

    def barrier(self):
        for en, eng in self.E.items():
            waits = []
            for o in self.E.values():
                if o is eng or o.n == 0:
                    continue
                if eng.seen.get(o.sem, 0) < o.n:
                    eng.seen[o.sem] = o.n
                    waits.append((o.sem, o.n))
            for s in range(5, len(self.sems)):
                v = self.semcnt[s]
                if v and eng.seen.get(s, 0) < v:
                    eng.seen[s] = v
                    waits.append((s, v))
            if waits:
                eng.prog.append((waits, None, None, 0))

    def mm(self, out, lhsT, rhs, start, stop, R, W):
        self.op("pe", lambda h: h.matmul(out, lhsT, rhs, start=start, stop=stop), R, W)

    def tr(self, out, in_, ident, R, W):
        self.op("pe", lambda h: h.transpose(out, in_, ident), R, W)

    def act(self, out, in_, func, R, W, bias=None, scale=None):
        kw = {}
        if bias is not None:
            kw["bias"] = bias
        if scale is not None:
            kw["scale"] = scale
        self.op("act", lambda h: h.activation(out=out, in_=in_, func=func, **kw), R, W)

    def replay(self, en, h):
        for waits, fn, s, inc in self.E[en].prog:
            for ws, wv in waits:
                h.wait_ge(self.sems[ws], wv)
            if fn is not None:
                fn(h).then_inc(self.sems[s], inc)


class Arena:
    def __init__(self, t):
        self.t = t
        self.off = 0

    def reset(self):
        self.off = 0

    def alloc(self, shape, dt):
        esz = 2 if dt == BF16 else 4
        n = 1
        for s in shape[1:]:
            n *= s
        nbytes = (n * esz + 63) // 64 * 64
        a = self.off
        self.off += nbytes
        assert self.off <= ARENA_BYTES, ("arena overflow", self.off)
        v = self.t[0:shape[0], a // 2:(a + n * esz) // 2]
        if dt != BF16:
            v = v.bitcast(dt)
        if len(shape) == 3:
            v = v.rearrange("p (a b) -> p a b", a=shape[1])
        elif len(shape) == 4:
            v = v.rearrange("p (a b c) -> p a b c", a=shape[1], b=shape[2])
        return v


def build_nc(S, layers, n_layers_total, dbg=False, stop=None):
    NT = S // 128
    NG = S // 512
    NBLK = S // 1024
    nc = bass.Bass("TRN2", target_bir_lowering=False)

    def din(name, shape, dt=F32):
        return nc.dram_tensor(name, shape, dt, kind="ExternalInput").ap()

    def dscr(name, shape, dt):
        return nc.dram_tensor(name, shape, dt, kind=("ExternalOutput" if dbg else "Internal")).ap()

    x_in = din("x", [S, D])
    lng = [din("ln1_g", [4, 128, D]), din("ln2_g", [4, 128, D])]
    lnb = [din("ln1_b", [4, 128, D]), din("ln2_b", [4, 128, D])]
    ev_w_in = din("ev_w_in", [2, D, 2056])
    ev_negb = din("ev_negb", [2, 8, 1])
    ev_pool_w = din("ev_pool_w", [2, 4, 128, 128])
    ev_pool_scale = din("ev_pool_scale", [2, 4, 128, 1])
    ev_w_out = din("ev_w_out", [2, D, D])
    ev_g = din("ev_ffn_gate", [2, 1, D, DFF])
    ev_u = din("ev_ffn_up", [2, 1, D, DFF])
    ev_d = din("ev_ffn_down", [2, 1, DFF, D])
    od_w_in = din("od_w_in", [2, D, 3072])
    od_w_out = din("od_w_out", [2, D, D])
    od_router = din("od_router", [2, 8, 128, D])
    od_g = din("od_exp_gate", [2, NE, D, DFF])
    od_u = din("od_exp_up", [2, NE, D, DFF])
    od_d = din("od_exp_down", [2, NE, DFF, D])
    c_ident_bf = din("c_ident_bf", [128, 128], BF16)
    c_ident_f = din("c_ident_f", [128, 128])
    c_tri = din("c_tri", [128, 128], BF16)
    c_koh = din("c_koh", [16, S], BF16)
    c_past = din("c_past", [NG, 128, 4, 16])
    c_own = din("c_own", [NG, 128, 4, 16])
    c_invcnt = din("c_invcnt", [4, 128, 16])
    NTILE = 2 * S // 512 + 8
    NSLOT = NTILE * 512
    c_ustrict = din("c_ustrict", [128, 128], BF16)
    c_misc = din("c_misc", [128, 64])
    y_out = nc.dram_tensor("y", [S, D], F32, kind="ExternalOutput").ap()

    XR = [dscr("xr0", [S, D], F32), dscr("xr1", [S, D], F32)]
    XT = dscr("xT", [D, S], BF16)
    QT = dscr("qT", [D, S], BF16)
    KT = dscr("kT", [D, S], BF16)
    VV = dscr("vv", [S, D], BF16)
    CATT = dscr("catT", [D, S], BF16)
    FAUG = dscr("faug", [8, 3, S], BF16)
    NFC = dscr("nfc", [128, NT * 8], F32)
    MB = dscr("mb", [256, S], BF16)
    COMB = dscr("comb", [S, 8], F32)
    ROUTE = dscr("route", [S, 24], F32)
    CNT = dscr("cnt", [128, 8], F32)
    XS = dscr("xs", [NSLOT, D], BF16)
    YS = dscr("ys", [NSLOT, D], F32)

    from contextlib import ExitStack
    with ExitStack() as st:
        arena_t = st.enter_context(nc.sbuf_tensor("arena", [128, ARENA_BYTES // 2], BF16))
        PS = [st.enter_context(nc.psum_tensor("ps%d" % i, [128, 512], F32)) for i in range(7)]
        PSB = st.enter_context(nc.psum_tensor("psb", [128, 1024], BF16))
        NSEM = 60
        sems = [st.enter_context(nc.semaphore("s%d" % i)) for i in range(NSEM)]
        K = Kern(nc, sems)
        A = Arena(arena_t)
        psb = [K.buf("ps%d" % i) for i in range(7)]
        psbb = K.buf("psb")
        PSBv = PSB[:, :].rearrange("p (a b) -> p a b", a=8)

        B_xin = K.buf("x_in")
        B_w = K.buf("weights")
        B_XR = [K.buf("xr0", True), K.buf("xr1", True)]
        B_XT = K.buf("xT", True)
        B_QT = K.buf("qT", True)
        B_KT = K.buf("kT", True)
        B_VV = K.buf("vv", True)
        B_CATT = K.buf("catT", True)
        B_FAUG = K.buf("faug", True)
        B_NFC = K.buf("nfc", True)
        B_MB = K.buf("mb", True)
        B_COMB = K.buf("comb", True)
        B_ROUTE = K.buf("route", True)
        B_CNT = K.buf("cnt", True)
        B_XS = K.buf("xs", "sw")
        B_YS = K.buf("ys", True)
        B_Y = K.buf("y", True)
        K.phase_sems = []
        K.phase_sems_sw = []

        def consts_common():
            ident = A.alloc([128, 128], BF16)
            b = K.buf("ident", True)
            K.dma("sp", ident, c_ident_bf[:, :], R=[B_w], W=[b], dst=b)
            return ident, b

        def emit_xT(xb_ap, xb_buf, tt, ident, identb, stg, stgb):
            for kt in range(8):
                K.tr(PSBv[:, kt, :], xb_ap[:, kt * 128:(kt + 1) * 128], ident, R=[xb_buf, identb], W=[psbb])
            K.op("dve", lambda h: h.tensor_copy(out=stg, in_=PSBv), R=[psbb], W=[stgb])
            K.dma("sp", XT[:, tt * 128:(tt + 1) * 128].rearrange("(kt p) t -> p kt t", p=128), stg,
                  R=[stgb], W=[B_XT], dst=B_XT)

        def layer_norm(z, zb, which, L, small, smallb, g_ap, b_ap, gbb):
            st6 = small[:, 0:12].rearrange("p (a b) -> p a b", a=2)
            K.op("dve", lambda h: h.bn_stats(out=st6[:, 0, :], in_=z[:, 0:512]), R=[zb], W=[smallb])
            K.op("dve", lambda h: h.bn_stats(out=st6[:, 1, :], in_=z[:, 512:1024]), R=[zb], W=[smallb])
            K.op("dve", lambda h: h.bn_aggr(out=small[:, 12:14], in_=small[:, 0:12]), R=[smallb], W=[smallb])
            K.act(small[:, 14:15], small[:, 13:14], AF.Sqrt, R=[smallb], W=[smallb], bias=EPS, scale=1.0)
            K.op("dve", lambda h: h.reciprocal(out=small[:, 15:16], in_=small[:, 14:15]), R=[smallb], W=[smallb])
            K.op("dve", lambda h: h.tensor_scalar(out=z, in0=z, scalar1=small[:, 12:13], scalar2=small[:, 15:16],
                                                  op0=ALU.subtract, op1=ALU.mult), R=[zb, smallb], W=[zb])
            K.op("pool", lambda h: h.tensor_tensor(out=z, in0=z, in1=g_ap, op=ALU.mult), R=[zb, gbb], W=[zb])
            K.op("pool", lambda h: h.tensor_tensor(out=z, in0=z, in1=b_ap, op=ALU.add), R=[zb, gbb], W=[zb])

        def phase_p0():
            A.reset()
            ident, identb = consts_common()
            xt = [A.alloc([128, D], F32) for _ in range(2)]
            xtb = [K.buf("p0x%d" % i, True) for i in range(2)]
            xb = [A.alloc([128, D], BF16) for _ in range(2)]
            xbb = [K.buf("p0xb%d" % i) for i in range(2)]
            stg = [A.alloc([128, 8, 128], BF16) for _ in range(2)]
            stgb = [K.buf("p0s%d" % i) for i in range(2)]
            for tt in range(NT):
                i = tt % 2
                K.dma("sp", xt[i], x_in[tt * 128:(tt + 1) * 128, :], R=[B_xin], W=[xtb[i]], dst=xtb[i])
                K.act(xb[i], xt[i], AF.Copy, R=[xtb[i]], W=[xbb[i]])
                emit_xT(xb[i], xbb[i], tt, ident, identb, stg[i], stgb[i])
            K.end_phase()

        def phase_p1(L):
            even = (L % 2 == 0)
            li = L // 2
            A.reset()
            xTs = A.alloc([128, 8, S], BF16)
            xTsb = [K.buf("xTs%d" % g, True) for g in range(NG)]
            for g in range(NG):
                K.dma("sp", xTs[:, :, g * 512:(g + 1) * 512],
                      XT[:, g * 512:(g + 1) * 512].rearrange("(kt p) t -> p kt t", p=128),
                      R=[B_XT], W=[xTsb[g]], dst=xTsb[g])
            w_in = ev_w_in if even else od_w_in
            nqc = 4 if even else 8
            qoff, koff, voff = (0, 512, 1024) if even else (0, 1024, 2048)
            nvh = 1 if even else 2
            wslot = [A.alloc([128, 8, 512], BF16) for _ in range(2)]
            wslotb = [K.buf("wslot%d" % i, "sw") for i in range(2)]
            stg = [A.alloc([128, 512], BF16) for _ in range(3)]
            stgb = [K.buf("p1stg%d" % i) for i in range(3)]
            cnt = {"w": 0, "s": 0, "p": 0}

            def load_w(col0, ncols=512):
                i = cnt["w"] % 2
                cnt["w"] += 1
                K.dma("pool", wslot[i][:, :, 0:ncols],
                      w_in[li, :, col0:col0 + ncols].rearrange("(kt p) f -> p kt f", p=128),
                      R=[B_w], W=[wslotb[i]], dst=wslotb[i])
                return wslot[i], wslotb[i]

            def next_ps():
                i = cnt["p"] % 6
                cnt["p"] += 1
                return PS[i], psb[i]

            def next_stg():
                i = cnt["s"] % 3
                cnt["s"] += 1
                return stg[i], stgb[i]

            if not even:
                ident, identb = consts_common()
                kmT = A.alloc([128, 16], BF16)
                kmTb = K.buf("kmT")
                kmf = A.alloc([128, 16], F32)
                kmfb = K.buf("kmf")
                K.op("pool", lambda h: h.memset(kmf, 0.0), R=[], W=[kmfb])
                gm = A.alloc([128, 4, 2, 16], F32)
                gmb = K.buf("gm")
                m8 = A.alloc([128, 8, 8], F32)
                m8b = K.buf("m8")
                selb_t = A.alloc([128, 4, 2, 16], F32)
                selbb = K.buf("sel")
                mbt = A.alloc([128, 4, 2, 16], BF16)
                mbtb = K.buf("mbt")
                mbs = A.alloc([32, 512], BF16)
                mbsb = K.buf("mbs")
                cpast = A.alloc([128, NG, 4, 16], F32)
                cown = A.alloc([128, NG, 4, 16], F32)
                cpb = K.buf("cpast", True)
                for g in range(NG):
                    K.dma("sp", cpast[:, g], c_past[g], R=[B_w], W=[cpb], dst=cpb)
                    K.dma("sp", cown[:, g], c_own[g], R=[B_w], W=[cpb], dst=cpb)

            for which in ("k", "q"):
                off = koff if which == "k" else qoff
                dstT, dstB = (KT, B_KT) if which == "k" else (QT, B_QT)
                for cg in range(nqc // 4):
                    wt, wtb = load_w(off + cg * 512)
                    for c4 in range(4):
                        ct = cg * 4 + c4
                        for g in range(NG):
                            ps, pb = next_ps()
                            for kt in range(8):
                                K.mm(ps[:, :], wt[:, kt, c4 * 128:(c4 + 1) * 128], xTs[:, kt, g * 512:(g + 1) * 512],
                                     kt == 0, kt == 7, R=[wtb, xTsb[g]], W=[pb])
                            sg, sgb = next_stg()
                            if which == "k":
                                if even:
                                    K.act(sg, ps[:, :], AF.Copy, R=[pb], W=[sgb])
                                else:
                                    for bb in range(2):
                                        K.op("act", lambda h, sg=sg, ps=ps, bb=bb, g=g: h.activation(
                                            out=sg[:, bb * 256:(bb + 1) * 256], in_=ps[:, bb * 256:(bb + 1) * 256], func=AF.Copy,
                                            accum_out=kmf[:, 2 * g + bb:2 * g + bb + 1]), R=[pb], W=[sgb, kmfb])
                            else:
                                K.act(sg, ps[:, :], AF.Copy, R=[pb], W=[sgb], scale=0.125)
                            K.dma("sp", dstT[ct * 128:(ct + 1) * 128, g * 512:(g + 1) * 512], sg,
                                  R=[sgb], W=[dstB], dst=dstB)
                            if (not even) and which == "q":
                                gp, gpb = next_ps()
                                for t4 in range(4):
                                    K.mm(gp[:, t4 * 32:(t4 + 1) * 32], sg[:, t4 * 128:(t4 + 1) * 128], kmTs[ct][:, :], True, True,
                                         R=[sgb, kmTsb[ct]], W=[gpb])
                                gpv = gp[:, 0:128].rearrange("p (a b c) -> p a b c", a=4, b=2)
                                for hh in range(2):
                                    K.op("dve", lambda h, o=gm[:, :, hh, :], i=gpv[:, :, hh, :], c=cpast[:, g]:
                                         h.tensor_tensor(out=o, in0=i, in1=c, op=ALU.add), R=[gpb, cpb], W=[gmb])
                                for t4 in range(4):
                                    for hh in range(2):
                                        K.op("dve", lambda h, o=m8[:, t4 * 2 + hh, :], i=gm[:, t4, hh, :]: h.max(out=o, in_=i),
                                             R=[gmb], W=[m8b])
                                for t4 in range(4):
                                    for hh in range(2):
                                        K.op("dve", lambda h, o=selb_t[:, t4, hh, :], i=gm[:, t4, hh, :], s=m8[:, t4 * 2 + hh, 2:3]:
                                             h.tensor_scalar(out=o, in0=i, scalar1=s, scalar2=None, op0=ALU.is_ge),
                                             R=[gmb, m8b], W=[selbb])
                                for hh in range(2):
                                    K.op("dve", lambda h, o=selb_t[:, :, hh, :], c=cown[:, g]:
                                         h.tensor_tensor(out=o, in0=o, in1=c, op=ALU.max), R=[selbb, cpb], W=[selbb])
                                K.op("dve", lambda h: h.tensor_scalar(out=mbt, in0=selb_t, scalar1=-NEG, scalar2=NEG,
                                                                      op0=ALU.mult, op1=ALU.add), R=[selbb], W=[mbtb])
                                for t4 in range(4):
                                    K.tr(PSB[0:32, t4 * 128:(t4 + 1) * 128],
                                         mbt[:, t4, :, :].rearrange("p a b -> p (a b)"), ident, R=[mbtb, identb], W=[psbb])
                                K.op("dve", lambda h: h.tensor_copy(out=mbs, in_=PSB[0:32, 0:512]), R=[psbb], W=[mbsb])
                                K.dma("sp", MB[ct * 32:(ct + 1) * 32, g * 512:(g + 1) * 512], mbs, R=[mbsb], W=[B_MB], dst=B_MB)
                        if (not even) and which == "k":
                            if ct == 0:
                                kmTs = [A.alloc([128, 32], BF16) for _ in range(nqc)]
                                kmTsb = [K.buf("kmTs%d" % i) for i in range(nqc)]
                                for i_ in range(nqc):
                                    K.op("pool", lambda h, o=kmTs[i_]: h.memset(o, 0.0), R=[], W=[kmTsb[i_]])
                            for hh in range(2):
                                K.op("dve", lambda h, o=kmTs[ct][hh * 64:(hh + 1) * 64, hh * 16:(hh + 1) * 16], i_=kmf[hh * 64:(hh + 1) * 64, :]:
                                     h.tensor_scalar(out=o, in0=i_, scalar1=1.0 / 256, scalar2=None, op0=ALU.mult),
                                     R=[kmfb], W=[kmTsb[ct]])

            for vh in range(nvh):
                wt, wtb = load_w(voff + vh * 512)
                for tt in range(NT):
                    ps, pb = next_ps()
                    g = tt // 4
                    for kt in range(8):
                        K.mm(ps[:, :], xTs[:, kt, tt * 128:(tt + 1) * 128], wt[:, kt, :], kt == 0, kt == 7,
                             R=[wtb, xTsb[g]], W=[pb])
                    sg, sgb = next_stg()
                    K.act(sg, ps[:, :], AF.Copy, R=[pb], W=[sgb])
                    K.dma("sp", VV[tt * 128:(tt + 1) * 128, vh * 512:(vh + 1) * 512], sg, R=[sgb], W=[B_VV], dst=B_VV)

            if even:
                wt, wtb = load_w(1536, 8)
                negb = A.alloc([8, 1], F32)
                negbb = K.buf("negb", True)
                K.dma("sp", negb, ev_negb[li], R=[B_w], W=[negbb], dst=negbb)
                K.op("dve", lambda h: h.tensor_scalar(out=negb, in0=negb, scalar1=-1.0, scalar2=None, op0=ALU.mult),
                     R=[negbb], W=[negbb])
                f_mark = A.off
                cs = [A.alloc([8, S], F32) for _ in range(2)]
                csb = [K.buf("cs%d" % i) for i in range(2)]
                for g in range(NG):
                    ps, pb = next_ps()
                    for kt in range(8):
                        K.mm(ps[0:8, :], wt[:, kt, 0:8], xTs[:, kt, g * 512:(g + 1) * 512], kt == 0, kt == 7,
                             R=[wtb, xTsb[g]], W=[pb])
                    K.act(cs[1][:, g * 512:(g + 1) * 512], ps[0:8, :], AF.Exp, R=[pb, negbb], W=[csb[1]], bias=negb, scale=-1.0)
                K.act(cs[0], cs[1], AF.Ln, R=[csb[1]], W=[csb[0]], bias=1.0, scale=1.0)
                cur = 0
                sh = 1
                while sh < S:
                    o, i_ = cs[1 - cur], cs[cur]
                    K.op("dve", lambda h, o=o, i_=i_, sh=sh: h.tensor_tensor(out=o[:, sh:S], in0=i_[:, sh:S], in1=i_[:, 0:S - sh], op=ALU.add),
                         R=[csb[cur]], W=[csb[1 - cur]])
                    K.op("dve", lambda h, o=o, i_=i_, sh=sh: h.tensor_copy(out=o[:, 0:sh], in_=i_[:, 0:sh]),
                         R=[csb[cur]], W=[csb[1 - cur]])
                    cur = 1 - cur
                    sh *= 2
                C, Cb = cs[cur], csb[cur]
                r1, r1b = cs[1 - cur], csb[1 - cur]
                hb = [A.alloc([8, S], BF16) for _ in range(3)]
                hbb = [K.buf("hb%d" % i) for i in range(3)]
                h32 = A.alloc([8, S], F32)
                h32b = K.buf("h32")
                K.op("dve", lambda h: h.tensor_scalar(out=hb[0], in0=C, scalar1=-1.0, scalar2=None, op0=ALU.mult), R=[Cb], W=[hbb[0]])
                K.op("dve", lambda h: h.tensor_copy(out=h32, in_=hb[0]), R=[hbb[0]], W=[h32b])
                K.op("dve", lambda h: h.scalar_tensor_tensor(out=r1, in0=C, scalar=-1.0, in1=h32, op0=ALU.mult, op1=ALU.subtract),
                     R=[Cb, h32b], W=[r1b])
                K.op("dve", lambda h: h.tensor_copy(out=hb[1], in_=r1), R=[r1b], W=[hbb[1]])
                K.op("dve", lambda h: h.tensor_copy(out=h32, in_=hb[1]), R=[hbb[1]], W=[h32b])
                K.op("dve", lambda h: h.tensor_tensor(out=r1, in0=r1, in1=h32, op=ALU.subtract), R=[r1b, h32b], W=[r1b])
                K.op("dve", lambda h: h.tensor_copy(out=hb[2], in_=r1), R=[r1b], W=[hbb[2]])
                for j in range(3):
                    K.dma("sp", FAUG[:, j, :], hb[j], R=[hbb[j]], W=[B_FAUG], dst=B_FAUG)
                identf = A.alloc([8, 8], F32)
                identfb = K.buf("identf", True)
                K.dma("sp", identf, c_ident_f[0:8, 0:8], R=[B_w], W=[identfb], dst=identfb)
                ps, pb = next_ps()
                for kt in range(NT):
                    K.tr(ps[:, kt * 8:(kt + 1) * 8], C[:, kt * 128:(kt + 1) * 128], identf, R=[Cb, identfb], W=[pb])
                nfc = A.alloc([128, NT * 8], F32)
                nfcb = K.buf("nfc_s")
                K.op("dve", lambda h, ps=ps: h.tensor_copy(out=nfc, in_=ps[:, 0:NT * 8]), R=[pb], W=[nfcb])
                K.dma("sp", NFC[:, :], nfc, R=[nfcb], W=[B_NFC], dst=B_NFC)
                K.barrier()
                A.off = f_mark

                wt, wtb = load_w(1544)
                pw = A.alloc([128, 4, 128], BF16)
                pwb = K.buf("pw", "sw")
                for g4 in range(4):
                    K.dma("pool", pw[:, g4, :], ev_pool_w[li, g4], R=[B_w], W=[pwb], dst=pwb)
                psc = A.alloc([128, 4], F32)
                pscb = K.buf("psc", True)
                for g4 in range(4):
                    K.dma("sp", psc[:, g4:g4 + 1], ev_pool_scale[li, g4], R=[B_w], W=[pscb], dst=pscb)
                icn = A.alloc([128, 4, 16], F32)
                icnb = K.buf("icn", True)
                for g4 in range(4):
                    K.dma("sp", icn[:, g4, :], c_invcnt[g4], R=[B_w], W=[icnb], dst=icnb)
                uu = A.alloc([128, S], F32)
                uub = K.buf("uu")
                sa = [A.alloc([128, S], F32) for _ in range(2)]
                sab = [K.buf("sa%d" % i) for i in range(2)]
                mx = A.alloc([128, S], BF16)
                mxb = K.buf("mx")
                fix = A.alloc([128, 16], F32)
                fixb = K.buf("fix")
                for g4 in range(4):
                    w = POOL_W[g4]
                    for g in range(NG):
                        ps, pb = next_ps()
                        for kt in range(8):
                            K.mm(ps[:, :], wt[:, kt, g4 * 128:(g4 + 1) * 128], xTs[:, kt, g * 512:(g + 1) * 512],
                                 kt == 0, kt == 7, R=[wtb, xTsb[g]], W=[pb])
                        K.act(uu[:, g * 512:(g + 1) * 512], ps[:, :], AF.Copy, R=[pb], W=[uub])
                    src, srcb = uu, uub
                    sh = 1
                    k = 0
                    while sh < w:
                        o, ob = sa[k % 2], sab[k % 2]
                        K.op("dve", lambda h, o=o, i_=src, sh=sh: h.tensor_tensor(out=o[:, sh:S], in0=i_[:, sh:S], in1=i_[:, 0:S - sh], op=ALU.add),
                             R=[srcb], W=[ob])
                        K.op("pool", lambda h, o=o, i_=src, sh=sh: h.tensor_copy(out=o[:, 0:sh], in_=i_[:, 0:sh]), R=[srcb], W=[ob])
                        src, srcb = o, ob
                        sh *= 2
                        k += 1
                    K.op("dve", lambda h, s_=src, w=w: h.scalar_tensor_tensor(out=mx, in0=s_, scalar=1.0 / w, in1=uu, op0=ALU.mult, op1=ALU.subtract),
                         R=[srcb, uub], W=[mxb])
                    K.op("dve", lambda h, s_=src, w=w, g4=g4: h.tensor_tensor(out=fix[:, 0:w - 1], in0=s_[:, 0:w - 1], in1=icn[:, g4, 0:w - 1], op=ALU.mult),
                         R=[srcb, icnb], W=[fixb])
                    K.op("dve", lambda h, w=w: h.tensor_tensor(out=mx[:, 0:w - 1], in0=fix[:, 0:w - 1], in1=uu[:, 0:w - 1], op=ALU.subtract),
                         R=[fixb, uub, mxb], W=[mxb])
                    for g in range(NG):
                        ps, pb = next_ps()
                        K.mm(ps[:, :], pw[:, g4, :], mx[:, g * 512:(g + 1) * 512], True, True, R=[pwb, mxb], W=[pb])
                        sg, sgb = next_stg()
                        K.op("dve", lambda h, sg=sg, ps=ps, g4=g4: h.tensor_scalar(out=sg, in0=ps[:, :], scalar1=psc[:, g4:g4 + 1], scalar2=None, op0=ALU.mult),
                             R=[pb, pscb], W=[sgb])
                        K.dma("sp", CATT[512 + g4 * 128:512 + (g4 + 1) * 128, g * 512:(g + 1) * 512], sg, R=[sgb], W=[B_CATT], dst=B_CATT)
            K.end_phase()

        def phase_p2(L):
            even = (L % 2 == 0)
            H = 8 if even else 16
            aug = 3 if even else 16
            KA = 64 + aug
            A.reset()
            qa = [A.alloc([128, S], BF16) for _ in range(2)]
            ka = [A.alloc([128, S], BF16) for _ in range(2)]
            va = [A.alloc([128, NT, 128], BF16) for _ in range(2)]
            qab = [K.buf("qa%d" % i, True) for i in range(2)]
            kab = [K.buf("ka%d" % i, True) for i in range(2)]
            vab = [K.buf("va%d" % i, True) for i in range(2)]
            pt = [A.alloc([128, 512], BF16) for _ in range(4)]
            ptb = [K.buf("pt%d" % i) for i in range(4)]
            rr = [A.alloc([128, 512], F32) for _ in range(2)]
            rrb = [K.buf("rr%d" % i) for i in range(2)]
            ot = [A.alloc([64, 512], BF16) for _ in range(2)]
            otb = [K.buf("ot%d" % i) for i in range(2)]
            tri = A.alloc([128, 128], BF16)
            trib = K.buf("tri", True)
            K.dma("sp", tri, c_tri[:, :], R=[B_w], W=[trib], dst=trib)
            if even:
                nfc = A.alloc([128, NT * 8], F32)
                nfcb = K.buf("nfc2", True)
                K.dma("sp", nfc, NFC[:, :], R=[B_NFC], W=[nfcb], dst=nfcb)
            for i in range(2):
                K.op("pool", lambda h, i=i: h.memset(va[i][:, :, 64:128], 1.0), R=[], W=[vab[i]])
                if even:
                    K.op("pool", lambda h, i=i: h.memset(ka[i][64:67, :], 1.0), R=[], W=[kab[i]])
                else:
                    K.dma("sp", ka[i][64:80, :], c_koh[:, :], R=[B_w], W=[kab[i]], dst=kab[i])

            def load_head(hd):
                b = hd % 2
                K.dma("sp", qa[b][0:64, :], QT[hd * 64:(hd + 1) * 64, :], R=[B_QT], W=[qab[b]], dst=qab[b])
                if even:
                    K.dma("sp", qa[b][64:67, :], FAUG[hd], R=[B_FAUG], W=[qab[b]], dst=qab[b])
                else:
                    K.dma("sp", qa[b][64:80, :], MB[hd * 16:(hd + 1) * 16, :], R=[B_MB], W=[qab[b]], dst=qab[b])
                K.dma("sp", ka[b][0:64, :], KT[hd * 64:(hd + 1) * 64, :], R=[B_KT], W=[kab[b]], dst=kab[b])
                for j in range(S // 1024):
                    K.dma("sp", va[b][:, 8 * j:8 * j + 8, 0:64],
                          VV[j * 1024:(j + 1) * 1024, hd * 64:(hd + 1) * 64].rearrange("(kt p) d -> p kt d", p=128),
                          R=[B_VV], W=[vab[b]], dst=vab[b])

            its = []
            for hd in range(H):
                for g in range(NG):
                    for kt in range(4 * g + 4):
                        its.append((hd, g, kt))
            SPS = [(PS[i], psb[i]) for i in range(3)]
            OPS = [(PS[3 + i], psb[3 + i]) for i in range(2)]

            def emit_s(idx):
                hd, g, kt = its[idx]
                b = hd % 2
                j = kt - 4 * g
                c0 = 128 * j if j > 0 else 0
                n = 512 - c0
                sp_, spb = SPS[idx % 3]
                K.mm(sp_[:, 0:n], ka[b][0:KA, kt * 128:(kt + 1) * 128], qa[b][0:KA, g * 512 + c0:(g + 1) * 512],
                     True, True, R=[kab[b], qab[b]], W=[spb])
                p, pb_ = pt[idx % 4], ptb[idx % 4]
                if even:
                    K.act(p[:, 0:n], sp_[:, 0:n], AF.Exp, R=[spb, nfcb], W=[pb_], bias=nfc[:, kt * 8 + hd:kt * 8 + hd + 1], scale=1.0)
                else:
                    K.act(p[:, 0:n], sp_[:, 0:n], AF.Exp, R=[spb], W=[pb_])
                if j >= 0:
                    K.op("pool", lambda h, p=p: h.tensor_tensor(out=p[:, 0:128], in0=p[:, 0:128], in1=tri, op=ALU.mult),
                         R=[pb_, trib], W=[pb_])

            def emit_pv(idx):
                hd, g, kt = its[idx]
                b = hd % 2
                j = kt - 4 * g
                c0 = 128 * j if j > 0 else 0
                n = 512 - c0
                last = 4 * g + 3
                gi = hd * NG + g
                if g == 0 and kt == 0 and hd >= 1 and hd + 1 < H:
                    load_head(hd + 1)
                o, ob = OPS[gi % 2]
                p, pb_ = pt[idx % 4], ptb[idx % 4]
                K.mm(o[:, c0:512], va[b][:, kt, :], p[:, 0:n], kt == 0, kt == last, R=[vab[b], pb_], W=[ob])
                if kt == last:
                    r, rb = rr[gi % 2], rrb[gi % 2]
                    t_, tb = ot[gi % 2], otb[gi % 2]
                    K.op("dve", lambda h: h.reciprocal(out=r[64:128, :], in_=o[64:128, :]), R=[ob], W=[rb])
                    K.op("dve", lambda h: h.tensor_tensor(out=t_, in0=o[0:64, :], in1=r[64:128, :], op=ALU.mult), R=[ob, rb], W=[tb])
                    K.dma("sp", CATT[hd * 64:(hd + 1) * 64, g * 512:(g + 1) * 512], t_, R=[tb], W=[B_CATT], dst=B_CATT)

            n_it = len(its)
            load_head(0)
            if H > 1:
                load_head(1)
            emit_s(0)
            if n_it > 1:
                emit_s(1)
            for idx in range(n_it):
                emit_pv(idx)
                if idx + 2 < n_it:
                    emit_s(idx + 2)
            K.end_phase()

        def phase_p3(L, xr_src, xr_src_b, xr_dst, xr_dst_b):
            even = (L % 2 == 0)
            li = L // 2
            A.reset()
            ident, identb = consts_common()
            wo = A.alloc([128, 8, D], BF16)
            wob = K.buf("wo", "sw")
            w_out = ev_w_out if even else od_w_out
            for hf in range(2):
                K.dma("pool", wo[:, :, hf * 512:(hf + 1) * 512],
                      w_out[li, :, hf * 512:(hf + 1) * 512].rearrange("(kt p) f -> p kt f", p=128), R=[B_w], W=[wob], dst=wob)
            g_ap = A.alloc([128, D], F32)
            b_ap = A.alloc([128, D], F32)
            gbb = K.buf("gb", True)
            K.dma("sp", g_ap, lng[0][L], R=[B_w], W=[gbb], dst=gbb)
            K.dma("sp", b_ap, lnb[0][L], R=[B_w], W=[gbb], dst=gbb)
            ct = [A.alloc([128, 8, 512], BF16) for _ in range(2)]
            ctb = [K.buf("ct%d" % i, True) for i in range(2)]
            xt = [A.alloc([128, D], F32) for _ in range(2)]
            xtb = [K.buf("p3x%d" % i, True) for i in range(2)]
            z = [A.alloc([128, D], F32) for _ in range(2)]
            zb = [K.buf("p3z%d" % i) for i in range(2)]
            xb = [A.alloc([128, D], BF16) for _ in range(2)]
            xbb = [K.buf("p3xb%d" % i) for i in range(2)]
            stg = [A.alloc([128, 8, 128], BF16) for _ in range(2)]
            stgb = [K.buf("p3s%d" % i) for i in range(2)]
            small = [A.alloc([128, 16], F32) for _ in range(2)]
            smallb = [K.buf("p3sm%d" % i) for i in range(2)]
            if not even:
                wr = A.alloc([128, 8, D], F32)
                wrb = K.buf("wr", True)
                for e in range(8):
                    K.dma("sp", wr[:, e, :], od_router[li, e], R=[B_w], W=[wrb], dst=wrb)
                junk = A.alloc([128, D], F32)
                junkb = K.buf("junk")
                rt = [A.alloc([128, 96], F32) for _ in range(2)]
                rtb = [K.buf("rt%d" % i) for i in range(2)]
                for i_ in range(2):
                    K.op("pool", lambda h, i_=i_: h.memset(rt[i_], 0.0), R=[], W=[rtb[i_]])
                ohb = [A.alloc([128, 8], BF16) for _ in range(2)]
                ohbb = [K.buf("ohb%d" % i) for i in range(2)]
                ustr = A.alloc([128, 128], BF16)
                ustrb = K.buf("ustr", True)
                K.dma("sp", ustr, c_ustrict[:, :], R=[B_w], W=[ustrb], dst=ustrb)
                onesb = A.alloc([128, 128], BF16)
                K.op("pool", lambda h: h.memset(onesb, 1.0), R=[], W=[ustrb])
                tot = A.alloc([128, 8], F32)
                totb = K.buf("tot")
                K.op("pool", lambda h: h.memset(tot, 0.0), R=[], W=[totb])

            def load_ct(g):
                K.dma("sp", ct[g % 2], CATT[:, g * 512:(g + 1) * 512].rearrange("(kt p) t -> p kt t", p=128),
                      R=[B_CATT], W=[ctb[g % 2]], dst=ctb[g % 2])

            load_ct(0)
            for tt in range(NT):
                g = tt // 4
                i = tt % 2
                if tt % 4 == 0 and g + 1 < NG:
                    load_ct(g + 1)
                K.dma("sp", xt[i], xr_src[tt * 128:(tt + 1) * 128, :], R=[xr_src_b], W=[xtb[i]], dst=xtb[i])
                c = ct[g % 2]
                t4 = tt % 4
                for hf in range(2):
                    ps, pb = PS[(tt * 2 + hf) % 4], psb[(tt * 2 + hf) % 4]
                    for kt in range(8):
                        K.mm(ps[:, :], c[:, kt, t4 * 128:(t4 + 1) * 128], wo[:, kt, hf * 512:(hf + 1) * 512], kt == 0, kt == 7,
                             R=[ctb[g % 2], wob], W=[pb])
                    K.op("dve", lambda h, ps=ps, hf=hf, i=i: h.scalar_tensor_tensor(
                        out=z[i][:, hf * 512:(hf + 1) * 512], in0=xt[i][:, hf * 512:(hf + 1) * 512], scalar=ALPHA,
                        in1=ps[:, :], op0=ALU.mult, op1=ALU.add), R=[pb, xtb[i]], W=[zb[i]])
                layer_norm(z[i], zb[i], 0, L, small[i], smallb[i], g_ap, b_ap, gbb)
                K.dma("sp", xr_dst[tt * 128:(tt + 1) * 128, :], z[i], R=[zb[i]], W=[xr_dst_b], dst=xr_dst_b)
                K.act(xb[i], z[i], AF.Copy, R=[zb[i]], W=[xbb[i]])
                emit_xT(xb[i], xbb[i], tt, ident, identb, stg[i], stgb[i])
                if not even:
                    r = rt[i]
                    rb = rtb[i]
                    lg, m8_, dd, g1, g2, c1, c2 = r[:, 0:8], r[:, 8:16], r[:, 16:17], r[:, 66:67], r[:, 67:68], r[:, 24:32], r[:, 32:40]
                    oh0, oh1, rank, j8 = r[:, 48:56], r[:, 56:64], r[:, 72:80], r[:, 80:88]
                    for e in range(8):
                        K.op("dve", lambda h, e=e, i=i, lg=lg: h.scalar_tensor_tensor(
                            out=junk, in0=z[i], scalar=1.0, in1=wr[:, e, :], op0=ALU.mult, op1=ALU.mult, accum_out=lg[:, e:e + 1]),
                            R=[zb[i], wrb], W=[junkb, rb])
                    K.op("dve", lambda h, m8_=m8_, lg=lg: h.max(out=m8_, in_=lg), R=[rb], W=[rb])
                    K.op("dve", lambda h, dd=dd, m8_=m8_: h.tensor_tensor(out=dd, in0=m8_[:, 1:2], in1=m8_[:, 0:1], op=ALU.subtract), R=[rb], W=[rb])
                    K.act(dd, dd, AF.Exp, R=[rb], W=[rb])
                    K.op("dve", lambda h, g1=g1, dd=dd: h.tensor_scalar(out=g1, in0=dd, scalar1=1.0, scalar2=None, op0=ALU.add), R=[rb], W=[rb])
                    K.op("dve", lambda h, g1=g1: h.reciprocal(out=g1, in_=g1), R=[rb], W=[rb])
                    K.op("dve", lambda h, g1=g1, g2=g2, dd=dd: h.tensor_tensor(out=g2, in0=dd, in1=g1, op=ALU.mult), R=[rb], W=[rb])
                    K.op("dve", lambda h, c1=c1, lg=lg, m8_=m8_, g1=g1: h.tensor_scalar(out=c1, in0=lg, scalar1=m8_[:, 0:1], scalar2=g1, op0=ALU.is_equal, op1=ALU.mult), R=[rb], W=[rb])
                    K.op("dve", lambda h, c2=c2, lg=lg, m8_=m8_, g2=g2: h.tensor_scalar(out=c2, in0=lg, scalar1=m8_[:, 1:2], scalar2=g2, op0=ALU.is_equal, op1=ALU.mult), R=[rb], W=[rb])
                    K.op("dve", lambda h, c1=c1, c2=c2: h.tensor_tensor(out=c1, in0=c1, in1=c2, op=ALU.add), R=[rb], W=[rb])
                    K.dma("sp", COMB[tt * 128:(tt + 1) * 128, :], c1, R=[rb], W=[B_COMB], dst=B_COMB)
                    K.op("dve", lambda h, oh0=oh0, lg=lg, m8_=m8_: h.tensor_scalar(out=oh0, in0=lg, scalar1=m8_[:, 0:1], scalar2=None, op0=ALU.is_equal), R=[rb], W=[rb])
                    K.op("dve", lambda h, oh1=oh1, lg=lg, m8_=m8_: h.tensor_scalar(out=oh1, in0=lg, scalar1=m8_[:, 1:2], scalar2=None, op0=ALU.is_equal), R=[rb], W=[rb])
                    K.op("dve", lambda h, i=i, oh0=oh0, oh1=oh1: h.tensor_tensor(out=ohb[i], in0=oh0, in1=oh1, op=ALU.add), R=[rb], W=[ohbb[i]])
                    pr, prb = PS[4 + i], psb[4 + i]
                    K.mm(pr[:, 0:8], ustr, ohb[i], True, True, R=[ustrb, ohbb[i]], W=[prb])
                    K.mm(pr[:, 8:16], onesb, ohb[i], True, True, R=[ustrb, ohbb[i]], W=[prb])
                    K.op("dve", lambda h, rank=rank, pr=pr: h.tensor_tensor(out=rank, in0=pr[:, 0:8], in1=tot, op=ALU.add), R=[prb, totb], W=[rb])
                    K.op("dve", lambda h, pr=pr: h.tensor_tensor(out=tot, in0=pr[:, 8:16], in1=tot, op=ALU.add), R=[prb, totb], W=[totb])
                    K.op("dve", lambda h, oh0=oh0, rank=rank, j8=j8, o=r[:, 64:65]: h.scalar_tensor_tensor(
                        out=j8, in0=oh0, scalar=1.0, in1=rank, op0=ALU.mult, op1=ALU.mult, accum_out=o), R=[rb], W=[rb])
                    K.op("dve", lambda h, oh1=oh1, rank=rank, j8=j8, o=r[:, 65:66]: h.scalar_tensor_tensor(
                        out=j8, in0=oh1, scalar=1.0, in1=rank, op0=ALU.mult, op1=ALU.mult, accum_out=o), R=[rb], W=[rb])
                    K.dma("sp", ROUTE[tt * 128:(tt + 1) * 128, :], r[:, 48:72], R=[rb], W=[B_ROUTE], dst=B_ROUTE)
            if not even:
                K.dma("sp", CNT[:, :], tot, R=[totb], W=[B_CNT], dst=B_CNT)
            K.end_phase()

        def phase_p4(L, xr_src, xr_src_b, xr_dst, xr_dst_b, last):
            even = (L % 2 == 0)
            li = L // 2
            A.reset()
            ident, identb = consts_common()
            Wg, Wu, Wd = (ev_g, ev_u, ev_d) if even else (od_g, od_u, od_d)
            experts = [0] if even else list(range(NE))
            g_ap = A.alloc([128, D], F32)
            b_ap = A.alloc([128, D], F32)
            gbb = K.buf("gb4", True)
            K.dma("sp", g_ap, lng[1][L], R=[B_w], W=[gbb], dst=gbb)
            K.dma("sp", b_ap, lnb[1][L], R=[B_w], W=[gbb], dst=gbb)
            xTb = A.alloc([128, 8, 1024], BF16)
            xTbb = K.buf("xTb", True)
            yacc = A.alloc([128, 8, D], F32)
            yab = [K.buf("yacc%d" % i, True) for i in range(8)]
            hT = A.alloc([128, 14, 1024], BF16)
            hTb = [K.buf("hT%d" % i) for i in range(14)]
            wgu = [A.alloc([128, 2, 8, 256], BF16) for _ in range(2)]
            wgub = [K.buf("wgu%d" % i, "sw") for i in range(2)]
            wd = [A.alloc([128, 14, D], BF16) for _ in range(2)]
            wdb = [K.buf("wd%d" % i, "sw") for i in range(2)]
            sg = [A.alloc([128, 512], F32) for _ in range(2)]
            sgb = [K.buf("sg%d" % i) for i in range(2)]
            xb = [A.alloc([128, D], BF16) for _ in range(2)]
            xbb = [K.buf("p4xb%d" % i) for i in range(2)]
            stg = [A.alloc([128, 8, 128], BF16) for _ in range(2)]
            stgb = [K.buf("p4s%d" % i) for i in range(2)]
            small = [A.alloc([128, 16], F32) for _ in range(2)]
            smallb = [K.buf("p4sm%d" % i) for i in range(2)]
            comb = A.alloc([128, 8, 8], F32)
            combb = K.buf("comb_s", True)

            gl = []
            dl = []
            for blk in range(NBLK):
                for e in experts:
                    for hf in range(2):
                        dl.append((blk, e, hf))
                        for fg in range(7):
                            gl.append((blk, e, hf, fg))
            st_ = {"g": 0, "d": 0}

            def load_g(n):
                if n >= len(gl):
                    return
                blk, e, hf, fg = gl[n]
                f0 = hf * 1792 + fg * 256
                i = n % 2
                K.dma("pool", wgu[i][:, 0], Wg[li, e, :, f0:f0 + 256].rearrange("(kt p) f -> p kt f", p=128), R=[B_w], W=[wgub[i]], dst=wgub[i])
                K.dma("pool", wgu[i][:, 1], Wu[li, e, :, f0:f0 + 256].rearrange("(kt p) f -> p kt f", p=128), R=[B_w], W=[wgub[i]], dst=wgub[i])

            def load_d(n):
                if n >= len(dl):
                    return
                blk, e, hf = dl[n]
                i = n % 2
                K.dma("pool", wd[i], Wd[li, e, hf * 1792:(hf + 1) * 1792, :].rearrange("(fc p) d -> p fc d", p=128), R=[B_w], W=[wdb[i]], dst=wdb[i])

            load_g(0)
            load_d(0)
            gi = 0
            di = 0
            pcount = 0
            ycount = 0
            for blk in range(NBLK):
                t0 = blk * 1024
                K.dma("sp", xTb, XT[:, t0:t0 + 1024].rearrange("(kt p) t -> p kt t", p=128), R=[B_XT], W=[xTbb], dst=xTbb)
                for tt in range(8):
                    K.dma("sp", yacc[:, tt, :], xr_src[t0 + tt * 128:t0 + (tt + 1) * 128, :], R=[xr_src_b], W=[yab[tt]], dst=yab[tt])
                    K.op("pool", lambda h, tt=tt: h.tensor_scalar(out=yacc[:, tt, :], in0=yacc[:, tt, :], scalar1=ALPHA, scalar2=0.0, op0=ALU.mult, op1=ALU.add),
                         R=[yab[tt]], W=[yab[tt]])
                if not even:
                    K.dma("sp", comb, COMB[t0:t0 + 1024, :].rearrange("(tt p) e -> p tt e", p=128), R=[B_COMB], W=[combb], dst=combb)
                for e in experts:
                    for hf in range(2):
                        load_d(di + 1)
                        wdt, wdtb = wd[di % 2], wdb[di % 2]
                        di += 1
                        for fg in range(7):
                            load_g(gi + 1)
                            w_, w_b = wgu[gi % 2], wgub[gi % 2]
                            gi += 1
                            for fc2 in range(2):
                                fcl = fg * 2 + fc2
                                for tg in range(2):
                                    pg, pgb = PS[(pcount % 2) * 2], psb[(pcount % 2) * 2]
                                    pu, pub = PS[(pcount % 2) * 2 + 1], psb[(pcount % 2) * 2 + 1]
                                    s_, s_b = sg[pcount % 2], sgb[pcount % 2]
                                    pcount += 1
                                    for kt in range(8):
                                        K.mm(pg[:, :], w_[:, 0, kt, fc2 * 128:(fc2 + 1) * 128], xTb[:, kt, tg * 512:(tg + 1) * 512],
                                             kt == 0, kt == 7, R=[w_b, xTbb], W=[pgb])
                                    for kt in range(8):
                                        K.mm(pu[:, :], w_[:, 1, kt, fc2 * 128:(fc2 + 1) * 128], xTb[:, kt, tg * 512:(tg + 1) * 512],
                                             kt == 0, kt == 7, R=[w_b, xTbb], W=[pub])
                                    K.act(s_, pg[:, :], AF.Silu, R=[pgb], W=[s_b])
                                    K.op("dve", lambda h, fcl=fcl, tg=tg, s_=s_, pu=pu: h.tensor_tensor(
                                        out=hT[:, fcl, tg * 512:(tg + 1) * 512], in0=pu[:, :], in1=s_, op=ALU.mult),
                                        R=[pub, s_b], W=[hTb[fcl]])
                        for tt in range(8):
                            for dh in range(2):
                                py, pyb = PS[4 + ycount % 2], psb[4 + ycount % 2]
                                ycount += 1
                                for fcl in range(14):
                                    K.mm(py[:, :], hT[:, fcl, tt * 128:(tt + 1) * 128], wdt[:, fcl, dh * 512:(dh + 1) * 512],
                                         fcl == 0, fcl == 13, R=[hTb[fcl], wdtb], W=[pyb])
                                sc = 1.0 if even else comb[:, tt, e:e + 1]
                                rds = [pyb, yab[tt]] + ([] if even else [combb])
                                K.op("dve", lambda h, py=py, tt=tt, dh=dh, sc=sc: h.scalar_tensor_tensor(
                                    out=yacc[:, tt, dh * 512:(dh + 1) * 512], in0=py[:, :], scalar=sc,
                                    in1=yacc[:, tt, dh * 512:(dh + 1) * 512], op0=ALU.mult, op1=ALU.add), R=rds, W=[yab[tt]])
                for tt in range(8):
                    i = tt % 2
                    zt = yacc[:, tt, :]
                    layer_norm(zt, yab[tt], 1, L, small[i], smallb[i], g_ap, b_ap, gbb)
                    gt = blk * 8 + tt
                    if last:
                        K.dma("sp", y_out[gt * 128:(gt + 1) * 128, :], zt, R=[yab[tt]], W=[B_Y], dst=B_Y)
                    else:
                        K.dma("sp", xr_dst[gt * 128:(gt + 1) * 128, :], zt, R=[yab[tt]], W=[xr_dst_b], dst=xr_dst_b)
                        K.act(xb[i], zt, AF.Copy, R=[yab[tt]], W=[xbb[i]])
                        emit_xT(xb[i], xbb[i], gt, ident, identb, stg[i], stgb[i])
            K.end_phase()

        def phase_p4s(L, xr_src, xr_src_b, xr_dst, xr_dst_b, last):
            li = L // 2
            A.reset()
            ident, identb = consts_common()
            g_ap = A.alloc([128, D], F32)
            b_ap = A.alloc([128, D], F32)
            gbb = K.buf("gb4", True)
            K.dma("sp", g_ap, lng[1][L], R=[B_w], W=[gbb], dst=gbb)
            K.dma("sp", b_ap, lnb[1][L], R=[B_w], W=[gbb], dst=gbb)
            misc = A.alloc([128, 64], F32)
            miscb = K.buf("misc", True)
            K.dma("sp", misc, c_misc[:, :], R=[B_w], W=[miscb], dst=miscb)
            cnt = A.alloc([128, 8], F32)
            cntb = K.buf("cnt_s", True)
            K.dma("sp", cnt, CNT[:, :], R=[B_CNT], W=[cntb], dst=cntb)
            tb_ = A.alloc([128, 64], F32)
            tbb = K.buf("tb")
            nt, tbase, tend, sbase, tmp8 = tb_[:, 0:8], tb_[:, 8:16], tb_[:, 16:24], tb_[:, 24:32], tb_[:, 32:40]
            K.op("dve", lambda h: h.tensor_scalar(out=nt, in0=cnt, scalar1=0.0, scalar2=None, op0=ALU.is_gt), R=[cntb], W=[tbb])
            for m in range(1, 8):
                K.op("dve", lambda h, m=m: h.scalar_tensor_tensor(out=nt, in0=cnt, scalar=512.0 * m, in1=nt, op0=ALU.is_gt, op1=ALU.add),
                     R=[cntb, tbb], W=[tbb])
            K.op("dve", lambda h: h.memset(tbase[:, 0:1], 0.0), R=[], W=[tbb])
            for e in range(1, 8):
                K.op("dve", lambda h, e=e: h.tensor_tensor(out=tbase[:, e:e + 1], in0=tbase[:, e - 1:e], in1=nt[:, e - 1:e], op=ALU.add), R=[tbb], W=[tbb])
            K.op("dve", lambda h: h.tensor_tensor(out=tend, in0=tbase, in1=nt, op=ALU.add), R=[tbb], W=[tbb])
            K.op("dve", lambda h: h.tensor_scalar(out=sbase, in0=tbase, scalar1=512.0, scalar2=None, op0=ALU.mult), R=[tbb], W=[tbb])
            te = A.alloc([128, NTILE], F32)
            teb = K.buf("te")
            jrow = misc[:, 0:NTILE]
            K.op("dve", lambda h: h.tensor_scalar(out=te, in0=jrow, scalar1=tend[:, 0:1], scalar2=None, op0=ALU.is_ge), R=[miscb, tbb], W=[teb])
            for e in range(1, 8):
                K.op("dve", lambda h, e=e: h.scalar_tensor_tensor(out=te, in0=jrow, scalar=tend[:, e:e + 1], in1=te, op0=ALU.is_ge, op1=ALU.add),
                     R=[miscb, tbb, teb], W=[teb])
            K.op("dve", lambda h: h.tensor_scalar(out=te, in0=te, scalar1=7.0, scalar2=None, op0=ALU.min), R=[teb], W=[teb])
            idxWf = A.alloc([128, NTILE, 8], F32)
            idxW = A.alloc([128, NTILE, 8], I32)
            idxDf = A.alloc([128, NTILE, 28], F32)
            idxD = A.alloc([128, NTILE, 28], I32)
            idxb = K.buf("idx")
            te2 = A.alloc([128, NTILE], F32)
            K.op("dve", lambda h: h.tensor_scalar(out=te2, in0=te, scalar1=1024.0, scalar2=float(li * 8192), op0=ALU.mult, op1=ALU.add), R=[teb], W=[idxb])
            for j in range(NTILE):
                K.op("dve", lambda h, j=j: h.tensor_scalar(out=idxWf[:, j, :], in0=misc[:, 53:61], scalar1=te2[:, j:j + 1], scalar2=None, op0=ALU.add),
                     R=[miscb, idxb], W=[idxb])
            K.op("dve", lambda h: h.tensor_scalar(out=idxWf, in0=idxWf, scalar1=4.0, scalar2=None, op0=ALU.mult), R=[idxb], W=[idxb])
            K.op("dve", lambda h: h.tensor_copy(out=idxW, in_=idxWf), R=[idxb], W=[idxb])
            K.op("dve", lambda h: h.tensor_scalar(out=te2, in0=te, scalar1=3584.0, scalar2=float(li * 28672), op0=ALU.mult, op1=ALU.add), R=[teb, idxb], W=[idxb])
            for j in range(NTILE):
                K.op("dve", lambda h, j=j: h.tensor_scalar(out=idxDf[:, j, :], in0=misc[:, 24:52], scalar1=te2[:, j:j + 1], scalar2=None, op0=ALU.add),
                     R=[miscb, idxb], W=[idxb])
            K.op("dve", lambda h: h.tensor_copy(out=idxD, in_=idxDf), R=[idxb], W=[idxb])

            xr = [A.alloc([128, 4, D], BF16) for _ in range(2)]
            xrb = [K.buf("xr%d" % i, True) for i in range(2)]
            K.op("pool", lambda h: h.memset(xr[0], 0.0), R=[], W=[xrb[0]])
            for j in range(NTILE):
                K.dma("pool", XS[j * 512:(j + 1) * 512, :].rearrange("(a p) d -> p a d", p=128), xr[0], R=[xrb[0]], W=[B_XS], dst=B_XS)
            slotf = A.alloc([128, NT, 2], F32)
            sloti = A.alloc([128, NT, 2], I32)
            gts = A.alloc([128, NT, 2], F32)
            slb = K.buf("slots")
            rte = [A.alloc([128, 24], F32) for _ in range(2)]
            rteb = [K.buf("rte%d" % i, True) for i in range(2)]
            xt = [A.alloc([128, D], F32) for _ in range(2)]
            xtb = [K.buf("p4x%d" % i, True) for i in range(2)]
            xb = [A.alloc([128, D], BF16) for _ in range(2)]
            xbb = [K.buf("p4xb%d" % i) for i in range(2)]
            j8 = A.alloc([128, 8], F32)
            j8b = K.buf("j8")
            for tt in range(NT):
                i = tt % 2
                K.dma("sp", rte[i], ROUTE[tt * 128:(tt + 1) * 128, :], R=[B_ROUTE], W=[rteb[i]], dst=rteb[i])
                K.dma("sp", xt[i], xr_src[tt * 128:(tt + 1) * 128, :], R=[xr_src_b], W=[xtb[i]], dst=xtb[i])
                K.act(xb[i], xt[i], AF.Copy, R=[xtb[i]], W=[xbb[i]])
                for k2 in range(2):
                    K.op("dve", lambda h, i=i, k2=k2, tt=tt: h.scalar_tensor_tensor(
                        out=j8, in0=rte[i][:, k2 * 8:(k2 + 1) * 8], scalar=1.0, in1=sbase, op0=ALU.mult, op1=ALU.mult,
                        accum_out=slotf[:, tt, k2:k2 + 1]), R=[rteb[i], tbb], W=[j8b, slb])
                K.op("dve", lambda h, i=i, tt=tt: h.tensor_tensor(out=slotf[:, tt, :], in0=slotf[:, tt, :], in1=rte[i][:, 16:18], op=ALU.add),
                     R=[rteb[i], slb], W=[slb])
                K.op("dve", lambda h, tt=tt: h.tensor_copy(out=sloti[:, tt, :], in_=slotf[:, tt, :]), R=[slb], W=[slb])
                K.op("dve", lambda h, i=i, tt=tt: h.tensor_copy(out=gts[:, tt, :], in_=rte[i][:, 18:20]), R=[rteb[i]], W=[slb])
                for k2 in range(2):
                    K.idma("pool", lambda h, i=i, tt=tt, k2=k2: h.indirect_dma_start(
                        out=XS[:, :], out_offset=bass.IndirectOffsetOnAxis(sloti[:, tt, k2:k2 + 1], 0), in_=xb[i], in_offset=None),
                        R=[xbb[i], slb], W=[B_XS], dst=B_XS)

            main_mark = A.off
            xTb = A.alloc([128, 8, 512], BF16)
            xTbb = K.buf("xTb")
            hT = A.alloc([128, 14, 512], BF16)
            hTb = [K.buf("hT%d" % i) for i in range(14)]
            wgu = [A.alloc([128, 2, 8, 896], BF16) for _ in range(2)]
            wgub = [K.buf("wgu%d" % i, "sw") for i in range(2)]
            wd = [A.alloc([128, 14, D], BF16) for _ in range(2)]
            wdb = [K.buf("wd%d" % i, "sw") for i in range(2)]
            sg = [A.alloc([128, 512], F32) for _ in range(2)]
            sgb = [K.buf("sg%d" % i) for i in range(2)]
            ysb = [A.alloc([128, 4, D], F32) for _ in range(1)]
            ysbb = [K.buf("ysb%d" % i) for i in range(1)]
            Wg_v = od_g.rearrange("l e k (q f) -> (l e k q) f", q=4)
            Wu_v = od_u.rearrange("l e k (q f) -> (l e k q) f", q=4)
            Wd_v = od_d.rearrange("l e f d -> (l e f) d")
            gl = [(j, q) for j in range(NTILE) for q in range(4)]
            dl = [(j, hf) for j in range(NTILE) for hf in range(2)]

            def load_g(n):
                if n >= len(gl):
                    return
                j, q = gl[n]
                i = n % 2
                for gu, Wv in ((0, Wg_v), (1, Wu_v)):
                    for kt in range(8):
                        K.idma("pool", lambda h, i=i, gu=gu, Wv=Wv, q=q, j=j, kt=kt: h.indirect_dma_start(
                            out=wgu[i][:, gu, kt, :], out_offset=None, in_=Wv[:, :],
                            in_offset=bass.IndirectOffsetOnAxis(idxW[:, j, kt:kt + 1], 0), element_offset=q * 896),
                            R=[B_w, idxb], W=[wgub[i]], dst=wgub[i])

            def load_d(n):
                if n >= len(dl):
                    return
                j, hf = dl[n]
                i = n % 2
                for fc in range(14):
                    K.idma("pool", lambda h, i=i, fc=fc, j=j, hf=hf: h.indirect_dma_start(
                        out=wd[i][:, fc, :], out_offset=None, in_=Wd_v[:, :],
                        in_offset=bass.IndirectOffsetOnAxis(idxD[:, j, hf * 14 + fc:hf * 14 + fc + 1], 0)), R=[B_w, idxb], W=[wdb[i]], dst=wdb[i])

            load_g(0)
            load_d(0)
            gi = 0
            di = 0
            pcount = 0
            ycount = 0
            for j in range(NTILE):
                xi = j % 2
                K.dma("sp", xr[xi], XS[j * 512:(j + 1) * 512, :].rearrange("(a p) d -> p a d", p=128), R=[B_XS], W=[xrb[xi]], dst=xrb[xi])
                for a in range(4):
                    xv = xr[xi][:, a, :].rearrange("p (q k) -> p k q", k=8)
                    for kt in range(8):
                        K.tr(PSBv[:, kt, :], xv[:, kt, :], ident, R=[xrb[xi], identb], W=[psbb])
                    K.op("dve", lambda h, a=a: h.tensor_copy(out=xTb[:, :, a * 128:(a + 1) * 128], in_=PSBv), R=[psbb], W=[xTbb])
                yb_, ybb_ = ysb[0], ysbb[0]
                for hf in range(2):
                    load_d(di + 1)
                    wdt, wdtb = wd[di % 2], wdb[di % 2]
                    di += 1
                    for q2 in range(2):
                        load_g(gi + 1)
                        w_, w_b = wgu[gi % 2], wgub[gi % 2]
                        gi += 1
                        for fc2 in range(7):
                            fcl = q2 * 7 + fc2
                            pg, pgb = PS[(pcount % 2) * 2], psb[(pcount % 2) * 2]
                            pu, pub = PS[(pcount % 2) * 2 + 1], psb[(pcount % 2) * 2 + 1]
                            s_, s_b = sg[pcount % 2], sgb[pcount % 2]
                            pcount += 1
                            for kt in range(8):
                                K.mm(pg[:, :], w_[:, 0, kt, fc2 * 128:(fc2 + 1) * 128], xTb[:, kt, :], kt == 0, kt == 7, R=[w_b, xTbb], W=[pgb])
                            for kt in range(8):
                                K.mm(pu[:, :], w_[:, 1, kt, fc2 * 128:(fc2 + 1) * 128], xTb[:, kt, :], kt == 0, kt == 7, R=[w_b, xTbb], W=[pub])
                            K.act(s_, pg[:, :], AF.Silu, R=[pgb], W=[s_b])
                            K.op("dve", lambda h, fcl=fcl, s_=s_, pu=pu: h.tensor_tensor(out=hT[:, fcl, :], in0=pu[:, :], in1=s_, op=ALU.mult),
                                 R=[pub, s_b], W=[hTb[fcl]])
                    for a in range(4):
                        for dh in range(2):
                            py, pyb = PS[4 + ycount % 2], psb[4 + ycount % 2]
                            ycount += 1
                            for fcl in range(14):
                                K.mm(py[:, :], hT[:, fcl, a * 128:(a + 1) * 128], wdt[:, fcl, dh * 512:(dh + 1) * 512],
                                     fcl == 0, fcl == 13, R=[hTb[fcl], wdtb], W=[pyb])
                            if hf == 0:
                                K.op("dve", lambda h, py=py, a=a, dh=dh, yb_=yb_: h.tensor_copy(out=yb_[:, a, dh * 512:(dh + 1) * 512], in_=py[:, :]),
                                     R=[pyb], W=[ybb_])
                            else:
                                K.op("dve", lambda h, py=py, a=a, dh=dh, yb_=yb_: h.tensor_tensor(
                                    out=yb_[:, a, dh * 512:(dh + 1) * 512], in0=py[:, :], in1=yb_[:, a, dh * 512:(dh + 1) * 512], op=ALU.add),
                                    R=[pyb, ybb_], W=[ybb_])
                K.dma("sp", YS[j * 512:(j + 1) * 512, :].rearrange("(a p) d -> p a d", p=128), yb_, R=[ybb_], W=[B_YS], dst=B_YS)

            K.barrier()
            A.off = main_mark
            ya = [[A.alloc([128, D], F32) for _ in range(2)] for _ in range(2)]
            yab_ = [[K.buf("ya%d%d" % (i, k2), "sw") for k2 in range(2)] for i in range(2)]
            stg = [A.alloc([128, 8, 128], BF16) for _ in range(2)]
            stgb = [K.buf("p4s%d" % i) for i in range(2)]
            small = [A.alloc([128, 16], F32) for _ in range(2)]
            smallb = [K.buf("p4sm%d" % i) for i in range(2)]
            for tt in range(NT):
                i = tt % 2
                K.dma("sp", xt[i], xr_src[tt * 128:(tt + 1) * 128, :], R=[xr_src_b], W=[xtb[i]], dst=xtb[i])
                for k2 in range(2):
                    K.idma("pool", lambda h, i=i, tt=tt, k2=k2: h.indirect_dma_start(
                        out=ya[i][k2], out_offset=None, in_=YS[:, :], in_offset=bass.IndirectOffsetOnAxis(sloti[:, tt, k2:k2 + 1], 0)),
                        R=[B_YS, slb], W=[yab_[i][k2]], dst=yab_[i][k2])
                zt = xt[i]
                K.op("dve", lambda h, i=i, tt=tt: h.tensor_scalar(out=ya[i][0], in0=ya[i][0], scalar1=gts[:, tt, 0:1], scalar2=None, op0=ALU.mult),
                     R=[yab_[i][0], slb], W=[yab_[i][0]])
                K.op("dve", lambda h, i=i, tt=tt: h.scalar_tensor_tensor(out=ya[i][0], in0=ya[i][1], scalar=gts[:, tt, 1:2], in1=ya[i][0], op0=ALU.mult, op1=ALU.add),
                     R=[yab_[i][0], yab_[i][1], slb], W=[yab_[i][0]])
                K.op("dve", lambda h, zt=zt, i=i: h.scalar_tensor_tensor(out=zt, in0=zt, scalar=ALPHA, in1=ya[i][0], op0=ALU.mult, op1=ALU.add),
                     R=[xtb[i], yab_[i][0]], W=[xtb[i]])
                layer_norm(zt, xtb[i], 1, L, small[i], smallb[i], g_ap, b_ap, gbb)
                if last:
                    K.dma("sp", y_out[tt * 128:(tt + 1) * 128, :], zt, R=[xtb[i]], W=[B_Y], dst=B_Y)
                else:
                    K.dma("sp", xr_dst[tt * 128:(tt + 1) * 128, :], zt, R=[xtb[i]], W=[xr_dst_b], dst=xr_dst_b)
                    K.act(xb[i], zt, AF.Copy, R=[xtb[i]], W=[xbb[i]])
                    emit_xT(xb[i], xbb[i], tt, ident, identb, stg[i], stgb[i])
            K.end_phase()

        phase_p0()
        cur, curb = x_in, B_xin
        for n, L in enumerate(layers):
            last = (n == len(layers) - 1)
            phase_p1(L)
            if last and stop == "p1":
                break
            phase_p2(L)
            if last and stop == "p2":
                break
            phase_p3(L, cur, curb, XR[0], B_XR[0])
            if last and stop == "p3":
                break
            if SPARSE and L % 2 == 1:
                phase_p4s(L, XR[0], B_XR[0], XR[1], B_XR[1], last)
            else:
                phase_p4(L, XR[0], B_XR[0], XR[1], B_XR[1], last)
            cur, curb = XR[1], B_XR[1]
        sp = K.E["sp"]
        sp.prog.append(([(B_Y.sem, K.semcnt[B_Y.sem])], None, None, 0))

        with nc.Block() as block:
            @block.tensor
            def _(h):
                K.replay("pe", h)

            @block.scalar
            def _(h):
                K.replay("act", h)

            @block.vector
            def _(h):
                K.replay("dve", h)

            @block.gpsimd
            def _(h):
                K.replay("pool", h)

            @block.sync
            def _(h):
                K.replay("sp", h)
    return nc


def make_consts(S):
    NG = S // 512
    bf = ml_dtypes.bfloat16
    c = {}
    c["c_ident_bf"] = np.eye(128, dtype=np.float32).astype(bf)
    c["c_ident_f"] = np.eye(128, dtype=np.float32)
    c["c_tri"] = np.triu(np.ones((128, 128), np.float32)).astype(bf)
    koh = np.zeros((16, S), np.float32)
    for n in range(min(16, S // 256)):
        koh[n, n * 256:(n + 1) * 256] = 1.0
    c["c_koh"] = koh.astype(bf)
    past = np.zeros((NG, 128, 4, 16), np.float32)
    own = np.zeros((NG, 128, 4, 16), np.float32)
    for g in range(NG):
        for t4 in range(4):
            qblk = (g * 4 + t4) // 2
            past[g, :, t4, qblk:] = -1e30
            own[g, :, t4, qblk] = 1.0
    c["c_past"] = past
    c["c_own"] = own
    inv = np.zeros((4, 128, 16), np.float32)
    for g4, w in enumerate(POOL_W):
        for t in range(16):
            inv[g4, :, t] = 1.0 / min(t + 1, w)
    c["c_invcnt"] = inv
    c["c_ustrict"] = np.triu(np.ones((128, 128), np.float32), k=1).astype(bf)
    misc = np.zeros((128, 64), np.float32)
    misc[:, 0:24] = np.arange(24, dtype=np.float32)[None, :]
    misc[:, 24:52] = np.arange(28, dtype=np.float32)[None, :] * 128 + np.arange(128, dtype=np.float32)[:, None]
    misc[:, 52] = np.arange(128, dtype=np.float32)
    misc[:, 53:61] = np.arange(128, dtype=np.float32)[:, None] * 8 + np.arange(8, dtype=np.float32)[None, :]
    c["c_misc"] = misc
    return c


def prep_shared(inp):
    f = lambda a: np.ascontiguousarray(np.asarray(a, dtype=np.float32))
    sh = {}
    for nm in ("ln1_g", "ln1_b", "ln2_g", "ln2_b"):
        a = f(inp[nm])
        sh[nm] = np.ascontiguousarray(np.broadcast_to(a[:, None, :], (a.shape[0], 128, a.shape[1])))
    sh["ev_w_in"] = f(inp["ev_w_in"])
    sh["ev_negb"] = f(inp["ev_b_forget"]).reshape(2, 8, 1)
    sh["ev_pool_w"] = f(inp["ev_pool_w"])
    sh["ev_pool_scale"] = f(inp["ev_pool_scale"]).reshape(2, 4, 128, 1)
    sh["ev_w_out"] = f(inp["ev_w_out"])
    sh["ev_ffn_gate"] = f(inp["ev_ffn_gate"])[:, None]
    sh["ev_ffn_up"] = f(inp["ev_ffn_up"])[:, None]
    sh["ev_ffn_down"] = f(inp["ev_ffn_down"])[:, None]
    sh["od_w_in"] = f(inp["od_w_in"])
    sh["od_w_out"] = f(inp["od_w_out"])
    r = f(inp["od_router"])
    rt = np.transpose(r, (0, 2, 1))
    sh["od_router"] = np.ascontiguousarray(np.broadcast_to(rt[:, :, None, :], (2, 8, 128, D)))
    sh["od_exp_gate"] = f(inp["od_exp_gate"])
    sh["od_exp_up"] = f(inp["od_exp_up"])
    sh["od_exp_down"] = f(inp["od_exp_down"])
    return sh


def run(inputs, S, layers, dbg=False, stop=None):
    x = np.asarray(inputs["x"], dtype=np.float32)
    B = x.shape[0]
    sh = prep_shared(inputs)
    sh.update(make_consts(S))
    nc = build_nc(S, layers, 4, dbg, stop)
    in_maps = []
    for b in range(B):
        m = dict(sh)
        m["x"] = np.ascontiguousarray(x[b, :S])
        in_maps.append(m)
    res = run_bass_kernel_spmd(nc, in_maps, core_ids=list(range(B)))
    if dbg:
        return res.results
    return np.stack([np.asarray(r["y"], dtype=np.float32) for r in res.results], axis=0)


def kernel(**inputs):
    return run(inputs, 4096, [0, 1, 2, 3])
```

```python
import numpy as np
import ml_dtypes
import concourse.bass as bass
import concourse.mybir as mybir
from concourse.bass_utils import run_bass_kernel_spmd

F32 = mybir.dt.float32
BF16 = mybir.dt.bfloat16
I32 = mybir.dt.int32
SPARSE = True
AF = mybir.ActivationFunctionType
ALU = mybir.AluOpType

D = 1024
DFF = 3584
NE = 8
ALPHA = float(8 ** 0.25)
EPS = 1e-5
POOL_W = (2, 4, 8, 16)
NEG = -30000.0
ARENA_BYTES = 206 * 1024


class Buf:
    __slots__ = ("name", "w", "r", "sem")

    def __init__(self, name, sem=None):
        self.name = name
        self.w = None
        self.r = {}
        self.sem = sem


class Eng:
    def __init__(self, name, sem):
        self.name = name
        self.sem = sem
        self.n = 0
        self.seen = {}
        self.prog = []


class Kern:
    def __init__(self, nc, sems):
        self.nc = nc
        self.sems = sems
        self.semcnt = [0] * len(sems)
        self.free = list(range(5, len(sems) - 12))
        self.free_sw = list(range(len(sems) - 12, len(sems)))
        self.phase_sems_sw = []
        self.E = {n: Eng(n, i) for i, n in enumerate(["pe", "act", "dve", "pool", "sp"])}
        self.phase_sems = []

    def buf(self, name, dma=False):
        s = None
        if dma == "sw":
            s = self.free_sw.pop()
            self.phase_sems_sw.append(s)
        elif dma:
            s = self.free.pop()
            self.phase_sems.append(s)
        return Buf(name, s)

    def end_phase(self):
        self.barrier()
        self.free.extend(self.phase_sems)
        self.phase_sems = []
        self.free_sw.extend(self.phase_sems_sw)
        self.phase_sems_sw = []

    def _waits(self, eng, R, W):
        deps = {}

        def add(s, v):
            if deps.get(s, 0) < v:
                deps[s] = v

        for b in R:
            if b.w is not None:
                add(*b.w)
        for b in W:
            if b.w is not None:
                add(*b.w)
            for s, v in b.r.items():
                add(s, v)
        waits = []
        for s, v in deps.items():
            if s == eng.sem and eng.name == "pe":
                continue
            if eng.seen.get(s, 0) < v:
                eng.seen[s] = v
                waits.append((s, v))
        return waits

    def op(self, en, fn, R=(), W=()):
        eng = self.E[en]
        waits = self._waits(eng, R, W)
        eng.n += 1
        tok = (eng.sem, eng.n)
        eng.prog.append((waits, fn, eng.sem, 1))
        for b in R:
            if b.r.get(tok[0], 0) < tok[1]:
                b.r[tok[0]] = tok[1]
        for b in W:
            b.w = tok
            b.r = {}

    def dma(self, qn, out, in_, R=(), W=(), dst=None):
        eng = self.E[qn]
        waits = self._waits(eng, R, W)
        s = dst.sem
        self.semcnt[s] += 16
        tok = (s, self.semcnt[s])
        eng.prog.append((waits, (lambda h, o=out, i=in_: h.dma_start(out=o, in_=i)), s, 16))
        for b in R:
            if b.r.get(s, 0) < tok[1]:
                b.r[s] = tok[1]
        for b in W:
            b.w = tok
            b.r = {}

    def idma(self, qn, fn, R=(), W=(), dst=None):
        eng = self.E[qn]
        waits = self._waits(eng, R, W)
        s = dst.sem
        self.semcnt[s] += 16
        tok = (s, self.semcnt[s])
        eng.prog.append((waits, fn, s, 16))
        for b in R:
            if b.r.get(s, 0) < tok[1]:
                b.r[s] = tok[1]
        for b in W:
            b.w = tok
            b.r = {}

    def barrier(self):
        for en, eng in self.E.items():
            waits = []
            for o in self.E.values():
                if o is eng or o.n == 0:
                    continue
                if eng.seen.get(o.sem, 0) < o.n:
                    eng.seen[o.sem] = o.n
                    waits.append((o.sem, o.n))
            for s in range(5, len(self.sems)):
                v = self.semcnt[s]
                if v and eng.seen.get(s, 0) < v:
                    eng.seen[s] = v
                    waits.append((s, v))
            if waits:
                eng.prog.append((waits, None, None, 0))

    def mm(self, out, lhsT, rhs, start, stop, R, W):
        self.op("pe", lambda h: h.matmul(out, lhsT, rhs, start=start, stop=stop), R, W)

    def tr(self, out, in_, ident, R, W):
        self.op("pe", lambda h: h.transpose(out, in_, ident), R, W)

    def act(self, out, in_, func, R, W, bias=None, scale=None):
        kw = {}
        if bias is not None:
            kw["bias"] = bias
        if scale is not None:
            kw["scale"] = scale
        self.op("act", lambda h: h.activation(out=out, in_=in_, func=func, **kw), R, W)

    def replay(self, en, h):
        for waits, fn, s, inc in self.E[en].prog:
            for ws, wv in waits:
                h.wait_ge(self.sems[ws], wv)
            if fn is not None:
                fn(h).then_inc(self.sems[s], inc)


class Arena:
    def __init__(self, t):
        self.t = t
        self.off = 0

    def reset(self):
        self.off = 0

    def alloc(self, shape, dt):
        esz = 2 if dt == BF16 else 4
        n = 1
        for s in shape[1:]:
            n *= s
        nbytes = (n * esz + 63) // 64 * 64
        a = self.off
        self.off += nbytes
        assert self.off <= ARENA_BYTES, ("arena overflow", self.off)
        v = self.t[0:shape[0], a // 2:(a + n * esz) // 2]
        if dt != BF16:
            v = v.bitcast(dt)
        if len(shape) == 3:
            v = v.rearrange("p (a b) -> p a b", a=shape[1])
        elif len(shape) == 4:
            v = v.rearrange("p (a b c) -> p a b c", a=shape[1], b=shape[2])
        return v


def build_nc(S, layers, n_layers_total, dbg=False, stop=None):
    NT = S // 128
    NG = S // 512
    NBLK = S // 1024
    nc = bass.Bass("TRN2", target_bir_lowering=False)

    def din(name, shape, dt=F32):
        return nc.dram_tensor(name, shape, dt, kind="ExternalInput").ap()

    def dscr(name, shape, dt):
        return nc.dram_tensor(name, shape, dt, kind=("ExternalOutput" if dbg else "Internal")).ap()

    x_in = din("x", [S, D])
    lng = [din("ln1_g", [4, 128, D]), din("ln2_g", [4, 128, D])]
    lnb = [din("ln1_b", [4, 128, D]), din("ln2_b", [4, 128, D])]
    ev_w_in = din("ev_w_in", [2, D, 2056])
    ev_negb = din("ev_negb", [2, 8, 1])
    ev_pool_w = din("ev_pool_w", [2, 4, 128, 128])
    ev_pool_scale = din("ev_pool_scale", [2, 4, 128, 1])
    ev_w_out = din("ev_w_out", [2, D, D])
    ev_g = din("ev_ffn_gate", [2, 1, D, DFF])
    ev_u = din("ev_ffn_up", [2, 1, D, DFF])
    ev_d = din("ev_ffn_down", [2, 1, DFF, D])
    od_w_in = din("od_w_in", [2, D, 3072])
    od_w_out = din("od_w_out", [2, D, D])
    od_router = din("od_router", [2, 8, 128, D])
    od_g = din("od_exp_gate", [2, NE, D, DFF])
    od_u = din("od_exp_up", [2, NE, D, DFF])
    od_d = din("od_exp_down", [2, NE, DFF, D])
    c_ident_bf = din("c_ident_bf", [128, 128], BF16)
    c_ident_f = din("c_ident_f", [128, 128])
    c_tri = din("c_tri", [128, 128], BF16)
    c_koh = din("c_koh", [16, S], BF16)
    c_past = din("c_past", [NG, 128, 4, 16])
    c_own = din("c_own", [NG, 128, 4, 16])
    c_invcnt = din("c_invcnt", [4, 128, 16])
    NTILE = 2 * S // 512 + 8
    NSLOT = NTILE * 512
    c_ustrict = din("c_ustrict", [128, 128], BF16)
    c_misc = din("c_misc", [128, 64])
    y_out = nc.dram_tensor("y", [S, D], F32, kind="ExternalOutput").ap()

    XR = [dscr("xr0", [S, D], F32), dscr("xr1", [S, D], F32)]
    XT = dscr("xT", [D, S], BF16)
    QT = dscr("qT", [D, S], BF16)
    KT = dscr("kT", [D, S], BF16)
    VV = dscr("vv", [S, D], BF16)
    CATT = dscr("catT", [D, S], BF16)
    FAUG = dscr("faug", [8, 3, S], BF16)
    NFC = dscr("nfc", [128, NT * 8], F32)
    MB = dscr("mb", [256, S], BF16)
    COMB = dscr("comb", [S, 8], F32)
    ROUTE = dscr("route", [S, 24], F32)
    CNT = dscr("cnt", [128, 8], F32)
    XS = dscr("xs", [NSLOT, D], BF16)
    YS = dscr("ys", [NSLOT, D], F32)

    from contextlib import ExitStack
    with ExitStack() as st:
        arena_t = st.enter_context(nc.sbuf_tensor("arena", [128, ARENA_BYTES // 2], BF16))
        PS = [st.enter_context(nc.psum_tensor("ps%d" % i, [128, 512], F32)) for i in range(7)]
        PSB = st.enter_context(nc.psum_tensor("psb", [128, 1024], BF16))
        NSEM = 60
        sems = [st.enter_context(nc.semaphore("s%d" % i)) for i in range(NSEM)]
        K = Kern(nc, sems)
        A = Arena(arena_t)
        psb = [K.buf("ps%d" % i) for i in range(7)]
        psbb = K.buf("psb")
        PSBv = PSB[:, :].rearrange("p (a b) -> p a b", a=8)

        B_xin = K.buf("x_in")
        B_w = K.buf("weights")
        B_XR = [K.buf("xr0", True), K.buf("xr1", True)]
        B_XT = K.buf("xT", True)
        B_QT = K.buf("qT", True)
        B_KT = K.buf("kT", True)
        B_VV = K.buf("vv", True)
        B_CATT = K.buf("catT", True)
        B_FAUG = K.buf("faug", True)
        B_NFC = K.buf("nfc", True)
        B_MB = K.buf("mb", True)
        B_COMB = K.buf("comb", True)
        B_ROUTE = K.buf("route", True)
        B_CNT = K.buf("cnt", True)
        B_XS = K.buf("xs", "sw")
        B_YS = K.buf("ys", True)
        B_Y = K.buf("y", True)
        K.phase_sems = []
        K.phase_sems_sw = []

        def consts_common():
            ident = A.alloc([128, 128], BF16)
            b = K.buf("ident", True)
            K.dma("sp", ident, c_ident_bf[:, :], R=[B_w], W=[b], dst=b)
            return ident, b

        def emit_xT(xb_ap, xb_buf, tt, ident, identb, stg, stgb):
            for kt in range(8):
                K.tr(PSBv[:, kt, :], xb_ap[:, kt * 128:(kt + 1) * 128], ident, R=[xb_buf, identb], W=[psbb])
            K.op("dve", lambda h: h.tensor_copy(out=stg, in_=PSBv), R=[psbb], W=[stgb])
            K.dma("sp", XT[:, tt * 128:(tt + 1) * 128].rearrange("(kt p) t -> p kt t", p=128), stg,
                  R=[stgb], W=[B_XT], dst=B_XT)

        def layer_norm(z, zb, which, L, small, smallb, g_ap, b_ap, gbb):
            st6 = small[:, 0:12].rearrange("p (a b) -> p a b", a=2)
            K.op("dve", lambda h: h.bn_stats(out=st6[:, 0, :], in_=z[:, 0:512]), R=[zb], W=[smallb])
            K.op("dve", lambda h: h.bn_stats(out=st6[:, 1, :], in_=z[:, 512:1024]), R=[zb], W=[smallb])
            K.op("dve", lambda h: h.bn_aggr(out=small[:, 12:14], in_=small[:, 0:12]), R=[smallb], W=[smallb])
            K.act(small[:, 14:15], small[:, 13:14], AF.Sqrt, R=[smallb], W=[smallb], bias=EPS, scale=1.0)
            K.op("dve", lambda h: h.reciprocal(out=small[:, 15:16], in_=small[:, 14:15]), R=[smallb], W=[smallb])
            K.op("dve", lambda h: h.tensor_scalar(out=z, in0=z, scalar1=small[:, 12:13], scalar2=small[:, 15:16],
                                                  op0=ALU.subtract, op1=ALU.mult), R=[zb, smallb], W=[zb])
            K.op("pool", lambda h: h.tensor_tensor(out=z, in0=z, in1=g_ap, op=ALU.mult), R=[zb, gbb], W=[zb])
            K.op("pool", lambda h: h.tensor_tensor(out=z, in0=z, in1=b_ap, op=ALU.add), R=[zb, gbb], W=[zb])

        def phase_p0():
            A.reset()
            ident, identb = consts_common()
            xt = [A.alloc([128, D], F32) for _ in range(2)]
            xtb = [K.buf("p0x%d" % i, True) for i in range(2)]
            xb = [A.alloc([128, D], BF16) for _ in range(2)]
            xbb = [K.buf("p0xb%d" % i) for i in range(2)]
            stg = [A.alloc([128, 8, 128], BF16) for _ in range(2)]
            stgb = [K.buf("p0s%d" % i) for i in range(2)]
            for tt in range(NT):
                i = tt % 2
                K.dma("sp", xt[i], x_in[tt * 128:(tt + 1) * 128, :], R=[B_xin], W=[xtb[i]], dst=xtb[i])
                K.act(xb[i], xt[i], AF.Copy, R=[xtb[i]], W=[xbb[i]])
                emit_xT(xb[i], xbb[i], tt, ident, identb, stg[i], stgb[i])
            K.end_phase()

        def phase_p1(L):
            even = (L % 2 == 0)
            li = L // 2
            A.reset()
            xTs = A.alloc([128, 8, S], BF16)
            xTsb = [K.buf("xTs%d" % g, True) for g in range(NG)]
            for g in range(NG):
                K.dma("sp", xTs[:, :, g * 512:(g + 1) * 512],
                      XT[:, g * 512:(g + 1) * 512].rearrange("(kt p) t -> p kt t", p=128),
                      R=[B_XT], W=[xTsb[g]], dst=xTsb[g])
            w_in = ev_w_in if even else od_w_in
            nqc = 4 if even else 8
            qoff, koff, voff = (0, 512, 1024) if even else (0, 1024, 2048)
            nvh = 1 if even else 2
            wslot = [A.alloc([128, 8, 512], BF16) for _ in range(2)]
            wslotb = [K.buf("wslot%d" % i, "sw") for i in range(2)]
            stg = [A.alloc([128, 512], BF16) for _ in range(3)]
            stgb = [K.buf("p1stg%d" % i) for i in range(3)]
            cnt = {"w": 0, "s": 0, "p": 0}

            def load_w(col0, ncols=512):
                i = cnt["w"] % 2
                cnt["w"] += 1
                K.dma("pool", wslot[i][:, :, 0:ncols],
                      w_in[li, :, col0:col0 + ncols].rearrange("(kt p) f -> p kt f", p=128),
                      R=[B_w], W=[wslotb[i]], dst=wslotb[i])
                return wslot[i], wslotb[i]

            def next_ps():
                i = cnt["p"] % 6
                cnt["p"] += 1
                return PS[i], psb[i]

            def next_stg():
                i = cnt["s"] % 3
                cnt["s"] += 1
                return stg[i], stgb[i]

            if not even:
                ident, identb = consts_common()
                kmT = A.alloc([128, 16], BF16)
                kmTb = K.buf("kmT")
                kmf = A.alloc([128, 16], F32)
                kmfb = K.buf("kmf")
                K.op("pool", lambda h: h.memset(kmf, 0.0), R=[], W=[kmfb])
                gm = A.alloc([128, 4, 2, 16], F32)
                gmb = K.buf("gm")
                m8 = A.alloc([128, 8, 8], F32)
                m8b = K.buf("m8")
                selb_t = A.alloc([128, 4, 2, 16], F32)
                selbb = K.buf("sel")
                mbt = A.alloc([128, 4, 2, 16], BF16)
                mbtb = K.buf("mbt")
                mbs = A.alloc([32, 512], BF16)
                mbsb = K.buf("mbs")
                cpast = A.alloc([128, NG, 4, 16], F32)
                cown = A.alloc([128, NG, 4, 16], F32)
                cpb = K.buf("cpast", True)
                for g in range(NG):
                    K.dma("sp", cpast[:, g], c_past[g], R=[B_w], W=[cpb], dst=cpb)
                    K.dma("sp", cown[:, g], c_own[g], R=[B_w], W=[cpb], dst=cpb)

            for which in ("k", "q"):
                off = koff if which == "k" else qoff
                dstT, dstB = (KT, B_KT) if which == "k" else (QT, B_QT)
                for cg in range(nqc // 4):
                    wt, wtb = load_w(off + cg * 512)
                    for c4 in range(4):
                        ct = cg * 4 + c4
                        for g in range(NG):
                            ps, pb = next_ps()
                            for kt in range(8):
                                K.mm(ps[:, :], wt[:, kt, c4 * 128:(c4 + 1) * 128], xTs[:, kt, g * 512:(g + 1) * 512],
                                     kt == 0, kt == 7, R=[wtb, xTsb[g]], W=[pb])
                            sg, sgb = next_stg()
                            if which == "k":
                                if even:
                                    K.act(sg, ps[:, :], AF.Copy, R=[pb], W=[sgb])
                                else:
                                    for bb in range(2):
                                        K.op("act", lambda h, sg=sg, ps=ps, bb=bb, g=g: h.activation(
                                            out=sg[:, bb * 256:(bb + 1) * 256], in_=ps[:, bb * 256:(bb + 1) * 256], func=AF.Copy,
                                            accum_out=kmf[:, 2 * g + bb:2 * g + bb + 1]), R=[pb], W=[sgb, kmfb])
                            else:
                                K.act(sg, ps[:, :], AF.Copy, R=[pb], W=[sgb], scale=0.125)
                            K.dma("sp", dstT[ct * 128:(ct + 1) * 128, g * 512:(g + 1) * 512], sg,
                                  R=[sgb], W=[dstB], dst=dstB)
                            if (not even) and which == "q":
                                gp, gpb = next_ps()
                                for t4 in range(4):
                                    K.mm(gp[:, t4 * 32:(t4 + 1) * 32], sg[:, t4 * 128:(t4 + 1) * 128], kmTs[ct][:, :], True, True,
                                         R=[sgb, kmTsb[ct]], W=[gpb])
                                gpv = gp[:, 0:128].rearrange("p (a b c) -> p a b c", a=4, b=2)
                                for hh in range(2):
                                    K.op("dve", lambda h, o=gm[:, :, hh, :], i=gpv[:, :, hh, :], c=cpast[:, g]:
                                         h.tensor_tensor(out=o, in0=i, in1=c, op=ALU.add), R=[gpb, cpb], W=[gmb])
                                for t4 in range(4):
                                    for hh in range(2):
                                        K.op("dve", lambda h, o=m8[:, t4 * 2 + hh, :], i=gm[:, t4, hh, :]: h.max(out=o, in_=i),
                                             R=[gmb], W=[m8b])
                                for t4 in range(4):
                                    for hh in range(2):
                                        K.op("dve", lambda h, o=selb_t[:, t4, hh, :], i=gm[:, t4, hh, :], s=m8[:, t4 * 2 + hh, 2:3]:
                                             h.tensor_scalar(out=o, in0=i, scalar1=s, scalar2=None, op0=ALU.is_ge),
                                             R=[gmb, m8b], W=[selbb])
                                for hh in range(2):
                                    K.op("dve", lambda h, o=selb_t[:, :, hh, :], c=cown[:, g]:
                                         h.tensor_tensor(out=o, in0=o, in1=c, op=ALU.max), R=[selbb, cpb], W=[selbb])
                                K.op("dve", lambda h: h.tensor_scalar(out=mbt, in0=selb_t, scalar1=-NEG, scalar2=NEG,
                                                                      op0=ALU.mult, op1=ALU.add), R=[selbb], W=[mbtb])
                                for t4 in range(4):
                                    K.tr(PSB[0:32, t4 * 128:(t4 + 1) * 128],
                                         mbt[:, t4, :, :].rearrange("p a b -> p (a b)"), ident, R=[mbtb, identb], W=[psbb])
                                K.op("dve", lambda h: h.tensor_copy(out=mbs, in_=PSB[0:32, 0:512]), R=[psbb], W=[mbsb])
                                K.dma("sp", MB[ct * 32:(ct + 1) * 32, g * 512:(g + 1) * 512], mbs, R=[mbsb], W=[B_MB], dst=B_MB)
                        if (not even) and which == "k":
                            if ct == 0:
                                kmTs = [A.alloc([128, 32], BF16) for _ in range(nqc)]
                                kmTsb = [K.buf("kmTs%d" % i) for i in range(nqc)]
                                for i_ in range(nqc):
                                    K.op("pool", lambda h, o=kmTs[i_]: h.memset(o, 0.0), R=[], W=[kmTsb[i_]])
                            for hh in range(2):
                                K.op("dve", lambda h, o=kmTs[ct][hh * 64:(hh + 1) * 64, hh * 16:(hh + 1) * 16], i_=kmf[hh * 64:(hh + 1) * 64, :]:
                                     h.tensor_scalar(out=o, in0=i_, scalar1=1.0 / 256, scalar2=None, op0=ALU.mult),
                                     R=[kmfb], W=[kmTsb[ct]])

            for vh in range(nvh):
                wt, wtb = load_w(voff + vh * 512)
                for tt in range(NT):
                    ps, pb = next_ps()
                    g = tt // 4
                    for kt in range(8):
                        K.mm(ps[:, :], xTs[:, kt, tt * 128:(tt + 1) * 128], wt[:, kt, :], kt == 0, kt == 7,
                             R=[wtb, xTsb[g]], W=[pb])
                    sg, sgb = next_stg()
                    K.act(sg, ps[:, :], AF.Copy, R=[pb], W=[sgb])
                    K.dma("sp", VV[tt * 128:(tt + 1) * 128, vh * 512:(vh + 1) * 512], sg, R=[sgb], W=[B_VV], dst=B_VV)

            if even:
                wt, wtb = load_w(1536, 8)
                negb = A.alloc([8, 1], F32)
                negbb = K.buf("negb", True)
                K.dma("sp", negb, ev_negb[li], R=[B_w], W=[negbb], dst=negbb)
                K.op("dve", lambda h: h.tensor_scalar(out=negb, in0=negb, scalar1=-1.0, scalar2=None, op0=ALU.mult),
                     R=[negbb], W=[negbb])
                f_mark = A.off
                cs = [A.alloc([8, S], F32) for _ in range(2)]
                csb = [K.buf("cs%d" % i) for i in range(2)]
                for g in range(NG):
                    ps, pb = next_ps()
                    for kt in range(8):
                        K.mm(ps[0:8, :], wt[:, kt, 0:8], xTs[:, kt, g * 512:(g + 1) * 512], kt == 0, kt == 7,
                             R=[wtb, xTsb[g]], W=[pb])
                    K.act(cs[1][:, g * 512:(g + 1) * 512], ps[0:8, :], AF.Exp, R=[pb, negbb], W=[csb[1]], bias=negb, scale=-1.0)
                K.act(cs[0], cs[1], AF.Ln, R=[csb[1]], W=[csb[0]], bias=1.0, scale=1.0)
                cur = 0
                sh = 1
                while sh < S:
                    o, i_ = cs[1 - cur], cs[cur]
                    K.op("dve", lambda h, o=o, i_=i_, sh=sh: h.tensor_tensor(out=o[:, sh:S], in0=i_[:, sh:S], in1=i_[:, 0:S - sh], op=ALU.add),
                         R=[csb[cur]], W=[csb[1 - cur]])
                    K.op("dve", lambda h, o=o, i_=i_, sh=sh: h.tensor_copy(out=o[:, 0:sh], in_=i_[:, 0:sh]),
                         R=[csb[cur]], W=[csb[1 - cur]])
                    cur = 1 - cur
                    sh *= 2
                C, Cb = cs[cur], csb[cur]
                r1, r1b = cs[1 - cur], csb[1 - cur]
                hb = [A.alloc([8, S], BF16) for _ in range(3)]
                hbb = [K.buf("hb%d" % i) for i in range(3)]
                h32 = A.alloc([8, S], F32)
                h32b = K.buf("h32")
                K.op("dve", lambda h: h.tensor_scalar(out=hb[0], in0=C, scalar1=-1.0, scalar2=None, op0=ALU.mult), R=[Cb], W=[hbb[0]])
                K.op("dve", lambda h: h.tensor_copy(out=h32, in_=hb[0]), R=[hbb[0]], W=[h32b])
                K.op("dve", lambda h: h.scalar_tensor_tensor(out=r1, in0=C, scalar=-1.0, in1=h32, op0=ALU.mult, op1=ALU.subtract),
                     R=[Cb, h32b], W=[r1b])
                K.op("dve", lambda h: h.tensor_copy(out=hb[1], in_=r1), R=[r1b], W=[hbb[1]])
                K.op("dve", lambda h: h.tensor_copy(out=h32, in_=hb[1]), R=[hbb[1]], W=[h32b])
                K.op("dve", lambda h: h.tensor_tensor(out=r1, in0=r1, in1=h32, op=ALU.subtract), R=[r1b, h32b], W=[r1b])
                K.op("dve", lambda h: h.tensor_copy(out=hb[2], in_=r1), R=[r1b], W=[hbb[2]])
                for j in range(3):
                    K.dma("sp", FAUG[:, j, :], hb[j], R=[hbb[j]], W=[B_FAUG], dst=B_FAUG)
                identf = A.alloc([8, 8], F32)
                identfb = K.buf("identf", True)
                K.dma("sp", identf, c_ident_f[0:8, 0:8], R=[B_w], W=[identfb], dst=identfb)
                ps, pb = next_ps()
                for kt in range(NT):
                    K.tr(ps[:, kt * 8:(kt + 1) * 8], C[:, kt * 128:(kt + 1) * 128], identf, R=[Cb, identfb], W=[pb])
                nfc = A.alloc([128, NT * 8], F32)
                nfcb = K.buf("nfc_s")
                K.op("dve", lambda h, ps=ps: h.tensor_copy(out=nfc, in_=ps[:, 0:NT * 8]), R=[pb], W=[nfcb])
                K.dma("sp", NFC[:, :], nfc, R=[nfcb], W=[B_NFC], dst=B_NFC)
                K.barrier()
                A.off = f_mark

                wt, wtb = load_w(1544)
                pw = A.alloc([128, 4, 128], BF16)
                pwb = K.buf("pw", "sw")
                for g4 in range(4):
                    K.dma("pool", pw[:, g4, :], ev_pool_w[li, g4], R=[B_w], W=[pwb], dst=pwb)
                psc = A.alloc([128, 4], F32)
                pscb = K.buf("psc", True)
                for g4 in range(4):
                    K.dma("sp", psc[:, g4:g4 + 1], ev_pool_scale[li, g4], R=[B_w], W=[pscb], dst=pscb)
                icn = A.alloc([128, 4, 16], F32)
                icnb = K.buf("icn", True)
                for g4 in range(4):
                    K.dma("sp", icn[:, g4, :], c_invcnt[g4], R=[B_w], W=[icnb], dst=icnb)
                uu = A.alloc([128, S], F32)
                uub = K.buf("uu")
                sa = [A.alloc([128, S], F32) for _ in range(2)]
                sab = [K.buf("sa%d" % i) for i in range(2)]
                mx = A.alloc([128, S], BF16)
                mxb = K.buf("mx")
                fix = A.alloc([128, 16], F32)
                fixb = K.buf("fix")
                for g4 in range(4):
                    w = POOL_W[g4]
                    for g in range(NG):
                        ps, pb = next_ps()
                        for kt in range(8):
                            K.mm(ps[:, :], wt[:, kt, g4 * 128:(g4 + 1) * 128], xTs[:, kt, g * 512:(g + 1) * 512],
                                 kt == 0, kt == 7, R=[wtb, xTsb[g]], W=[pb])
                        K.act(uu[:, g * 512:(g + 1) * 512], ps[:, :], AF.Copy, R=[pb], W=[uub])
                    src, srcb = uu, uub
                    sh = 1
                    k = 0
                    while sh < w:
                        o, ob = sa[k % 2], sab[k % 2]
                        K.op("dve", lambda h, o=o, i_=src, sh=sh: h.tensor_tensor(out=o[:, sh:S], in0=i_[:, sh:S], in1=i_[:, 0:S - sh], op=ALU.add),
                             R=[srcb], W=[ob])
                        K.op("pool", lambda h, o=o, i_=src, sh=sh: h.tensor_copy(out=o[:, 0:sh], in_=i_[:, 0:sh]), R=[srcb], W=[ob])
                        src, srcb = o, ob
                        sh *= 2
                        k += 1
                    K.op("dve", lambda h, s_=src, w=w: h.scalar_tensor_tensor(out=mx, in0=s_, scalar=1.0 / w, in1=uu, op0=ALU.mult, op1=ALU.subtract),
                         R=[srcb, uub], W=[mxb])
                    K.op("dve", lambda h, s_=src, w=w, g4=g4: h.tensor_tensor(out=fix[:, 0:w - 1], in0=s_[:, 0:w - 1], in1=icn[:, g4, 0:w - 1], op=ALU.mult),
                         R=[srcb, icnb], W=[fixb])
                    K.op("dve", lambda h, w=w: h.tensor_tensor(out=mx[:, 0:w - 1], in0=fix[:, 0:w - 1], in1=uu[:, 0:w - 1], op=ALU.subtract),
                         R=[fixb, uub, mxb], W=[mxb])
                    for g in range(NG):
                        ps, pb = next_ps()
                        K.mm(ps[:, :], pw[:, g4, :], mx[:, g * 512:(g + 1) * 512], True, True, R=[pwb, mxb], W=[pb])
                        sg, sgb = next_stg()
                        K.op("dve", lambda h, sg=sg, ps=ps, g4=g4: h.tensor_scalar(out=sg, in0=ps[:, :], scalar1=psc[:, g4:g4 + 1], scalar2=None, op0=ALU.mult),
                             R=[pb, pscb], W=[sgb])
                        K.dma("sp", CATT[512 + g4 * 128:512 + (g4 + 1) * 128, g * 512:(g + 1) * 512], sg, R=[sgb], W=[B_CATT], dst=B_CATT)
            K.end_phase()

        def phase_p2(L):
            even = (L % 2 == 0)
            H = 8 if even else 16
            aug = 3 if even else 16
            KA = 64 + aug
            A.reset()
            qa = [A.alloc([128, S], BF16) for _ in range(2)]
            ka = [A.alloc([128, S], BF16) for _ in range(2)]
            va = [A.alloc([128, NT, 128], BF16) for _ in range(2)]
            qab = [K.buf("qa%d" % i, True) for i in range(2)]
            kab = [K.buf("ka%d" % i, True) for i in range(2)]
            vab = [K.buf("va%d" % i, True) for i in range(2)]
            pt = [A.alloc([128, 512], BF16) for _ in range(4)]
            ptb = [K.buf("pt%d" % i) for i in range(4)]
            rr = [A.alloc([128, 512], F32) for _ in range(2)]
            rrb = [K.buf("rr%d" % i) for i in range(2)]
            ot = [A.alloc([64, 512], BF16) for _ in range(2)]
            otb = [K.buf("ot%d" % i) for i in range(2)]
            tri = A.alloc([128, 128], BF16)
            trib = K.buf("tri", True)
            K.dma("sp", tri, c_tri[:, :], R=[B_w], W=[trib], dst=trib)
            if even:
                nfc = A.alloc([128, NT * 8], F32)
                nfcb = K.buf("nfc2", True)
                K.dma("sp", nfc, NFC[:, :], R=[B_NFC], W=[nfcb], dst=nfcb)
            for i in range(2):
                K.op("pool", lambda h, i=i: h.memset(va[i][:, :, 64:128], 1.0), R=[], W=[vab[i]])
                if even:
                    K.op("pool", lambda h, i=i: h.memset(ka[i][64:67, :], 1.0), R=[], W=[kab[i]])
                else:
                    K.dma("sp", ka[i][64:80, :], c_koh[:, :], R=[B_w], W=[kab[i]], dst=kab[i])

            def load_head(hd):
                b = hd % 2
                K.dma("sp", qa[b][0:64, :], QT[hd * 64:(hd + 1) * 64, :], R=[B_QT], W=[qab[b]], dst=qab[b])
                if even:
                    K.dma("sp", qa[b][64:67, :], FAUG[hd], R=[B_FAUG], W=[qab[b]], dst=qab[b])
                else:
                    K.dma("sp", qa[b][64:80, :], MB[hd * 16:(hd + 1) * 16, :], R=[B_MB], W=[qab[b]], dst=qab[b])
                K.dma("sp", ka[b][0:64, :], KT[hd * 64:(hd + 1) * 64, :], R=[B_KT], W=[kab[b]], dst=kab[b])
                for j in range(S // 1024):
                    K.dma("sp", va[b][:, 8 * j:8 * j + 8, 0:64],
                          VV[j * 1024:(j + 1) * 1024, hd * 64:(hd + 1) * 64].rearrange("(kt p) d -> p kt d", p=128),
                          R=[B_VV], W=[vab[b]], dst=vab[b])

            its = []
            for hd in range(H):
                for g in range(NG):
                    for kt in range(4 * g + 4):
                        its.append((hd, g, kt))
            SPS = [(PS[i], psb[i]) for i in range(3)]
            OPS = [(PS[3 + i], psb[3 + i]) for i in range(2)]

            def emit_s(idx):
                hd, g, kt = its[idx]
                b = hd % 2
                j = kt - 4 * g
                c0 = 128 * j if j > 0 else 0
                n = 512 - c0
                sp_, spb = SPS[idx % 3]
                K.mm(sp_[:, 0:n], ka[b][0:KA, kt * 128:(kt + 1) * 128], qa[b][0:KA, g * 512 + c0:(g + 1) * 512],
                     True, True, R=[kab[b], qab[b]], W=[spb])
                p, pb_ = pt[idx % 4], ptb[idx % 4]
                if even:
                    K.act(p[:, 0:n], sp_[:, 0:n], AF.Exp, R=[spb, nfcb], W=[pb_], bias=nfc[:, kt * 8 + hd:kt * 8 + hd + 1], scale=1.0)
                else:
                    K.act(p[:, 0:n], sp_[:, 0:n], AF.Exp, R=[spb], W=[pb_])
                if j >= 0:
                    K.op("pool", lambda h, p=p: h.tensor_tensor(out=p[:, 0:128], in0=p[:, 0:128], in1=tri, op=ALU.mult),
                         R=[pb_, trib], W=[pb_])

            def emit_pv(idx):
                hd, g, kt = its[idx]
                b = hd % 2
                j = kt - 4 * g
                c0 = 128 * j if j > 0 else 0
                n = 512 - c0
                last = 4 * g + 3
                gi = hd * NG + g
                if g == 0 and kt == 0 and hd >= 1 and hd + 1 < H:
                    load_head(hd + 1)
                o, ob = OPS[gi % 2]
                p, pb_ = pt[idx % 4], ptb[idx % 4]
                K.mm(o[:, c0:512], va[b][:, kt, :], p[:, 0:n], kt == 0, kt == last, R=[vab[b], pb_], W=[ob])
                if kt == last:
                    r, rb = rr[gi % 2], rrb[gi % 2]
                    t_, tb = ot[gi % 2], otb[gi % 2]
                    K.op("dve", lambda h: h.reciprocal(out=r[64:128, :], in_=o[64:128, :]), R=[ob], W=[rb])
                    K.op("dve", lambda h: h.tensor_tensor(out=t_, in0=o[0:64, :], in1=r[64:128, :], op=ALU.mult), R=[ob, rb], W=[tb])
                    K.dma("sp", CATT[hd * 64:(hd + 1) * 64, g * 512:(g + 1) * 512], t_, R=[tb], W=[B_CATT], dst=B_CATT)

            n_it = len(its)
            load_head(0)
            if H > 1:
                load_head(1)
            emit_s(0)
            if n_it > 1:
                emit_s(1)
            for idx in range(n_it):
                emit_pv(idx)
                if idx + 2 < n_it:
                    emit_s(idx + 2)
            K.end_phase()

        def phase_p3(L, xr_src, xr_src_b, xr_dst, xr_dst_b):
            even = (L % 2 == 0)
            li = L // 2
            A.reset()
            ident, identb = consts_common()
            wo = A.alloc([128, 8, D], BF16)
            wob = K.buf("wo", "sw")
            w_out = ev_w_out if even else od_w_out
            for hf in range(2):
                K.dma("pool", wo[:, :, hf * 512:(hf + 1) * 512],
                      w_out[li, :, hf * 512:(hf + 1) * 512].rearrange("(kt p) f -> p kt f", p=128), R=[B_w], W=[wob], dst=wob)
            g_ap = A.alloc([128, D], F32)
            b_ap = A.alloc([128, D], F32)
            gbb = K.buf("gb", True)
            K.dma("sp", g_ap, lng[0][L], R=[B_w], W=[gbb], dst=gbb)
            K.dma("sp", b_ap, lnb[0][L], R=[B_w], W=[gbb], dst=gbb)
            ct = [A.alloc([128, 8, 512], BF16) for _ in range(2)]
            ctb = [K.buf("ct%d" % i, True) for i in range(2)]
            xt = [A.alloc([128, D], F32) for _ in range(4)]
            xtb = [K.buf("p3x%d" % i, True) for i in range(4)]
            z = [A.alloc([128, D], F32) for _ in range(4)]
            zb = [K.buf("p3z%d" % i) for i in range(4)]
            xb = [A.alloc([128, D], BF16) for _ in range(4)]
            xbb = [K.buf("p3xb%d" % i) for i in range(4)]
            stg = [A.alloc([128, 8, 128], BF16) for _ in range(4)]
            stgb = [K.buf("p3s%d" % i) for i in range(4)]
            small = [A.alloc([128, 16], F32) for _ in range(4)]
            smallb = [K.buf("p3sm%d" % i) for i in range(4)]
            if not even:
                wr = A.alloc([128, 8, D], F32)
                wrb = K.buf("wr", True)
                for e in range(8):
                    K.dma("sp", wr[:, e, :], od_router[li, e], R=[B_w], W=[wrb], dst=wrb)
                junk = A.alloc([128, D], F32)
                junkb = K.buf("junk")
                rt = [A.alloc([128, 96], F32) for _ in range(4)]
                rtb = [K.buf("rt%d" % i) for i in range(4)]
                for i_ in range(4):
                    K.op("pool", lambda h, i_=i_: h.memset(rt[i_], 0.0), R=[], W=[rtb[i_]])
                ohb = [A.alloc([128, 8], BF16) for _ in range(4)]
                ohbb = [K.buf("ohb%d" % i) for i in range(4)]
                ustr = A.alloc([128, 128], BF16)
                ustrb = K.buf("ustr", True)
                K.dma("sp", ustr, c_ustrict[:, :], R=[B_w], W=[ustrb], dst=ustrb)
                onesb = A.alloc([128, 128], BF16)
                K.op("pool", lambda h: h.memset(onesb, 1.0), R=[], W=[ustrb])
                tot = A.alloc([128, 8], F32)
                totb = K.buf("tot")
                K.op("pool", lambda h: h.memset(tot, 0.0), R=[], W=[totb])

            def load_ct(g):
                K.dma("sp", ct[g % 2], CATT[:, g * 512:(g + 1) * 512].rearrange("(kt p) t -> p kt t", p=128),
                      R=[B_CATT], W=[ctb[g % 2]], dst=ctb[g % 2])

            load_ct(0)
            for tt in range(NT):
                g = tt // 4
                i = tt % 4
                if tt % 4 == 0 and g + 1 < NG:
                    load_ct(g + 1)
                K.dma("sp", xt[i], xr_src[tt * 128:(tt + 1) * 128, :], R=[xr_src_b], W=[xtb[i]], dst=xtb[i])
                c = ct[g % 2]
                t4 = tt % 4
                for hf in range(2):
                    ps, pb = PS[(tt * 2 + hf) % 4], psb[(tt * 2 + hf) % 4]
                    for kt in range(8):
                        K.mm(ps[:, :], c[:, kt, t4 * 128:(t4 + 1) * 128], wo[:, kt, hf * 512:(hf + 1) * 512], kt == 0, kt == 7,
                             R=[ctb[g % 2], wob], W=[pb])
                    K.op("dve", lambda h, ps=ps, hf=hf, i=i: h.scalar_tensor_tensor(
                        out=z[i][:, hf * 512:(hf + 1) * 512], in0=xt[i][:, hf * 512:(hf + 1) * 512], scalar=ALPHA,
                        in1=ps[:, :], op0=ALU.mult, op1=ALU.add), R=[pb, xtb[i]], W=[zb[i]])
                layer_norm(z[i], zb[i], 0, L, small[i], smallb[i], g_ap, b_ap, gbb)
                K.dma("sp", xr_dst[tt * 128:(tt + 1) * 128, :], z[i], R=[zb[i]], W=[xr_dst_b], dst=xr_dst_b)
                K.act(xb[i], z[i], AF.Copy, R=[zb[i]], W=[xbb[i]])
                emit_xT(xb[i], xbb[i], tt, ident, identb, stg[i], stgb[i])
                if not even:
                    r = rt[i]
                    rb = rtb[i]
                    lg, m8_, dd, g1, g2, c1, c2 = r[:, 0:8], r[:, 8:16], r[:, 16:17], r[:, 66:67], r[:, 67:68], r[:, 24:32], r[:, 32:40]
                    oh0, oh1, rank, j8 = r[:, 48:56], r[:, 56:64], r[:, 72:80], r[:, 80:88]
                    for e in range(8):
                        K.op("dve", lambda h, e=e, i=i, lg=lg: h.scalar_tensor_tensor(
                            out=junk, in0=z[i], scalar=1.0, in1=wr[:, e, :], op0=ALU.mult, op1=ALU.mult, accum_out=lg[:, e:e + 1]),
                            R=[zb[i], wrb], W=[junkb, rb])
                    K.op("dve", lambda h, m8_=m8_, lg=lg: h.max(out=m8_, in_=lg), R=[rb], W=[rb])
                    K.op("dve", lambda h, dd=dd, m8_=m8_: h.tensor_tensor(out=dd, in0=m8_[:, 1:2], in1=m8_[:, 0:1], op=ALU.subtract), R=[rb], W=[rb])
                    K.act(dd, dd, AF.Exp, R=[rb], W=[rb])
                    K.op("dve", lambda h, g1=g1, dd=dd: h.tensor_scalar(out=g1, in0=dd, scalar1=1.0, scalar2=None, op0=ALU.add), R=[rb], W=[rb])
                    K.op("dve", lambda h, g1=g1: h.reciprocal(out=g1, in_=g1), R=[rb], W=[rb])
                    K.op("dve", lambda h, g1=g1, g2=g2, dd=dd: h.tensor_tensor(out=g2, in0=dd, in1=g1, op=ALU.mult), R=[rb], W=[rb])
                    K.op("dve", lambda h, c1=c1, lg=lg, m8_=m8_, g1=g1: h.tensor_scalar(out=c1, in0=lg, scalar1=m8_[:, 0:1], scalar2=g1, op0=ALU.is_equal, op1=ALU.mult), R=[rb], W=[rb])
                    K.op("dve", lambda h, c2=c2, lg=lg, m8_=m8_, g2=g2: h.tensor_scalar(out=c2, in0=lg, scalar1=m8_[:, 1:2], scalar2=g2, op0=ALU.is_equal, op1=ALU.mult), R=[rb], W=[rb])
                    K.op("dve", lambda h, c1=c1, c2=c2: h.tensor_tensor(out=c1, in0=c1, in1=c2, op=ALU.add), R=[rb], W=[rb])
                    K.dma("sp", COMB[tt * 128:(tt + 1) * 128, :], c1, R=[rb], W=[B_COMB], dst=B_COMB)
                    K.op("dve", lambda h, oh0=oh0, lg=lg, m8_=m8_: h.tensor_scalar(out=oh0, in0=lg, scalar1=m8_[:, 0:1], scalar2=None, op0=ALU.is_equal), R=[rb], W=[rb])
                    K.op("dve", lambda h, oh1=oh1, lg=lg, m8_=m8_: h.tensor_scalar(out=oh1, in0=lg, scalar1=m8_[:, 1:2], scalar2=None, op0=ALU.is_equal), R=[rb], W=[rb])
                    K.op("dve", lambda h, i=i, oh0=oh0, oh1=oh1: h.tensor_tensor(out=ohb[i], in0=oh0, in1=oh1, op=ALU.add), R=[rb], W=[ohbb[i]])
                    pr, prb = PS[4 + tt % 2], psb[4 + tt % 2]
                    K.mm(pr[:, 0:8], ustr, ohb[i], True, True, R=[ustrb, ohbb[i]], W=[prb])
                    K.mm(pr[:, 8:16], onesb, ohb[i], True, True, R=[ustrb, ohbb[i]], W=[prb])
                    K.op("dve", lambda h, rank=rank, pr=pr: h.tensor_tensor(out=rank, in0=pr[:, 0:8], in1=tot, op=ALU.add), R=[prb, totb], W=[rb])
                    K.op("dve", lambda h, pr=pr: h.tensor_tensor(out=tot, in0=pr[:, 8:16], in1=tot, op=ALU.add), R=[prb, totb], W=[totb])
                    K.op("dve", lambda h, oh0=oh0, rank=rank, j8=j8, o=r[:, 64:65]: h.scalar_tensor_tensor(
                        out=j8, in0=oh0, scalar=1.0, in1=rank, op0=ALU.mult, op1=ALU.mult, accum_out=o), R=[rb], W=[rb])
                    K.op("dve", lambda h, oh1=oh1, rank=rank, j8=j8, o=r[:, 65:66]: h.scalar_tensor_tensor(
                        out=j8, in0=oh1, scalar=1.0, in1=rank, op0=ALU.mult, op1=ALU.mult, accum_out=o), R=[rb], W=[rb])
                    K.dma("sp", ROUTE[tt * 128:(tt + 1) * 128, :], r[:, 48:72], R=[rb], W=[B_ROUTE], dst=B_ROUTE)
            if not even:
                K.dma("sp", CNT[:, :], tot, R=[totb], W=[B_CNT], dst=B_CNT)
            K.end_phase()

        def phase_p4(L, xr_src, xr_src_b, xr_dst, xr_dst_b, last):
            even = (L % 2 == 0)
            li = L // 2
            A.reset()
            ident, identb = consts_common()
            Wg, Wu, Wd = (ev_g, ev_u, ev_d) if even else (od_g, od_u, od_d)
            experts = [0] if even else list(range(NE))
            g_ap = A.alloc([128, D], F32)
            b_ap = A.alloc([128, D], F32)
            gbb = K.buf("gb4", True)
            K.dma("sp", g_ap, lng[1][L], R=[B_w], W=[gbb], dst=gbb)
            K.dma("sp", b_ap, lnb[1][L], R=[B_w], W=[gbb], dst=gbb)
            xTb = A.alloc([128, 8, 1024], BF16)
            xTbb = K.buf("xTb", True)
            yacc = A.alloc([128, 8, D], F32)
            yab = [K.buf("yacc%d" % i, True) for i in range(8)]
            hT = A.alloc([128, 14, 1024], BF16)
            hTb = [K.buf("hT%d" % i) for i in range(14)]
            wgu = [A.alloc([128, 2, 8, 256], BF16) for _ in range(2)]
            wgub = [K.buf("wgu%d" % i, "sw") for i in range(2)]
            wd = [A.alloc([128, 14, D], BF16) for _ in range(2)]
            wdb = [K.buf("wd%d" % i, "sw") for i in range(2)]
            sg = [A.alloc([128, 512], F32) for _ in range(2)]
            sgb = [K.buf("sg%d" % i) for i in range(2)]
            xb = [A.alloc([128, D], BF16) for _ in range(2)]
            xbb = [K.buf("p4xb%d" % i) for i in range(2)]
            stg = [A.alloc([128, 8, 128], BF16) for _ in range(2)]
            stgb = [K.buf("p4s%d" % i) for i in range(2)]
            small = [A.alloc([128, 16], F32) for _ in range(2)]
            smallb = [K.buf("p4sm%d" % i) for i in range(2)]
            comb = A.alloc([128, 8, 8], F32)
            combb = K.buf("comb_s", True)

            gl = []
            dl = []
            for blk in range(NBLK):
                for e in experts:
                    for hf in range(2):
                        dl.append((blk, e, hf))
                        for fg in range(7):
                            gl.append((blk, e, hf, fg))
            st_ = {"g": 0, "d": 0}

            def load_g(n):
                if n >= len(gl):
                    return
                blk, e, hf, fg = gl[n]
                f0 = hf * 1792 + fg * 256
                i = n % 2
                K.dma("pool", wgu[i][:, 0], Wg[li, e, :, f0:f0 + 256].rearrange("(kt p) f -> p kt f", p=128), R=[B_w], W=[wgub[i]], dst=wgub[i])
                K.dma("pool", wgu[i][:, 1], Wu[li, e, :, f0:f0 + 256].rearrange("(kt p) f -> p kt f", p=128), R=[B_w], W=[wgub[i]], dst=wgub[i])

            def load_d(n):
                if n >= len(dl):
                    return
                blk, e, hf = dl[n]
                i = n % 2
                K.dma("pool", wd[i], Wd[li, e, hf * 1792:(hf + 1) * 1792, :].rearrange("(fc p) d -> p fc d", p=128), R=[B_w], W=[wdb[i]], dst=wdb[i])

            load_g(0)
            load_d(0)
            gi = 0
            di = 0
            pcount = 0
            ycount = 0
            for blk in range(NBLK):
                t0 = blk * 1024
                K.dma("sp", xTb, XT[:, t0:t0 + 1024].rearrange("(kt p) t -> p kt t", p=128), R=[B_XT], W=[xTbb], dst=xTbb)
                for tt in range(8):
                    K.dma("sp", yacc[:, tt, :], xr_src[t0 + tt * 128:t0 + (tt + 1) * 128, :], R=[xr_src_b], W=[yab[tt]], dst=yab[tt])
                    K.op("pool", lambda h, tt=tt: h.tensor_scalar(out=yacc[:, tt, :], in0=yacc[:, tt, :], scalar1=ALPHA, scalar2=0.0, op0=ALU.mult, op1=ALU.add),
                         R=[yab[tt]], W=[yab[tt]])
                if not even:
                    K.dma("sp", comb, COMB[t0:t0 + 1024, :].rearrange("(tt p) e -> p tt e", p=128), R=[B_COMB], W=[combb], dst=combb)
                for e in experts:
                    for hf in range(2):
                        load_d(di + 1)
                        wdt, wdtb = wd[di % 2], wdb[di % 2]
                        di += 1
                        for fg in range(7):
                            load_g(gi + 1)
                            w_, w_b = wgu[gi % 2], wgub[gi % 2]
                            gi += 1
                            for fc2 in range(2):
                                fcl = fg * 2 + fc2
                                for tg in range(2):
                                    pg, pgb = PS[(pcount % 2) * 2], psb[(pcount % 2) * 2]
                                    pu, pub = PS[(pcount % 2) * 2 + 1], psb[(pcount % 2) * 2 + 1]
                                    s_, s_b = sg[pcount % 2], sgb[pcount % 2]
                                    pcount += 1
                                    for kt in range(8):
                                        K.mm(pg[:, :], w_[:, 0, kt, fc2 * 128:(fc2 + 1) * 128], xTb[:, kt, tg * 512:(tg + 1) * 512],
                                             kt == 0, kt == 7, R=[w_b, xTbb], W=[pgb])
                                    for kt in range(8):
                                        K.mm(pu[:, :], w_[:, 1, kt, fc2 * 128:(fc2 + 1) * 128], xTb[:, kt, tg * 512:(tg + 1) * 512],
                                             kt == 0, kt == 7, R=[w_b, xTbb], W=[pub])
                                    K.act(s_, pg[:, :], AF.Silu, R=[pgb], W=[s_b])
                                    K.op("dve", lambda h, fcl=fcl, tg=tg, s_=s_, pu=pu: h.tensor_tensor(
                                        out=hT[:, fcl, tg * 512:(tg + 1) * 512], in0=pu[:, :], in1=s_, op=ALU.mult),
                                        R=[pub, s_b], W=[hTb[fcl]])
                        for tt in range(8):
                            for dh in range(2):
                                py, pyb = PS[4 + ycount % 2], psb[4 + ycount % 2]
                                ycount += 1
                                for fcl in range(14):
                                    K.mm(py[:, :], hT[:, fcl, tt * 128:(tt + 1) * 128], wdt[:, fcl, dh * 512:(dh + 1) * 512],
                                         fcl == 0, fcl == 13, R=[hTb[fcl], wdtb], W=[pyb])
                                sc = 1.0 if even else comb[:, tt, e:e + 1]
                                rds = [pyb, yab[tt]] + ([] if even else [combb])
                                K.op("dve", lambda h, py=py, tt=tt, dh=dh, sc=sc: h.scalar_tensor_tensor(
                                    out=yacc[:, tt, dh * 512:(dh + 1) * 512], in0=py[:, :], scalar=sc,
                                    in1=yacc[:, tt, dh * 512:(dh + 1) * 512], op0=ALU.mult, op1=ALU.add), R=rds, W=[yab[tt]])
                for tt in range(8):
                    i = tt % 2
                    zt = yacc[:, tt, :]
                    layer_norm(zt, yab[tt], 1, L, small[i], smallb[i], g_ap, b_ap, gbb)
                    gt = blk * 8 + tt
                    if last:
                        K.dma("sp", y_out[gt * 128:(gt + 1) * 128, :], zt, R=[yab[tt]], W=[B_Y], dst=B_Y)
                    else:
                        K.dma("sp", xr_dst[gt * 128:(gt + 1) * 128, :], zt, R=[yab[tt]], W=[xr_dst_b], dst=xr_dst_b)
                        K.act(xb[i], zt, AF.Copy, R=[yab[tt]], W=[xbb[i]])
                        emit_xT(xb[i], xbb[i], gt, ident, identb, stg[i], stgb[i])
            K.end_phase()

        def phase_p4s(L, xr_src, xr_src_b, xr_dst, xr_dst_b, last):
            li = L // 2
            A.reset()
            ident, identb = consts_common()
            g_ap = A.alloc([128, D], F32)
            b_ap = A.alloc([128, D], F32)
            gbb = K.buf("gb4", True)
            K.dma("sp", g_ap, lng[1][L], R=[B_w], W=[gbb], dst=gbb)
            K.dma("sp", b_ap, lnb[1][L], R=[B_w], W=[gbb], dst=gbb)
            misc = A.alloc([128, 64], F32)
            miscb = K.buf("misc", True)
            K.dma("sp", misc, c_misc[:, :], R=[B_w], W=[miscb], dst=miscb)
            cnt = A.alloc([128, 8], F32)
            cntb = K.buf("cnt_s", True)
            K.dma("sp", cnt, CNT[:, :], R=[B_CNT], W=[cntb], dst=cntb)
            tb_ = A.alloc([128, 64], F32)
            tbb = K.buf("tb")
            nt, tbase, tend, sbase, tmp8 = tb_[:, 0:8], tb_[:, 8:16], tb_[:, 16:24], tb_[:, 24:32], tb_[:, 32:40]
            K.op("dve", lambda h: h.tensor_scalar(out=nt, in0=cnt, scalar1=0.0, scalar2=None, op0=ALU.is_gt), R=[cntb], W=[tbb])
            for m in range(1, 8):
                K.op("dve", lambda h, m=m: h.scalar_tensor_tensor(out=nt, in0=cnt, scalar=512.0 * m, in1=nt, op0=ALU.is_gt, op1=ALU.add),
                     R=[cntb, tbb], W=[tbb])
            K.op("dve", lambda h: h.memset(tbase[:, 0:1], 0.0), R=[], W=[tbb])
            for e in range(1, 8):
                K.op("dve", lambda h, e=e: h.tensor_tensor(out=tbase[:, e:e + 1], in0=tbase[:, e - 1:e], in1=nt[:, e - 1:e], op=ALU.add), R=[tbb], W=[tbb])
            K.op("dve", lambda h: h.tensor_tensor(out=tend, in0=tbase, in1=nt, op=ALU.add), R=[tbb], W=[tbb])
            K.op("dve", lambda h: h.tensor_scalar(out=sbase, in0=tbase, scalar1=512.0, scalar2=None, op0=ALU.mult), R=[tbb], W=[tbb])
            te = A.alloc([128, NTILE], F32)
            teb = K.buf("te")
            jrow = misc[:, 0:NTILE]
            K.op("dve", lambda h: h.tensor_scalar(out=te, in0=jrow, scalar1=tend[:, 0:1], scalar2=None, op0=ALU.is_ge), R=[miscb, tbb], W=[teb])
            for e in range(1, 8):
                K.op("dve", lambda h, e=e: h.scalar_tensor_tensor(out=te, in0=jrow, scalar=tend[:, e:e + 1], in1=te, op0=ALU.is_ge, op1=ALU.add),
                     R=[miscb, tbb, teb], W=[teb])
            K.op("dve", lambda h: h.tensor_scalar(out=te, in0=te, scalar1=7.0, scalar2=None, op0=ALU.min), R=[teb], W=[teb])
            idxWf = A.alloc([128, NTILE, 8], F32)
            idxW = A.alloc([128, NTILE, 8], I32)
            idxDf = A.alloc([128, NTILE, 28], F32)
            idxD = A.alloc([128, NTILE, 28], I32)
            idxb = K.buf("idx")
            te2 = A.alloc([128, NTILE], F32)
            K.op("dve", lambda h: h.tensor_scalar(out=te2, in0=te, scalar1=1024.0, scalar2=float(li * 8192), op0=ALU.mult, op1=ALU.add), R=[teb], W=[idxb])
            for j in range(NTILE):
                K.op("dve", lambda h, j=j: h.tensor_scalar(out=idxWf[:, j, :], in0=misc[:, 53:61], scalar1=te2[:, j:j + 1], scalar2=None, op0=ALU.add),
                     R=[miscb, idxb], W=[idxb])
            K.op("dve", lambda h: h.tensor_scalar(out=idxWf, in0=idxWf, scalar1=2.0, scalar2=None, op0=ALU.mult), R=[idxb], W=[idxb])
            K.op("dve", lambda h: h.tensor_copy(out=idxW, in_=idxWf), R=[idxb], W=[idxb])
            K.op("dve", lambda h: h.tensor_scalar(out=te2, in0=te, scalar1=3584.0, scalar2=float(li * 28672), op0=ALU.mult, op1=ALU.add), R=[teb, idxb], W=[idxb])
            for j in range(NTILE):
                K.op("dve", lambda h, j=j: h.tensor_scalar(out=idxDf[:, j, :], in0=misc[:, 24:52], scalar1=te2[:, j:j + 1], scalar2=None, op0=ALU.add),
                     R=[miscb, idxb], W=[idxb])
            K.op("dve", lambda h: h.tensor_copy(out=idxD, in_=idxDf), R=[idxb], W=[idxb])

            xr = [A.alloc([128, 4, D], BF16) for _ in range(2)]
            xrb = [K.buf("xr%d" % i, True) for i in range(2)]
            K.op("pool", lambda h: h.memset(xr[0], 0.0), R=[], W=[xrb[0]])
            for j in range(NTILE):
                K.dma("pool", XS[j * 512:(j + 1) * 512, :].rearrange("(a p) d -> p a d", p=128), xr[0], R=[xrb[0]], W=[B_XS], dst=B_XS)
            slotf = A.alloc([128, NT, 2], F32)
            sloti = A.alloc([128, NT, 2], I32)
            gts = A.alloc([128, NT, 2], F32)
            slb = K.buf("slots")
            rte = [A.alloc([128, 24], F32) for _ in range(2)]
            rteb = [K.buf("rte%d" % i, True) for i in range(2)]
            xt = [A.alloc([128, D], F32) for _ in range(2)]
            xtb = [K.buf("p4x%d" % i, True) for i in range(2)]
            xb = [A.alloc([128, D], BF16) for _ in range(2)]
            xbb = [K.buf("p4xb%d" % i) for i in range(2)]
            j8 = A.alloc([128, 8], F32)
            j8b = K.buf("j8")
            for tt in range(NT):
                i = tt % 2
                K.dma("sp", rte[i], ROUTE[tt * 128:(tt + 1) * 128, :], R=[B_ROUTE], W=[rteb[i]], dst=rteb[i])
                K.dma("sp", xt[i], xr_src[tt * 128:(tt + 1) * 128, :], R=[xr_src_b], W=[xtb[i]], dst=xtb[i])
                K.act(xb[i], xt[i], AF.Copy, R=[xtb[i]], W=[xbb[i]])
                for k2 in range(2):
                    K.op("dve", lambda h, i=i, k2=k2, tt=tt: h.scalar_tensor_tensor(
                        out=j8, in0=rte[i][:, k2 * 8:(k2 + 1) * 8], scalar=1.0, in1=sbase, op0=ALU.mult, op1=ALU.mult,
                        accum_out=slotf[:, tt, k2:k2 + 1]), R=[rteb[i], tbb], W=[j8b, slb])
                K.op("dve", lambda h, i=i, tt=tt: h.tensor_tensor(out=slotf[:, tt, :], in0=slotf[:, tt, :], in1=rte[i][:, 16:18], op=ALU.add),
                     R=[rteb[i], slb], W=[slb])
                K.op("dve", lambda h, tt=tt: h.tensor_copy(out=sloti[:, tt, :], in_=slotf[:, tt, :]), R=[slb], W=[slb])
                K.op("dve", lambda h, i=i, tt=tt: h.tensor_copy(out=gts[:, tt, :], in_=rte[i][:, 18:20]), R=[rteb[i]], W=[slb])
                for k2 in range(2):
                    K.idma("pool", lambda h, i=i, tt=tt, k2=k2: h.indirect_dma_start(
                        out=XS[:, :], out_offset=bass.IndirectOffsetOnAxis(sloti[:, tt, k2:k2 + 1], 0), in_=xb[i], in_offset=None),
                        R=[xbb[i], slb], W=[B_XS], dst=B_XS)

            main_mark = A.off
            xTb = A.alloc([128, 8, 512], BF16)
            xTbb = K.buf("xTb")
            hT = A.alloc([128, 7, 512], BF16)
            hTb = [K.buf("hT%d" % i) for i in range(7)]
            wgu = [A.alloc([128, 8, 1792], BF16) for _ in range(3)]
            wgub = [K.buf("wgu%d" % i, "sw") for i in range(3)]
            wd = [A.alloc([128, 7, D], BF16) for _ in range(2)]
            wdb = [K.buf("wd%d" % i, "sw") for i in range(2)]
            sg = [A.alloc([128, 512], F32) for _ in range(2)]
            sgb = [K.buf("sg%d" % i) for i in range(2)]
            ysb = [A.alloc([128, 4, D], F32) for _ in range(1)]
            ysbb = [K.buf("ysb%d" % i) for i in range(1)]
            Wg_v = od_g.rearrange("l e k (q f) -> (l e k q) f", q=2)
            Wu_v = od_u.rearrange("l e k (q f) -> (l e k q) f", q=2)
            Wd_v = od_d.rearrange("l e f d -> (l e f) d")
            gl = [(j, hf, gu) for j in range(NTILE) for hf in range(2) for gu in range(2)]
            dl = [(j, q) for j in range(NTILE) for q in range(4)]
            issued = {"g": 0, "d": 0}

            def ensure_g(n):
                while issued["g"] <= n and issued["g"] < len(gl):
                    m = issued["g"]
                    j, hf, gu = gl[m]
                    i = m % 3
                    Wv = Wg_v if gu == 0 else Wu_v
                    for kt in range(8):
                        K.idma("pool", lambda h, i=i, Wv=Wv, hf=hf, j=j, kt=kt: h.indirect_dma_start(
                            out=wgu[i][:, kt, :], out_offset=None, in_=Wv[:, :],
                            in_offset=bass.IndirectOffsetOnAxis(idxW[:, j, kt:kt + 1], 0), element_offset=hf * 1792),
                            R=[B_w, idxb], W=[wgub[i]], dst=wgub[i])
                    issued["g"] += 1

            def ensure_d(n):
                while issued["d"] <= n and issued["d"] < len(dl):
                    m = issued["d"]
                    j, q = dl[m]
                    i = m % 2
                    for fc in range(7):
                        K.idma("pool", lambda h, i=i, fc=fc, j=j, q=q: h.indirect_dma_start(
                            out=wd[i][:, fc, :], out_offset=None, in_=Wd_v[:, :],
                            in_offset=bass.IndirectOffsetOnAxis(idxD[:, j, q * 7 + fc:q * 7 + fc + 1], 0)),
                            R=[B_w, idxb], W=[wdb[i]], dst=wdb[i])
                    issued["d"] += 1

            pcount = 0
            ycount = 0
            for j in range(NTILE):
                xi = j % 2
                K.dma("sp", xr[xi], XS[j * 512:(j + 1) * 512, :].rearrange("(a p) d -> p a d", p=128), R=[B_XS], W=[xrb[xi]], dst=xrb[xi])
                for a in range(4):
                    xv = xr[xi][:, a, :].rearrange("p (q k) -> p k q", k=8)
                    for kt in range(8):
                        K.tr(PSBv[:, kt, :], xv[:, kt, :], ident, R=[xrb[xi], identb], W=[psbb])
                    K.op("dve", lambda h, a=a: h.tensor_copy(out=xTb[:, :, a * 128:(a + 1) * 128], in_=PSBv), R=[psbb], W=[xTbb])
                yb_, ybb_ = ysb[0], ysbb[0]
                for hf in range(2):
                    n0 = (j * 2 + hf) * 2
                    ensure_g(n0 + 1)
                    ensure_d((j * 2 + hf) * 2 + 1)
                    ensure_g(n0 + 2)
                    wg_, wg_b = wgu[n0 % 3], wgub[n0 % 3]
                    wu_, wu_b = wgu[(n0 + 1) % 3], wgub[(n0 + 1) % 3]
                    for q2 in range(2):
                        q = hf * 2 + q2
                        dq = j * 4 + q
                        wdt, wdtb = wd[dq % 2], wdb[dq % 2]
                        for fc2 in range(7):
                            c0 = (q2 * 7 + fc2) * 128
                            pg, pgb = PS[(pcount % 2) * 2], psb[(pcount % 2) * 2]
                            pu, pub = PS[(pcount % 2) * 2 + 1], psb[(pcount % 2) * 2 + 1]
                            s_, s_b = sg[pcount % 2], sgb[pcount % 2]
                            pcount += 1
                            for kt in range(8):
                                K.mm(pg[:, :], wg_[:, kt, c0:c0 + 128], xTb[:, kt, :], kt == 0, kt == 7, R=[wg_b, xTbb], W=[pgb])
                            for kt in range(8):
                                K.mm(pu[:, :], wu_[:, kt, c0:c0 + 128], xTb[:, kt, :], kt == 0, kt == 7, R=[wu_b, xTbb], W=[pub])
                            K.act(s_, pg[:, :], AF.Silu, R=[pgb], W=[s_b])
                            K.op("dve", lambda h, fc2=fc2, s_=s_, pu=pu: h.tensor_tensor(out=hT[:, fc2, :], in0=pu[:, :], in1=s_, op=ALU.mult),
                                 R=[pub, s_b], W=[hTb[fc2]])
                        if q2 == 1:
                            ensure_g(n0 + 3)
                        for a in range(4):
                            for dh in range(2):
                                py, pyb = PS[4 + ycount % 2], psb[4 + ycount % 2]
                                ycount += 1
                                for fc2 in range(7):
                                    K.mm(py[:, :], hT[:, fc2, a * 128:(a + 1) * 128], wdt[:, fc2, dh * 512:(dh + 1) * 512],
                                         fc2 == 0, fc2 == 6, R=[hTb[fc2], wdtb], W=[pyb])
                                if q == 0:
                                    K.op("dve", lambda h, py=py, a=a, dh=dh, yb_=yb_: h.tensor_copy(out=yb_[:, a, dh * 512:(dh + 1) * 512], in_=py[:, :]),
                                         R=[pyb], W=[ybb_])
                                else:
                                    K.op("dve", lambda h, py=py, a=a, dh=dh, yb_=yb_: h.tensor_tensor(
                                        out=yb_[:, a, dh * 512:(dh + 1) * 512], in0=py[:, :], in1=yb_[:, a, dh * 512:(dh + 1) * 512], op=ALU.add),
                                        R=[pyb, ybb_], W=[ybb_])
                K.dma("sp", YS[j * 512:(j + 1) * 512, :].rearrange("(a p) d -> p a d", p=128), yb_, R=[ybb_], W=[B_YS], dst=B_YS)


            K.barrier()
            A.off = main_mark
            ya = [[A.alloc([128, D], F32) for _ in range(2)] for _ in range(3)]
            yab_ = [[K.buf("ya%d%d" % (i, k2), "sw") for k2 in range(2)] for i in range(3)]
            stg = [A.alloc([128, 8, 128], BF16) for _ in range(3)]
            stgb = [K.buf("p4s%d" % i) for i in range(3)]
            small = [A.alloc([128, 16], F32) for _ in range(3)]
            smallb = [K.buf("p4sm%d" % i) for i in range(3)]
            zt_ = [A.alloc([128, D], F32) for _ in range(3)]
            ztb_ = [K.buf("p4y%d" % i, True) for i in range(3)]
            zb_ = [A.alloc([128, D], BF16) for _ in range(3)]
            zbb_ = [K.buf("p4yb%d" % i) for i in range(3)]
            for tt in range(NT):
                i = tt % 3
                K.dma("sp", zt_[i], xr_src[tt * 128:(tt + 1) * 128, :], R=[xr_src_b], W=[ztb_[i]], dst=ztb_[i])
                for k2 in range(2):
                    K.idma("pool", lambda h, i=i, tt=tt, k2=k2: h.indirect_dma_start(
                        out=ya[i][k2], out_offset=None, in_=YS[:, :], in_offset=bass.IndirectOffsetOnAxis(sloti[:, tt, k2:k2 + 1], 0)),
                        R=[B_YS, slb], W=[yab_[i][k2]], dst=yab_[i][k2])
                zt = zt_[i]
                K.op("dve", lambda h, i=i, tt=tt: h.tensor_scalar(out=ya[i][0], in0=ya[i][0], scalar1=gts[:, tt, 0:1], scalar2=None, op0=ALU.mult),
                     R=[yab_[i][0], slb], W=[yab_[i][0]])
                K.op("dve", lambda h, i=i, tt=tt: h.scalar_tensor_tensor(out=ya[i][0], in0=ya[i][1], scalar=gts[:, tt, 1:2], in1=ya[i][0], op0=ALU.mult, op1=ALU.add),
                     R=[yab_[i][0], yab_[i][1], slb], W=[yab_[i][0]])
                K.op("dve", lambda h, zt=zt, i=i: h.scalar_tensor_tensor(out=zt, in0=zt, scalar=ALPHA, in1=ya[i][0], op0=ALU.mult, op1=ALU.add),
                     R=[ztb_[i], yab_[i][0]], W=[ztb_[i]])
                layer_norm(zt, ztb_[i], 1, L, small[i], smallb[i], g_ap, b_ap, gbb)
                if last:
                    K.dma("sp", y_out[tt * 128:(tt + 1) * 128, :], zt, R=[ztb_[i]], W=[B_Y], dst=B_Y)
                else:
                    K.dma("sp", xr_dst[tt * 128:(tt + 1) * 128, :], zt, R=[ztb_[i]], W=[xr_dst_b], dst=xr_dst_b)
                    K.act(zb_[i], zt, AF.Copy, R=[ztb_[i]], W=[zbb_[i]])
                    emit_xT(zb_[i], zbb_[i], tt, ident, identb, stg[i], stgb[i])
            K.end_phase()

        phase_p0()
        cur, curb = x_in, B_xin
        for n, L in enumerate(layers):
            last = (n == len(layers) - 1)
            phase_p1(L)
            if last and stop == "p1":
                break
            phase_p2(L)
            if last and stop == "p2":
                break
            phase_p3(L, cur, curb, XR[0], B_XR[0])
            if last and stop == "p3":
                break
            if SPARSE and L % 2 == 1:
                phase_p4s(L, XR[0], B_XR[0], XR[1], B_XR[1], last)
            else:
                phase_p4(L, XR[0], B_XR[0], XR[1], B_XR[1], last)
            cur, curb = XR[1], B_XR[1]
        sp = K.E["sp"]
        sp.prog.append(([(B_Y.sem, K.semcnt[B_Y.sem])], None, None, 0))

        with nc.Block() as block:
            @block.tensor
            def _(h):
                K.replay("pe", h)

            @block.scalar
            def _(h):
                K.replay("act", h)

            @block.vector
            def _(h):
                K.replay("dve", h)

            @block.gpsimd
            def _(h):
                K.replay("pool", h)

            @block.sync
            def _(h):
                K.replay("sp", h)
    return nc


def make_consts(S):
    NG = S // 512
    bf = ml_dtypes.bfloat16
    c = {}
    c["c_ident_bf"] = np.eye(128, dtype=np.float32).astype(bf)
    c["c_ident_f"] = np.eye(128, dtype=np.float32)
    c["c_tri"] = np.triu(np.ones((128, 128), np.float32)).astype(bf)
    koh = np.zeros((16, S), np.float32)
    for n in range(min(16, S // 256)):
        koh[n, n * 256:(n + 1) * 256] = 1.0
    c["c_koh"] = koh.astype(bf)
    past = np.zeros((NG, 128, 4, 16), np.float32)
    own = np.zeros((NG, 128, 4, 16), np.float32)
    for g in range(NG):
        for t4 in range(4):
            qblk = (g * 4 + t4) // 2
            past[g, :, t4, qblk:] = -1e30
            own[g, :, t4, qblk] = 1.0
    c["c_past"] = past
    c["c_own"] = own
    inv = np.zeros((4, 128, 16), np.float32)
    for g4, w in enumerate(POOL_W):
        for t in range(16):
            inv[g4, :, t] = 1.0 / min(t + 1, w)
    c["c_invcnt"] = inv
    c["c_ustrict"] = np.triu(np.ones((128, 128), np.float32), k=1).astype(bf)
    misc = np.zeros((128, 64), np.float32)
    misc[:, 0:24] = np.arange(24, dtype=np.float32)[None, :]
    misc[:, 24:52] = np.arange(28, dtype=np.float32)[None, :] * 128 + np.arange(128, dtype=np.float32)[:, None]
    misc[:, 52] = np.arange(128, dtype=np.float32)
    misc[:, 53:61] = np.arange(128, dtype=np.float32)[:, None] * 8 + np.arange(8, dtype=np.float32)[None, :]
    c["c_misc"] = misc
    return c


def prep_shared(inp):
    f = lambda a: np.ascontiguousarray(np.asarray(a, dtype=np.float32))
    sh = {}
    for nm in ("ln1_g", "ln1_b", "ln2_g", "ln2_b"):
        a = f(inp[nm])
        sh[nm] = np.ascontiguousarray(np.broadcast_to(a[:, None, :], (a.shape[0], 128, a.shape[1])))
    sh["ev_w_in"] = f(inp["ev_w_in"])
    sh["ev_negb"] = f(inp["ev_b_forget"]).reshape(2, 8, 1)
    sh["ev_pool_w"] = f(inp["ev_pool_w"])
    sh["ev_pool_scale"] = f(inp["ev_pool_scale"]).reshape(2, 4, 128, 1)
    sh["ev_w_out"] = f(inp["ev_w_out"])
    sh["ev_ffn_gate"] = f(inp["ev_ffn_gate"])[:, None]
    sh["ev_ffn_up"] = f(inp["ev_ffn_up"])[:, None]
    sh["ev_ffn_down"] = f(inp["ev_ffn_down"])[:, None]
    sh["od_w_in"] = f(inp["od_w_in"])
    sh["od_w_out"] = f(inp["od_w_out"])
    r = f(inp["od_router"])
    rt = np.transpose(r, (0, 2, 1))
    sh["od_router"] = np.ascontiguousarray(np.broadcast_to(rt[:, :, None, :], (2, 8, 128, D)))
    sh["od_exp_gate"] = f(inp["od_exp_gate"])
    sh["od_exp_up"] = f(inp["od_exp_up"])
    sh["od_exp_down"] = f(inp["od_exp_down"])
    return sh


def run(inputs, S, layers, dbg=False, stop=None):
    x = np.asarray(inputs["x"], dtype=np.float32)
    B = x.shape[0]
    sh = prep_shared(inputs)
    sh.update(make_consts(S))
    nc = build_nc(S, layers, 4, dbg, stop)
    in_maps = []
    for b in range(B):
        m = dict(sh)
        m["x"] = np.ascontiguousarray(x[b, :S])
        in_maps.append(m)
    res = run_bass_kernel_spmd(nc, in_maps, core_ids=list(range(B)))
    if dbg:
        return res.results
    return np.stack([np.asarray(r["y"], dtype=np.float32) for r in res.results], axis=0)


def kernel(**inputs):
    return run(inputs, 4096, [0, 1, 2, 3])
```

```python
import numpy as np
import ml_dtypes
import concourse.bass as bass
import concourse.mybir as mybir
from concourse.bass_utils import run_bass_kernel_spmd

F32 = mybir.dt.float32
BF16 = mybir.dt.bfloat16
I32 = mybir.dt.int32
SPARSE = True
AF = mybir.ActivationFunctionType
ALU = mybir.AluOpType

D = 1024
DFF = 3584
NE = 8
ALPHA = float(8 ** 0.25)
EPS = 1e-5
POOL_W = (2, 4, 8, 16)
NEG = -30000.0
ARENA_BYTES = 206 * 1024


class Buf:
    __slots__ = ("name", "w", "r", "sem")

    def __init__(self, name, sem=None):
        self.name = name
        self.w = None
        self.r = {}
        self.sem = sem


class Eng:
    def __init__(self, name, sem):
        self.name = name
        self.sem = sem
        self.n = 0
        self.seen = {}
        self.prog = []


class Kern:
    def __init__(self, nc, sems):
        self.nc = nc
        self.sems = sems
        self.semcnt = [0] * len(sems)
        self.free = list(range(5, len(sems) - 12))
        self.free_sw = list(range(len(sems) - 12, len(sems)))
        self.phase_sems_sw = []
        self.E = {n: Eng(n, i) for i, n in enumerate(["pe", "act", "dve", "pool", "sp"])}
        self.phase_sems = []

    def buf(self, name, dma=False):
        s = None
        if dma == "sw":
            s = self.free_sw.pop()
            self.phase_sems_sw.append(s)
        elif dma:
            s = self.free.pop()
            self.phase_sems.append(s)
        return Buf(name, s)

    def end_phase(self):
        self.barrier()
        self.free.extend(self.phase_sems)
        self.phase_sems = []
        self.free_sw.extend(self.phase_sems_sw)
        self.phase_sems_sw = []

    def _waits(self, eng, R, W):
        deps = {}

        def add(s, v):
            if deps.get(s, 0) < v:
                deps[s] = v

        for b in R:
            if b.w is not None:
                add(*b.w)
        for b in W:
            if b.w is not None:
                add(*b.w)
            for s, v in b.r.items():
                add(s, v)
        waits = []
        for s, v in deps.items():
            if s == eng.sem and eng.name == "pe":
                continue
            if eng.seen.get(s, 0) < v:
                eng.seen[s] = v
                waits.append((s, v))
        return waits

    def op(self, en, fn, R=(), W=()):
        eng = self.E[en]
        waits = self._waits(eng, R, W)
        eng.n += 1
        tok = (eng.sem, eng.n)
        eng.prog.append((waits, fn, eng.sem, 1))
        for b in R:
            if b.r.get(tok[0], 0) < tok[1]:
                b.r[tok[0]] = tok[1]
        for b in W:
            b.w = tok
            b.r = {}

    def dma(self, qn, out, in_, R=(), W=(), dst=None):
        eng = self.E[qn]
        waits = self._waits(eng, R, W)
        s = dst.sem
        self.semcnt[s] += 16
        tok = (s, self.semcnt[s])
        eng.prog.append((waits, (lambda h, o=out, i=in_: h.dma_start(out=o, in_=i)), s, 16))
        for b in R:
            if b.r.get(s, 0) < tok[1]:
                b.r[s] = tok[1]
        for b in W:
            b.w = tok
            b.r = {}

    def idma(self, qn, fn, R=(), W=(), dst=None):
        eng = self.E[qn]
        waits = self._waits(eng, R, W)
        s = dst.sem
        self.semcnt[s] += 16
        tok = (s, self.semcnt[s])
        eng.prog.append((waits, fn, s, 16))
        for b in R:
            if b.r.get(s, 0) < tok[1]:
                b.r[s] = tok[1]
        for b in W:
            b.w = tok
            b.r = {}

    def barrier(self):
        for en, eng in self.E.items():
            waits = []
            for o in self.E.values():
                if o is eng or o.n == 0:
                    continue
                if eng.seen.get(o.sem, 0) < o.n:
                    eng.seen[o.sem] = o.n
                    waits.append((o.sem, o.n))
            for s in range(5, len(self.sems)):
                v = self.semcnt[s]
                if v and eng.seen.get(s, 0) < v:
                    eng.seen[s] = v
                    waits.append((s, v))
            if waits:
                eng.prog.append((waits, None, None, 0))

    def mm(self, out, lhsT, rhs, start, stop, R, W):
        self.op("pe", lambda h: h.matmul(out, lhsT, rhs, start=start, stop=stop), R, W)

    def tr(self, out, in_, ident, R, W):
        self.op("pe", lambda h: h.transpose(out, in_, ident), R, W)

    def act(self, out, in_, func, R, W, bias=None, scale=None):
        kw = {}
        if bias is not None:
            kw["bias"] = bias
        if scale is not None:
            kw["scale"] = scale
        self.op("act", lambda h: h.activation(out=out, in_=in_, func=func, **kw), R, W)

    def replay(self, en, h):
        for waits, fn, s, inc in self.E[en].prog:
            for ws, wv in waits:
                h.wait_ge(self.sems[ws], wv)
            if fn is not None:
                fn(h).then_inc(self.sems[s], inc)


class Arena:
    def __init__(self, t):
        self.t = t
        self.off = 0

    def reset(self):
        self.off = 0

    def alloc(self, shape, dt):
        esz = 2 if dt == BF16 else 4
        n = 1
        for s in shape[1:]:
            n *= s
        nbytes = (n * esz + 63) // 64 * 64
        a = self.off
        self.off += nbytes
        assert self.off <= ARENA_BYTES, ("arena overflow", self.off)
        v = self.t[0:shape[0], a // 2:(a + n * esz) // 2]
        if dt != BF16:
            v = v.bitcast(dt)
        if len(shape) == 3:
            v = v.rearrange("p (a b) -> p a b", a=shape[1])
        elif len(shape) == 4:
            v = v.rearrange("p (a b c) -> p a b c", a=shape[1], b=shape[2])
        return v


def build_nc(S, layers, n_layers_total, dbg=False, stop=None):
    NT = S // 128
    NG = S // 512
    NBLK = S // 1024
    nc = bass.Bass("TRN2", target_bir_lowering=False)

    def din(name, shape, dt=F32):
        return nc.dram_tensor(name, shape, dt, kind="ExternalInput").ap()

    def dscr(name, shape, dt):
        return nc.dram_tensor(name, shape, dt, kind=("ExternalOutput" if dbg else "Internal")).ap()

    x_in = din("x", [S, D])
    lng = [din("ln1_g", [4, 128, D]), din("ln2_g", [4, 128, D])]
    lnb = [din("ln1_b", [4, 128, D]), din("ln2_b", [4, 128, D])]
    ev_w_in = din("ev_w_in", [2, D, 2056])
    ev_negb = din("ev_negb", [2, 8, 1])
    ev_pool_w = din("ev_pool_w", [2, 4, 128, 128])
    ev_pool_scale = din("ev_pool_scale", [2, 4, 128, 1])
    ev_w_out = din("ev_w_out", [2, D, D])
    ev_g = din("ev_ffn_gate", [2, 1, D, DFF])
    ev_u = din("ev_ffn_up", [2, 1, D, DFF])
    ev_d = din("ev_ffn_down", [2, 1, DFF, D])
    od_w_in = din("od_w_in", [2, D, 3072])
    od_w_out = din("od_w_out", [2, D, D])
    od_router = din("od_router", [2, 8, 128, D])
    od_g = din("od_exp_gate", [2, NE, D, DFF])
    od_u = din("od_exp_up", [2, NE, D, DFF])
    od_d = din("od_exp_down", [2, NE, DFF, D])
    c_ident_bf = din("c_ident_bf", [128, 128], BF16)
    c_ident_f = din("c_ident_f", [128, 128])
    c_tri = din("c_tri", [128, 128], BF16)
    c_koh = din("c_koh", [16, S], BF16)
    c_past = din("c_past", [NG, 128, 4, 16])
    c_own = din("c_own", [NG, 128, 4, 16])
    c_invcnt = din("c_invcnt", [4, 128, 16])
    NTILE = 2 * S // 512 + 8
    NSLOT = NTILE * 512
    c_ustrict = din("c_ustrict", [128, 128], BF16)
    c_misc = din("c_misc", [128, 64])
    y_out = nc.dram_tensor("y", [S, D], F32, kind="ExternalOutput").ap()

    XR = [dscr("xr0", [S, D], F32), dscr("xr1", [S, D], F32)]
    XT = dscr("xT", [D, S], BF16)
    QT = dscr("qT", [D, S], BF16)
    KT = dscr("kT", [D, S], BF16)
    VV = dscr("vv", [S, D], BF16)
    CATT = dscr("catT", [D, S], BF16)
    FAUG = dscr("faug", [8, 3, S], BF16)
    NFC = dscr("nfc", [128, NT * 8], F32)
    MB = dscr("mb", [256, S], BF16)
    COMB = dscr("comb", [S, 8], F32)
    ROUTE = dscr("route", [S, 24], F32)
    CNT = dscr("cnt", [128, 8], F32)
    XS = dscr("xs", [NSLOT, D], BF16)
    YS = dscr("ys", [NSLOT, D], F32)

    from contextlib import ExitStack
    with ExitStack() as st:
        arena_t = st.enter_context(nc.sbuf_tensor("arena", [128, ARENA_BYTES // 2], BF16))
        PS = [st.enter_context(nc.psum_tensor("ps%d" % i, [128, 512], F32)) for i in range(7)]
        PSB = st.enter_context(nc.psum_tensor("psb", [128, 1024], BF16))
        NSEM = 60
        sems = [st.enter_context(nc.semaphore("s%d" % i)) for i in range(NSEM)]
        K = Kern(nc, sems)
        A = Arena(arena_t)
        psb = [K.buf("ps%d" % i) for i in range(7)]
        psbb = K.buf("psb")
        PSBv = PSB[:, :].rearrange("p (a b) -> p a b", a=8)

        B_xin = K.buf("x_in")
        B_w = K.buf("weights")
        B_XR = [K.buf("xr0", True), K.buf("xr1", True)]
        B_XT = K.buf("xT", True)
        B_QT = K.buf("qT", True)
        B_KT = K.buf("kT", True)
        B_VV = K.buf("vv", True)
        B_CATT = K.buf("catT", True)
        B_FAUG = K.buf("faug", True)
        B_NFC = K.buf("nfc", True)
        B_MB = K.buf("mb", True)
        B_COMB = K.buf("comb", True)
        B_ROUTE = K.buf("route", True)
        B_CNT = K.buf("cnt", True)
        B_XS = K.buf("xs", "sw")
        B_YS = K.buf("ys", True)
        B_Y = K.buf("y", True)
        K.phase_sems = []
        K.phase_sems_sw = []

        def consts_common():
            ident = A.alloc([128, 128], BF16)
            b = K.buf("ident", True)
            K.dma("sp", ident, c_ident_bf[:, :], R=[B_w], W=[b], dst=b)
            return ident, b

        def emit_xT(xb_ap, xb_buf, tt, ident, identb, stg, stgb):
            for kt in range(8):
                K.tr(PSBv[:, kt, :], xb_ap[:, kt * 128:(kt + 1) * 128], ident, R=[xb_buf, identb], W=[psbb])
            K.op("dve", lambda h: h.tensor_copy(out=stg, in_=PSBv), R=[psbb], W=[stgb])
            K.dma("sp", XT[:, tt * 128:(tt + 1) * 128].rearrange("(kt p) t -> p kt t", p=128), stg,
                  R=[stgb], W=[B_XT], dst=B_XT)

        def layer_norm(z, zb, which, L, small, smallb, g_ap, b_ap, gbb):
            st6 = small[:, 0:12].rearrange("p (a b) -> p a b", a=2)
            K.op("dve", lambda h: h.bn_stats(out=st6[:, 0, :], in_=z[:, 0:512]), R=[zb], W=[smallb])
            K.op("dve", lambda h: h.bn_stats(out=st6[:, 1, :], in_=z[:, 512:1024]), R=[zb], W=[smallb])
            K.op("dve", lambda h: h.bn_aggr(out=small[:, 12:14], in_=small[:, 0:12]), R=[smallb], W=[smallb])
            K.act(small[:, 14:15], small[:, 13:14], AF.Sqrt, R=[smallb], W=[smallb], bias=EPS, scale=1.0)
            K.op("dve", lambda h: h.reciprocal(out=small[:, 15:16], in_=small[:, 14:15]), R=[smallb], W=[smallb])
            K.op("dve", lambda h: h.tensor_scalar(out=z, in0=z, scalar1=small[:, 12:13], scalar2=small[:, 15:16],
                                                  op0=ALU.subtract, op1=ALU.mult), R=[zb, smallb], W=[zb])
            K.op("pool", lambda h: h.tensor_tensor(out=z, in0=z, in1=g_ap, op=ALU.mult), R=[zb, gbb], W=[zb])
            K.op("pool", lambda h: h.tensor_tensor(out=z, in0=z, in1=b_ap, op=ALU.add), R=[zb, gbb], W=[zb])

        def phase_p0():
            A.reset()
            ident, identb = consts_common()
            xt = [A.alloc([128, D], F32) for _ in range(2)]
            xtb = [K.buf("p0x%d" % i, True) for i in range(2)]
            xb = [A.alloc([128, D], BF16) for _ in range(2)]
            xbb = [K.buf("p0xb%d" % i) for i in range(2)]
            stg = [A.alloc([128, 8, 128], BF16) for _ in range(2)]
            stgb = [K.buf("p0s%d" % i) for i in range(2)]
            for tt in range(NT):
                i = tt % 2
                K.dma("sp", xt[i], x_in[tt * 128:(tt + 1) * 128, :], R=[B_xin], W=[xtb[i]], dst=xtb[i])
                K.act(xb[i], xt[i], AF.Copy, R=[xtb[i]], W=[xbb[i]])
                emit_xT(xb[i], xbb[i], tt, ident, identb, stg[i], stgb[i])
            K.end_phase()

        def phase_p1(L):
            even = (L % 2 == 0)
            li = L // 2
            A.reset()
            xTs = A.alloc([128, 8, S], BF16)
            xTsb = [K.buf("xTs%d" % g, True) for g in range(NG)]
            for g in range(NG):
                K.dma("sp", xTs[:, :, g * 512:(g + 1) * 512],
                      XT[:, g * 512:(g + 1) * 512].rearrange("(kt p) t -> p kt t", p=128),
                      R=[B_XT], W=[xTsb[g]], dst=xTsb[g])
            w_in = ev_w_in if even else od_w_in
            nqc = 4 if even else 8
            qoff, koff, voff = (0, 512, 1024) if even else (0, 1024, 2048)
            nvh = 1 if even else 2
            wslot = [A.alloc([128, 8, 512], BF16) for _ in range(2)]
            wslotb = [K.buf("wslot%d" % i, "sw") for i in range(2)]
            stg = [A.alloc([128, 512], BF16) for _ in range(3)]
            stgb = [K.buf("p1stg%d" % i) for i in range(3)]
            cnt = {"w": 0, "s": 0, "p": 0}

            def load_w(col0, ncols=512):
                i = cnt["w"] % 2
                cnt["w"] += 1
                K.dma("pool", wslot[i][:, :, 0:ncols],
                      w_in[li, :, col0:col0 + ncols].rearrange("(kt p) f -> p kt f", p=128),
                      R=[B_w], W=[wslotb[i]], dst=wslotb[i])
                return wslot[i], wslotb[i]

            def next_ps():
                i = cnt["p"] % 6
                cnt["p"] += 1
                return PS[i], psb[i]

            def next_stg():
                i = cnt["s"] % 3
                cnt["s"] += 1
                return stg[i], stgb[i]

            if not even:
                ident, identb = consts_common()
                kmT = A.alloc([128, 16], BF16)
                kmTb = K.buf("kmT")
                kmf = A.alloc([128, 16], F32)
                kmfb = K.buf("kmf")
                K.op("pool", lambda h: h.memset(kmf, 0.0), R=[], W=[kmfb])
                gm = A.alloc([128, 4, 2, 16], F32)
                gmb = K.buf("gm")
                m8 = A.alloc([128, 8, 8], F32)
                m8b = K.buf("m8")
                selb_t = A.alloc([128, 4, 2, 16], F32)
                selbb = K.buf("sel")
                mbt = A.alloc([128, 4, 2, 16], BF16)
                mbtb = K.buf("mbt")
                mbs = A.alloc([32, 512], BF16)
                mbsb = K.buf("mbs")
                cpast = A.alloc([128, NG, 4, 16], F32)
                cown = A.alloc([128, NG, 4, 16], F32)
                cpb = K.buf("cpast", True)
                for g in range(NG):
                    K.dma("sp", cpast[:, g], c_past[g], R=[B_w], W=[cpb], dst=cpb)
                    K.dma("sp", cown[:, g], c_own[g], R=[B_w], W=[cpb], dst=cpb)

            for which in ("k", "q"):
                off = koff if which == "k" else qoff
                dstT, dstB = (KT, B_KT) if which == "k" else (QT, B_QT)
                for cg in range(nqc // 4):
                    wt, wtb = load_w(off + cg * 512)
                    for c4 in range(4):
                        ct = cg * 4 + c4
                        for g in range(NG):
                            ps, pb = next_ps()
                            for kt in range(8):
                                K.mm(ps[:, :], wt[:, kt, c4 * 128:(c4 + 1) * 128], xTs[:, kt, g * 512:(g + 1) * 512],
                                     kt == 0, kt == 7, R=[wtb, xTsb[g]], W=[pb])
                            sg, sgb = next_stg()
                            if which == "k":
                                if even:
                                    K.act(sg, ps[:, :], AF.Copy, R=[pb], W=[sgb])
                                else:
                                    for bb in range(2):
                                        K.op("act", lambda h, sg=sg, ps=ps, bb=bb, g=g: h.activation(
                                            out=sg[:, bb * 256:(bb + 1) * 256], in_=ps[:, bb * 256:(bb + 1) * 256], func=AF.Copy,
                                            accum_out=kmf[:, 2 * g + bb:2 * g + bb + 1]), R=[pb], W=[sgb, kmfb])
                            else:
                                K.act(sg, ps[:, :], AF.Copy, R=[pb], W=[sgb], scale=0.125)
                            K.dma("sp", dstT[ct * 128:(ct + 1) * 128, g * 512:(g + 1) * 512], sg,
                                  R=[sgb], W=[dstB], dst=dstB)
                            if (not even) and which == "q":
                                gp, gpb = next_ps()
                                for t4 in range(4):
                                    K.mm(gp[:, t4 * 32:(t4 + 1) * 32], sg[:, t4 * 128:(t4 + 1) * 128], kmTs[ct][:, :], True, True,
                                         R=[sgb, kmTsb[ct]], W=[gpb])
                                gpv = gp[:, 0:128].rearrange("p (a b c) -> p a b c", a=4, b=2)
                                for hh in range(2):
                                    K.op("dve", lambda h, o=gm[:, :, hh, :], i=gpv[:, :, hh, :], c=cpast[:, g]:
                                         h.tensor_tensor(out=o, in0=i, in1=c, op=ALU.add), R=[gpb, cpb], W=[gmb])
                                for t4 in range(4):
                                    for hh in range(2):
                                        K.op("dve", lambda h, o=m8[:, t4 * 2 + hh, :], i=gm[:, t4, hh, :]: h.max(out=o, in_=i),
                                             R=[gmb], W=[m8b])
                                for t4 in range(4):
                                    for hh in range(2):
                                        K.op("dve", lambda h, o=selb_t[:, t4, hh, :], i=gm[:, t4, hh, :], s=m8[:, t4 * 2 + hh, 2:3]:
                                             h.tensor_scalar(out=o, in0=i, scalar1=s, scalar2=None, op0=ALU.is_ge),
                                             R=[gmb, m8b], W=[selbb])
                                for hh in range(2):
                                    K.op("dve", lambda h, o=selb_t[:, :, hh, :], c=cown[:, g]:
                                         h.tensor_tensor(out=o, in0=o, in1=c, op=ALU.max), R=[selbb, cpb], W=[selbb])
                                K.op("dve", lambda h: h.tensor_scalar(out=mbt, in0=selb_t, scalar1=-NEG, scalar2=NEG,
                                                                      op0=ALU.mult, op1=ALU.add), R=[selbb], W=[mbtb])
                                for t4 in range(4):
                                    K.tr(PSB[0:32, t4 * 128:(t4 + 1) * 128],
                                         mbt[:, t4, :, :].rearrange("p a b -> p (a b)"), ident, R=[mbtb, identb], W=[psbb])
                                K.op("dve", lambda h: h.tensor_copy(out=mbs, in_=PSB[0:32, 0:512]), R=[psbb], W=[mbsb])
                                K.dma("sp", MB[ct * 32:(ct + 1) * 32, g * 512:(g + 1) * 512], mbs, R=[mbsb], W=[B_MB], dst=B_MB)
                        if (not even) and which == "k":
                            if ct == 0:
                                kmTs = [A.alloc([128, 32], BF16) for _ in range(nqc)]
                                kmTsb = [K.buf("kmTs%d" % i) for i in range(nqc)]
                                for i_ in range(nqc):
                                    K.op("pool", lambda h, o=kmTs[i_]: h.memset(o, 0.0), R=[], W=[kmTsb[i_]])
                            for hh in range(2):
                                K.op("dve", lambda h, o=kmTs[ct][hh * 64:(hh + 1) * 64, hh * 16:(hh + 1) * 16], i_=kmf[hh * 64:(hh + 1) * 64, :]:
                                     h.tensor_scalar(out=o, in0=i_, scalar1=1.0 / 256, scalar2=None, op0=ALU.mult),
                                     R=[kmfb], W=[kmTsb[ct]])

            for vh in range(nvh):
                wt, wtb = load_w(voff + vh * 512)
                for tt in range(NT):
                    ps, pb = next_ps()
                    g = tt // 4
                    for kt in range(8):
                        K.mm(ps[:, :], xTs[:, kt, tt * 128:(tt + 1) * 128], wt[:, kt, :], kt == 0, kt == 7,
                             R=[wtb, xTsb[g]], W=[pb])
                    sg, sgb = next_stg()
                    K.act(sg, ps[:, :], AF.Copy, R=[pb], W=[sgb])
                    K.dma("sp", VV[tt * 128:(tt + 1) * 128, vh * 512:(vh + 1) * 512], sg, R=[sgb], W=[B_VV], dst=B_VV)

            if even:
                wt, wtb = load_w(1536, 8)
                negb = A.alloc([8, 1], F32)
                negbb = K.buf("negb", True)
                K.dma("sp", negb, ev_negb[li], R=[B_w], W=[negbb], dst=negbb)
                K.op("dve", lambda h: h.tensor_scalar(out=negb, in0=negb, scalar1=-1.0, scalar2=None, op0=ALU.mult),
                     R=[negbb], W=[negbb])
                f_mark = A.off
                cs = [A.alloc([8, S], F32) for _ in range(2)]
                csb = [K.buf("cs%d" % i) for i in range(2)]
                for g in range(NG):
                    ps, pb = next_ps()
                    for kt in range(8):
                        K.mm(ps[0:8, :], wt[:, kt, 0:8], xTs[:, kt, g * 512:(g + 1) * 512], kt == 0, kt == 7,
                             R=[wtb, xTsb[g]], W=[pb])
                    K.act(cs[1][:, g * 512:(g + 1) * 512], ps[0:8, :], AF.Exp, R=[pb, negbb], W=[csb[1]], bias=negb, scale=-1.0)
                K.act(cs[0], cs[1], AF.Ln, R=[csb[1]], W=[csb[0]], bias=1.0, scale=1.0)
                cur = 0
                sh = 1
                while sh < S:
                    o, i_ = cs[1 - cur], cs[cur]
                    K.op("dve", lambda h, o=o, i_=i_, sh=sh: h.tensor_tensor(out=o[:, sh:S], in0=i_[:, sh:S], in1=i_[:, 0:S - sh], op=ALU.add),
                         R=[csb[cur]], W=[csb[1 - cur]])
                    K.op("dve", lambda h, o=o, i_=i_, sh=sh: h.tensor_copy(out=o[:, 0:sh], in_=i_[:, 0:sh]),
                         R=[csb[cur]], W=[csb[1 - cur]])
                    cur = 1 - cur
                    sh *= 2
                C, Cb = cs[cur], csb[cur]
                r1, r1b = cs[1 - cur], csb[1 - cur]
                hb = [A.alloc([8, S], BF16) for _ in range(3)]
                hbb = [K.buf("hb%d" % i) for i in range(3)]
                h32 = A.alloc([8, S], F32)
                h32b = K.buf("h32")
                K.op("dve", lambda h: h.tensor_scalar(out=hb[0], in0=C, scalar1=-1.0, scalar2=None, op0=ALU.mult), R=[Cb], W=[hbb[0]])
                K.op("dve", lambda h: h.tensor_copy(out=h32, in_=hb[0]), R=[hbb[0]], W=[h32b])
                K.op("dve", lambda h: h.scalar_tensor_tensor(out=r1, in0=C, scalar=-1.0, in1=h32, op0=ALU.mult, op1=ALU.subtract),
                     R=[Cb, h32b], W=[r1b])
                K.op("dve", lambda h: h.tensor_copy(out=hb[1], in_=r1), R=[r1b], W=[hbb[1]])
                K.op("dve", lambda h: h.tensor_copy(out=h32, in_=hb[1]), R=[hbb[1]], W=[h32b])
                K.op("dve", lambda h: h.tensor_tensor(out=r1, in0=r1, in1=h32, op=ALU.subtract), R=[r1b, h32b], W=[r1b])
                K.op("dve", lambda h: h.tensor_copy(out=hb[2], in_=r1), R=[r1b], W=[hbb[2]])
                for j in range(3):
                    K.dma("sp", FAUG[:, j, :], hb[j], R=[hbb[j]], W=[B_FAUG], dst=B_FAUG)
                identf = A.alloc([8, 8], F32)
                identfb = K.buf("identf", True)
                K.dma("sp", identf, c_ident_f[0:8, 0:8], R=[B_w], W=[identfb], dst=identfb)
                ps, pb = next_ps()
                for kt in range(NT):
                    K.tr(ps[:, kt * 8:(kt + 1) * 8], C[:, kt * 128:(kt + 1) * 128], identf, R=[Cb, identfb], W=[pb])
                nfc = A.alloc([128, NT * 8], F32)
                nfcb = K.buf("nfc_s")
                K.op("dve", lambda h, ps=ps: h.tensor_copy(out=nfc, in_=ps[:, 0:NT * 8]), R=[pb], W=[nfcb])
                K.dma("sp", NFC[:, :], nfc, R=[nfcb], W=[B_NFC], dst=B_NFC)
                K.barrier()
                A.off = f_mark

                wt, wtb = load_w(1544)
                pw = A.alloc([128, 4, 128], BF16)
                pwb = K.buf("pw", "sw")
                for g4 in range(4):
                    K.dma("pool", pw[:, g4, :], ev_pool_w[li, g4], R=[B_w], W=[pwb], dst=pwb)
                psc = A.alloc([128, 4], F32)
                pscb = K.buf("psc", True)
                for g4 in range(4):
                    K.dma("sp", psc[:, g4:g4 + 1], ev_pool_scale[li, g4], R=[B_w], W=[pscb], dst=pscb)
                icn = A.alloc([128, 4, 16], F32)
                icnb = K.buf("icn", True)
                for g4 in range(4):
                    K.dma("sp", icn[:, g4, :], c_invcnt[g4], R=[B_w], W=[icnb], dst=icnb)
                uu = A.alloc([128, S], F32)
                uub = K.buf("uu")
                sa = [A.alloc([128, S], F32) for _ in range(2)]
                sab = [K.buf("sa%d" % i) for i in range(2)]
                mx = A.alloc([128, S], BF16)
                mxb = K.buf("mx")
                fix = A.alloc([128, 16], F32)
                fixb = K.buf("fix")
                for g4 in range(4):
                    w = POOL_W[g4]
                    for g in range(NG):
                        ps, pb = next_ps()
                        for kt in range(8):
                            K.mm(ps[:, :], wt[:, kt, g4 * 128:(g4 + 1) * 128], xTs[:, kt, g * 512:(g + 1) * 512],
                                 kt == 0, kt == 7, R=[wtb, xTsb[g]], W=[pb])
                        K.act(uu[:, g * 512:(g + 1) * 512], ps[:, :], AF.Copy, R=[pb], W=[uub])
                    src, srcb = uu, uub
                    sh = 1
                    k = 0
                    while sh < w:
                        o, ob = sa[k % 2], sab[k % 2]
                        K.op("dve", lambda h, o=o, i_=src, sh=sh: h.tensor_tensor(out=o[:, sh:S], in0=i_[:, sh:S], in1=i_[:, 0:S - sh], op=ALU.add),
                             R=[srcb], W=[ob])
                        K.op("pool", lambda h, o=o, i_=src, sh=sh: h.tensor_copy(out=o[:, 0:sh], in_=i_[:, 0:sh]), R=[srcb], W=[ob])
                        src, srcb = o, ob
                        sh *= 2
                        k += 1
                    K.op("dve", lambda h, s_=src, w=w: h.scalar_tensor_tensor(out=mx, in0=s_, scalar=1.0 / w, in1=uu, op0=ALU.mult, op1=ALU.subtract),
                         R=[srcb, uub], W=[mxb])
                    K.op("dve", lambda h, s_=src, w=w, g4=g4: h.tensor_tensor(out=fix[:, 0:w - 1], in0=s_[:, 0:w - 1], in1=icn[:, g4, 0:w - 1], op=ALU.mult),
                         R=[srcb, icnb], W=[fixb])
                    K.op("dve", lambda h, w=w: h.tensor_tensor(out=mx[:, 0:w - 1], in0=fix[:, 0:w - 1], in1=uu[:, 0:w - 1], op=ALU.subtract),
                         R=[fixb, uub, mxb], W=[mxb])
                    for g in range(NG):
                        ps, pb = next_ps()
                        K.mm(ps[:, :], pw[:, g4, :], mx[:, g * 512:(g + 1) * 512], True, True, R=[pwb, mxb], W=[pb])
                        sg, sgb = next_stg()
                        K.op("dve", lambda h, sg=sg, ps=ps, g4=g4: h.tensor_scalar(out=sg, in0=ps[:, :], scalar1=psc[:, g4:g4 + 1], scalar2=None, op0=ALU.mult),
                             R=[pb, pscb], W=[sgb])
                        K.dma("sp", CATT[512 + g4 * 128:512 + (g4 + 1) * 128, g * 512:(g + 1) * 512], sg, R=[sgb], W=[B_CATT], dst=B_CATT)
            K.end_phase()

        def phase_p2(L):
            even = (L % 2 == 0)
            H = 8 if even else 16
            aug = 3 if even else 16
            KA = 64 + aug
            A.reset()
            qa = [A.alloc([128, S], BF16) for _ in range(2)]
            ka = [A.alloc([128, S], BF16) for _ in range(2)]
            va = [A.alloc([128, NT, 128], BF16) for _ in range(2)]
            qab = [K.buf("qa%d" % i, True) for i in range(2)]
            kab = [K.buf("ka%d" % i, True) for i in range(2)]
            vab = [K.buf("va%d" % i, True) for i in range(2)]
            pt = [A.alloc([128, 512], BF16) for _ in range(4)]
            ptb = [K.buf("pt%d" % i) for i in range(4)]
            rr = [A.alloc([128, 512], F32) for _ in range(2)]
            rrb = [K.buf("rr%d" % i) for i in range(2)]
            ot = [A.alloc([64, 512], BF16) for _ in range(2)]
            otb = [K.buf("ot%d" % i) for i in range(2)]
            tri = A.alloc([128, 128], BF16)
            trib = K.buf("tri", True)
            K.dma("sp", tri, c_tri[:, :], R=[B_w], W=[trib], dst=trib)
            if even:
                nfc = A.alloc([128, NT * 8], F32)
                nfcb = K.buf("nfc2", True)
                K.dma("sp", nfc, NFC[:, :], R=[B_NFC], W=[nfcb], dst=nfcb)
            for i in range(2):
                K.op("pool", lambda h, i=i: h.memset(va[i][:, :, 64:128], 1.0), R=[], W=[vab[i]])
                if even:
                    K.op("pool", lambda h, i=i: h.memset(ka[i][64:67, :], 1.0), R=[], W=[kab[i]])
                else:
                    K.dma("sp", ka[i][64:80, :], c_koh[:, :], R=[B_w], W=[kab[i]], dst=kab[i])

            def load_head(hd):
                b = hd % 2
                K.dma("sp", qa[b][0:64, :], QT[hd * 64:(hd + 1) * 64, :], R=[B_QT], W=[qab[b]], dst=qab[b])
                if even:
                    K.dma("sp", qa[b][64:67, :], FAUG[hd], R=[B_FAUG], W=[qab[b]], dst=qab[b])
                else:
                    K.dma("sp", qa[b][64:80, :], MB[hd * 16:(hd + 1) * 16, :], R=[B_MB], W=[qab[b]], dst=qab[b])
                K.dma("sp", ka[b][0:64, :], KT[hd * 64:(hd + 1) * 64, :], R=[B_KT], W=[kab[b]], dst=kab[b])
                for j in range(S // 1024):
                    K.dma("sp", va[b][:, 8 * j:8 * j + 8, 0:64],
                          VV[j * 1024:(j + 1) * 1024, hd * 64:(hd + 1) * 64].rearrange("(kt p) d -> p kt d", p=128),
                          R=[B_VV], W=[vab[b]], dst=vab[b])

            its = []
            for hd in range(H):
                for g in range(NG):
                    for kt in range(4 * g + 4):
                        its.append((hd, g, kt))
            SPS = [(PS[i], psb[i]) for i in range(3)]
            OPS = [(PS[3 + i], psb[3 + i]) for i in range(2)]

            def emit_s(idx):
                hd, g, kt = its[idx]
                b = hd % 2
                j = kt - 4 * g
                c0 = 128 * j if j > 0 else 0
                n = 512 - c0
                sp_, spb = SPS[idx % 3]
                K.mm(sp_[:, 0:n], ka[b][0:KA, kt * 128:(kt + 1) * 128], qa[b][0:KA, g * 512 + c0:(g + 1) * 512],
                     True, True, R=[kab[b], qab[b]], W=[spb])
                p, pb_ = pt[idx % 4], ptb[idx % 4]
                if even:
                    K.act(p[:, 0:n], sp_[:, 0:n], AF.Exp, R=[spb, nfcb], W=[pb_], bias=nfc[:, kt * 8 + hd:kt * 8 + hd + 1], scale=1.0)
                else:
                    K.act(p[:, 0:n], sp_[:, 0:n], AF.Exp, R=[spb], W=[pb_])
                if j >= 0:
                    K.op("pool", lambda h, p=p: h.tensor_tensor(out=p[:, 0:128], in0=p[:, 0:128], in1=tri, op=ALU.mult),
                         R=[pb_, trib], W=[pb_])

            def emit_pv(idx):
                hd, g, kt = its[idx]
                b = hd % 2
                j = kt - 4 * g
                c0 = 128 * j if j > 0 else 0
                n = 512 - c0
                last = 4 * g + 3
                gi = hd * NG + g
                if g == 0 and kt == 0 and hd >= 1 and hd + 1 < H:
                    load_head(hd + 1)
                o, ob = OPS[gi % 2]
                p, pb_ = pt[idx % 4], ptb[idx % 4]
                K.mm(o[:, c0:512], va[b][:, kt, :], p[:, 0:n], kt == 0, kt == last, R=[vab[b], pb_], W=[ob])
                if kt == last:
                    r, rb = rr[gi % 2], rrb[gi % 2]
                    t_, tb = ot[gi % 2], otb[gi % 2]
                    K.op("dve", lambda h: h.reciprocal(out=r[64:128, :], in_=o[64:128, :]), R=[ob], W=[rb])
                    K.op("dve", lambda h: h.tensor_tensor(out=t_, in0=o[0:64, :], in1=r[64:128, :], op=ALU.mult), R=[ob, rb], W=[tb])
                    K.dma("sp", CATT[hd * 64:(hd + 1) * 64, g * 512:(g + 1) * 512], t_, R=[tb], W=[B_CATT], dst=B_CATT)

            n_it = len(its)
            load_head(0)
            if H > 1:
                load_head(1)
            emit_s(0)
            if n_it > 1:
                emit_s(1)
            for idx in range(n_it):
                emit_pv(idx)
                if idx + 2 < n_it:
                    emit_s(idx + 2)
            K.end_phase()

        def phase_p3(L, xr_src, xr_src_b, xr_dst, xr_dst_b):
            even = (L % 2 == 0)
            li = L // 2
            A.reset()
            ident, identb = consts_common()
            wo = A.alloc([128, 8, D], BF16)
            wob = K.buf("wo", "sw")
            w_out = ev_w_out if even else od_w_out
            for hf in range(2):
                K.dma("pool", wo[:, :, hf * 512:(hf + 1) * 512],
                      w_out[li, :, hf * 512:(hf + 1) * 512].rearrange("(kt p) f -> p kt f", p=128), R=[B_w], W=[wob], dst=wob)
            g_ap = A.alloc([128, D], F32)
            b_ap = A.alloc([128, D], F32)
            gbb = K.buf("gb", True)
            K.dma("sp", g_ap, lng[0][L], R=[B_w], W=[gbb], dst=gbb)
            K.dma("sp", b_ap, lnb[0][L], R=[B_w], W=[gbb], dst=gbb)
            ct = [A.alloc([128, 8, 512], BF16) for _ in range(2)]
            ctb = [K.buf("ct%d" % i, True) for i in range(2)]
            xt = [A.alloc([128, D], F32) for _ in range(2)]
            xtb = [K.buf("p3x%d" % i, True) for i in range(2)]
            z = [A.alloc([128, D], F32) for _ in range(2)]
            zb = [K.buf("p3z%d" % i) for i in range(2)]
            xb = [A.alloc([128, D], BF16) for _ in range(2)]
            xbb = [K.buf("p3xb%d" % i) for i in range(2)]
            stg = [A.alloc([128, 8, 128], BF16) for _ in range(2)]
            stgb = [K.buf("p3s%d" % i) for i in range(2)]
            small = [A.alloc([128, 16], F32) for _ in range(2)]
            smallb = [K.buf("p3sm%d" % i) for i in range(2)]
            if not even:
                wr = A.alloc([128, 8, D], F32)
                wrb = K.buf("wr", True)
                for e in range(8):
                    K.dma("sp", wr[:, e, :], od_router[li, e], R=[B_w], W=[wrb], dst=wrb)
                junk = A.alloc([128, D], F32)
                junkb = K.buf("junk")
                rt = [A.alloc([128, 96], F32) for _ in range(2)]
                rtb = [K.buf("rt%d" % i) for i in range(2)]
                for i_ in range(2):
                    K.op("pool", lambda h, i_=i_: h.memset(rt[i_], 0.0), R=[], W=[rtb[i_]])
                ohb = [A.alloc([128, 8], BF16) for _ in range(2)]
                ohbb = [K.buf("ohb%d" % i) for i in range(2)]
                ustr = A.alloc([128, 128], BF16)
                ustrb = K.buf("ustr", True)
                K.dma("sp", ustr, c_ustrict[:, :], R=[B_w], W=[ustrb], dst=ustrb)
                onesb = A.alloc([128, 128], BF16)
                K.op("pool", lambda h: h.memset(onesb, 1.0), R=[], W=[ustrb])
                tot = A.alloc([128, 8], F32)
                totb = K.buf("tot")
                K.op("pool", lambda h: h.memset(tot, 0.0), R=[], W=[totb])

            def load_ct(g):
                K.dma("sp", ct[g % 2], CATT[:, g * 512:(g + 1) * 512].rearrange("(kt p) t -> p kt t", p=128),
                      R=[B_CATT], W=[ctb[g % 2]], dst=ctb[g % 2])

            load_ct(0)
            for tt in range(NT):
                g = tt // 4
                i = tt % 2
                if tt % 4 == 0 and g + 1 < NG:
                    load_ct(g + 1)
                K.dma("sp", xt[i], xr_src[tt * 128:(tt + 1) * 128, :], R=[xr_src_b], W=[xtb[i]], dst=xtb[i])
                c = ct[g % 2]
                t4 = tt % 4
                for hf in range(2):
                    ps, pb = PS[(tt * 2 + hf) % 4], psb[(tt * 2 + hf) % 4]
                    for kt in range(8):
                        K.mm(ps[:, :], c[:, kt, t4 * 128:(t4 + 1) * 128], wo[:, kt, hf * 512:(hf + 1) * 512], kt == 0, kt == 7,
                             R=[ctb[g % 2], wob], W=[pb])
                    K.op("dve", lambda h, ps=ps, hf=hf, i=i: h.scalar_tensor_tensor(
                        out=z[i][:, hf * 512:(hf + 1) * 512], in0=xt[i][:, hf * 512:(hf + 1) * 512], scalar=ALPHA,
                        in1=ps[:, :], op0=ALU.mult, op1=ALU.add), R=[pb, xtb[i]], W=[zb[i]])
                layer_norm(z[i], zb[i], 0, L, small[i], smallb[i], g_ap, b_ap, gbb)
                K.dma("sp", xr_dst[tt * 128:(tt + 1) * 128, :], z[i], R=[zb[i]], W=[xr_dst_b], dst=xr_dst_b)
                K.act(xb[i], z[i], AF.Copy, R=[zb[i]], W=[xbb[i]])
                emit_xT(xb[i], xbb[i], tt, ident, identb, stg[i], stgb[i])
                if not even:
                    r = rt[i]
                    rb = rtb[i]
                    lg, m8_, dd, g1, g2, c1, c2 = r[:, 0:8], r[:, 8:16], r[:, 16:17], r[:, 66:67], r[:, 67:68], r[:, 24:32], r[:, 32:40]
                    oh0, oh1, rank, j8 = r[:, 48:56], r[:, 56:64], r[:, 72:80], r[:, 80:88]
                    for e in range(8):
                        K.op("dve", lambda h, e=e, i=i, lg=lg: h.scalar_tensor_tensor(
                            out=junk, in0=z[i], scalar=1.0, in1=wr[:, e, :], op0=ALU.mult, op1=ALU.mult, accum_out=lg[:, e:e + 1]),
                            R=[zb[i], wrb], W=[junkb, rb])
                    K.op("dve", lambda h, m8_=m8_, lg=lg: h.max(out=m8_, in_=lg), R=[rb], W=[rb])
                    K.op("dve", lambda h, dd=dd, m8_=m8_: h.tensor_tensor(out=dd, in0=m8_[:, 1:2], in1=m8_[:, 0:1], op=ALU.subtract), R=[rb], W=[rb])
                    K.act(dd, dd, AF.Exp, R=[rb], W=[rb])
                    K.op("dve", lambda h, g1=g1, dd=dd: h.tensor_scalar(out=g1, in0=dd, scalar1=1.0, scalar2=None, op0=ALU.add), R=[rb], W=[rb])
                    K.op("dve", lambda h, g1=g1: h.reciprocal(out=g1, in_=g1), R=[rb], W=[rb])
                    K.op("dve", lambda h, g1=g1, g2=g2, dd=dd: h.tensor_tensor(out=g2, in0=dd, in1=g1, op=ALU.mult), R=[rb], W=[rb])
                    K.op("dve", lambda h, c1=c1, lg=lg, m8_=m8_, g1=g1: h.tensor_scalar(out=c1, in0=lg, scalar1=m8_[:, 0:1], scalar2=g1, op0=ALU.is_equal, op1=ALU.mult), R=[rb], W=[rb])
                    K.op("dve", lambda h, c2=c2, lg=lg, m8_=m8_, g2=g2: h.tensor_scalar(out=c2, in0=lg, scalar1=m8_[:, 1:2], scalar2=g2, op0=ALU.is_equal, op1=ALU.mult), R=[rb], W=[rb])
                    K.op("dve", lambda h, c1=c1, c2=c2: h.tensor_tensor(out=c1, in0=c1, in1=c2, op=ALU.add), R=[rb], W=[rb])
                    K.dma("sp", COMB[tt * 128:(tt + 1) * 128, :], c1, R=[rb], W=[B_COMB], dst=B_COMB)
                    K.op("dve", lambda h, oh0=oh0, lg=lg, m8_=m8_: h.tensor_scalar(out=oh0, in0=lg, scalar1=m8_[:, 0:1], scalar2=None, op0=ALU.is_equal), R=[rb], W=[rb])
                    K.op("dve", lambda h, oh1=oh1, lg=lg, m8_=m8_: h.tensor_scalar(out=oh1, in0=lg, scalar1=m8_[:, 1:2], scalar2=None, op0=ALU.is_equal), R=[rb], W=[rb])
                    K.op("dve", lambda h, i=i, oh0=oh0, oh1=oh1: h.tensor_tensor(out=ohb[i], in0=oh0, in1=oh1, op=ALU.add), R=[rb], W=[ohbb[i]])
                    pr, prb = PS[4 + i], psb[4 + i]
                    K.mm(pr[:, 0:8], ustr, ohb[i], True, True, R=[ustrb, ohbb[i]], W=[prb])
                    K.mm(pr[:, 8:16], onesb, ohb[i], True, True, R=[ustrb, ohbb[i]], W=[prb])
                    K.op("dve", lambda h, rank=rank, pr=pr: h.tensor_tensor(out=rank, in0=pr[:, 0:8], in1=tot, op=ALU.add), R=[prb, totb], W=[rb])
                    K.op("dve", lambda h, pr=pr: h.tensor_tensor(out=tot, in0=pr[:, 8:16], in1=tot, op=ALU.add), R=[prb, totb], W=[totb])
                    K.op("dve", lambda h, oh0=oh0, rank=rank, j8=j8, o=r[:, 64:65]: h.scalar_tensor_tensor(
                        out=j8, in0=oh0, scalar=1.0, in1=rank, op0=ALU.mult, op1=ALU.mult, accum_out=o), R=[rb], W=[rb])
                    K.op("dve", lambda h, oh1=oh1, rank=rank, j8=j8, o=r[:, 65:66]: h.scalar_tensor_tensor(
                        out=j8, in0=oh1, scalar=1.0, in1=rank, op0=ALU.mult, op1=ALU.mult, accum_out=o), R=[rb], W=[rb])
                    K.dma("sp", ROUTE[tt * 128:(tt + 1) * 128, :], r[:, 48:72], R=[rb], W=[B_ROUTE], dst=B_ROUTE)
            if not even:
                K.dma("sp", CNT[:, :], tot, R=[totb], W=[B_CNT], dst=B_CNT)
            K.end_phase()

        def phase_p4(L, xr_src, xr_src_b, xr_dst, xr_dst_b, last):
            even = (L % 2 == 0)
            li = L // 2
            A.reset()
            ident, identb = consts_common()
            Wg, Wu, Wd = (ev_g, ev_u, ev_d) if even else (od_g, od_u, od_d)
            experts = [0] if even else list(range(NE))
            g_ap = A.alloc([128, D], F32)
            b_ap = A.alloc([128, D], F32)
            gbb = K.buf("gb4", True)
            K.dma("sp", g_ap, lng[1][L], R=[B_w], W=[gbb], dst=gbb)
            K.dma("sp", b_ap, lnb[1][L], R=[B_w], W=[gbb], dst=gbb)
            xTb = A.alloc([128, 8, 1024], BF16)
            xTbb = K.buf("xTb", True)
            yacc = A.alloc([128, 8, D], F32)
            yab = [K.buf("yacc%d" % i, True) for i in range(8)]
            hT = A.alloc([128, 14, 1024], BF16)
            hTb = [K.buf("hT%d" % i) for i in range(14)]
            wgu = [A.alloc([128, 2, 8, 256], BF16) for _ in range(2)]
            wgub = [K.buf("wgu%d" % i, "sw") for i in range(2)]
            wd = [A.alloc([128, 14, D], BF16) for _ in range(2)]
            wdb = [K.buf("wd%d" % i, "sw") for i in range(2)]
            sg = [A.alloc([128, 512], F32) for _ in range(2)]
            sgb = [K.buf("sg%d" % i) for i in range(2)]
            xb = [A.alloc([128, D], BF16) for _ in range(2)]
            xbb = [K.buf("p4xb%d" % i) for i in range(2)]
            stg = [A.alloc([128, 8, 128], BF16) for _ in range(2)]
            stgb = [K.buf("p4s%d" % i) for i in range(2)]
            small = [A.alloc([128, 16], F32) for _ in range(2)]
            smallb = [K.buf("p4sm%d" % i) for i in range(2)]
            comb = A.alloc([128, 8, 8], F32)
            combb = K.buf("comb_s", True)

            gl = []
            dl = []
            for blk in range(NBLK):
                for e in experts:
                    for hf in range(2):
                        dl.append((blk, e, hf))
                        for fg in range(7):
                            gl.append((blk, e, hf, fg))
            st_ = {"g": 0, "d": 0}

            def load_g(n):
                if n >= len(gl):
                    return
                blk, e, hf, fg = gl[n]
                f0 = hf * 1792 + fg * 256
                i = n % 2
                K.dma("pool", wgu[i][:, 0], Wg[li, e, :, f0:f0 + 256].rearrange("(kt p) f -> p kt f", p=128), R=[B_w], W=[wgub[i]], dst=wgub[i])
                K.dma("pool", wgu[i][:, 1], Wu[li, e, :, f0:f0 + 256].rearrange("(kt p) f -> p kt f", p=128), R=[B_w], W=[wgub[i]], dst=wgub[i])

            def load_d(n):
                if n >= len(dl):
                    return
                blk, e, hf = dl[n]
                i = n % 2
                K.dma("pool", wd[i], Wd[li, e, hf * 1792:(hf + 1) * 1792, :].rearrange("(fc p) d -> p fc d", p=128), R=[B_w], W=[wdb[i]], dst=wdb[i])

            load_g(0)
            load_d(0)
            gi = 0
            di = 0
            pcount = 0
            ycount = 0
            for blk in range(NBLK):
                t0 = blk * 1024
                K.dma("sp", xTb, XT[:, t0:t0 + 1024].rearrange("(kt p) t -> p kt t", p=128), R=[B_XT], W=[xTbb], dst=xTbb)
                for tt in range(8):
                    K.dma("sp", yacc[:, tt, :], xr_src[t0 + tt * 128:t0 + (tt + 1) * 128, :], R=[xr_src_b], W=[yab[tt]], dst=yab[tt])
                    K.op("pool", lambda h, tt=tt: h.tensor_scalar(out=yacc[:, tt, :], in0=yacc[:, tt, :], scalar1=ALPHA, scalar2=0.0, op0=ALU.mult, op1=ALU.add),
                         R=[yab[tt]], W=[yab[tt]])
                if not even:
                    K.dma("sp", comb, COMB[t0:t0 + 1024, :].rearrange("(tt p) e -> p tt e", p=128), R=[B_COMB], W=[combb], dst=combb)
                for e in experts:
                    for hf in range(2):
                        load_d(di + 1)
                        wdt, wdtb = wd[di % 2], wdb[di % 2]
                        di += 1
                        for fg in range(7):
                            load_g(gi + 1)
                            w_, w_b = wgu[gi % 2], wgub[gi % 2]
                            gi += 1
                            for fc2 in range(2):
                                fcl = fg * 2 + fc2
                                for tg in range(2):
                                    pg, pgb = PS[(pcount % 2) * 2], psb[(pcount % 2) * 2]
                                    pu, pub = PS[(pcount % 2) * 2 + 1], psb[(pcount % 2) * 2 + 1]
                                    s_, s_b = sg[pcount % 2], sgb[pcount % 2]
                                    pcount += 1
                                    for kt in range(8):
                                        K.mm(pg[:, :], w_[:, 0, kt, fc2 * 128:(fc2 + 1) * 128], xTb[:, kt, tg * 512:(tg + 1) * 512],
                                             kt == 0, kt == 7, R=[w_b, xTbb], W=[pgb])
                                    for kt in range(8):
                                        K.mm(pu[:, :], w_[:, 1, kt, fc2 * 128:(fc2 + 1) * 128], xTb[:, kt, tg * 512:(tg + 1) * 512],
                                             kt == 0, kt == 7, R=[w_b, xTbb], W=[pub])
                                    K.act(s_, pg[:, :], AF.Silu, R=[pgb], W=[s_b])
                                    K.op("dve", lambda h, fcl=fcl, tg=tg, s_=s_, pu=pu: h.tensor_tensor(
                                        out=hT[:, fcl, tg * 512:(tg + 1) * 512], in0=pu[:, :], in1=s_, op=ALU.mult),
                                        R=[pub, s_b], W=[hTb[fcl]])
                        for tt in range(8):
                            for dh in range(2):
                                py, pyb = PS[4 + ycount % 2], psb[4 + ycount % 2]
                                ycount += 1
                                for fcl in range(14):
                                    K.mm(py[:, :], hT[:, fcl, tt * 128:(tt + 1) * 128], wdt[:, fcl, dh * 512:(dh + 1) * 512],
                                         fcl == 0, fcl == 13, R=[hTb[fcl], wdtb], W=[pyb])
                                sc = 1.0 if even else comb[:, tt, e:e + 1]
                                rds = [pyb, yab[tt]] + ([] if even else [combb])
                                K.op("dve", lambda h, py=py, tt=tt, dh=dh, sc=sc: h.scalar_tensor_tensor(
                                    out=yacc[:, tt, dh * 512:(dh + 1) * 512], in0=py[:, :], scalar=sc,
                                    in1=yacc[:, tt, dh * 512:(dh + 1) * 512], op0=ALU.mult, op1=ALU.add), R=rds, W=[yab[tt]])
                for tt in range(8):
                    i = tt % 2
                    zt = yacc[:, tt, :]
                    layer_norm(zt, yab[tt], 1, L, small[i], smallb[i], g_ap, b_ap, gbb)
                    gt = blk * 8 + tt
                    if last:
                        K.dma("sp", y_out[gt * 128:(gt + 1) * 128, :], zt, R=[yab[tt]], W=[B_Y], dst=B_Y)
                    else:
                        K.dma("sp", xr_dst[gt * 128:(gt + 1) * 128, :], zt, R=[yab[tt]], W=[xr_dst_b], dst=xr_dst_b)
                        K.act(xb[i], zt, AF.Copy, R=[yab[tt]], W=[xbb[i]])
                        emit_xT(xb[i], xbb[i], gt, ident, identb, stg[i], stgb[i])
            K.end_phase()

        def phase_p4s(L, xr_src, xr_src_b, xr_dst, xr_dst_b, last):
            li = L // 2
            A.reset()
            ident, identb = consts_common()
            g_ap = A.alloc([128, D], F32)
            b_ap = A.alloc([128, D], F32)
            gbb = K.buf("gb4", True)
            K.dma("sp", g_ap, lng[1][L], R=[B_w], W=[gbb], dst=gbb)
            K.dma("sp", b_ap, lnb[1][L], R=[B_w], W=[gbb], dst=gbb)
            misc = A.alloc([128, 64], F32)
            miscb = K.buf("misc", True)
            K.dma("sp", misc, c_misc[:, :], R=[B_w], W=[miscb], dst=miscb)
            cnt = A.alloc([128, 8], F32)
            cntb = K.buf("cnt_s", True)
            K.dma("sp", cnt, CNT[:, :], R=[B_CNT], W=[cntb], dst=cntb)
            tb_ = A.alloc([128, 64], F32)
            tbb = K.buf("tb")
            nt, tbase, tend, sbase, tmp8 = tb_[:, 0:8], tb_[:, 8:16], tb_[:, 16:24], tb_[:, 24:32], tb_[:, 32:40]
            K.op("dve", lambda h: h.tensor_scalar(out=nt, in0=cnt, scalar1=0.0, scalar2=None, op0=ALU.is_gt), R=[cntb], W=[tbb])
            for m in range(1, 8):
                K.op("dve", lambda h, m=m: h.scalar_tensor_tensor(out=nt, in0=cnt, scalar=512.0 * m, in1=nt, op0=ALU.is_gt, op1=ALU.add),
                     R=[cntb, tbb], W=[tbb])
            K.op("dve", lambda h: h.memset(tbase[:, 0:1], 0.0), R=[], W=[tbb])
            for e in range(1, 8):
                K.op("dve", lambda h, e=e: h.tensor_tensor(out=tbase[:, e:e + 1], in0=tbase[:, e - 1:e], in1=nt[:, e - 1:e], op=ALU.add), R=[tbb], W=[tbb])
            K.op("dve", lambda h: h.tensor_tensor(out=tend, in0=tbase, in1=nt, op=ALU.add), R=[tbb], W=[tbb])
            K.op("dve", lambda h: h.tensor_scalar(out=sbase, in0=tbase, scalar1=512.0, scalar2=None, op0=ALU.mult), R=[tbb], W=[tbb])
            te = A.alloc([128, NTILE], F32)
            teb = K.buf("te")
            jrow = misc[:, 0:NTILE]
            K.op("dve", lambda h: h.tensor_scalar(out=te, in0=jrow, scalar1=tend[:, 0:1], scalar2=None, op0=ALU.is_ge), R=[miscb, tbb], W=[teb])
            for e in range(1, 8):
                K.op("dve", lambda h, e=e: h.scalar_tensor_tensor(out=te, in0=jrow, scalar=tend[:, e:e + 1], in1=te, op0=ALU.is_ge, op1=ALU.add),
                     R=[miscb, tbb, teb], W=[teb])
            K.op("dve", lambda h: h.tensor_scalar(out=te, in0=te, scalar1=7.0, scalar2=None, op0=ALU.min), R=[teb], W=[teb])
            idxWf = A.alloc([128, NTILE, 8], F32)
            idxW = A.alloc([128, NTILE, 8], I32)
            idxDf = A.alloc([128, NTILE, 14], F32)
            idxD = A.alloc([128, NTILE, 14], I32)
            idxb = K.buf("idx")
            te2 = A.alloc([128, NTILE], F32)
            K.op("dve", lambda h: h.tensor_scalar(out=te2, in0=te, scalar1=1024.0, scalar2=float(li * 8192), op0=ALU.mult, op1=ALU.add), R=[teb], W=[idxb])
            for j in range(NTILE):
                K.op("dve", lambda h, j=j: h.tensor_scalar(out=idxWf[:, j, :], in0=misc[:, 53:61], scalar1=te2[:, j:j + 1], scalar2=None, op0=ALU.add),
                     R=[miscb, idxb], W=[idxb])
            K.op("dve", lambda h: h.tensor_scalar(out=idxWf, in0=idxWf, scalar1=2.0, scalar2=None, op0=ALU.mult), R=[idxb], W=[idxb])
            K.op("dve", lambda h: h.tensor_copy(out=idxW, in_=idxWf), R=[idxb], W=[idxb])
            K.op("dve", lambda h: h.tensor_scalar(out=te2, in0=te, scalar1=1792.0, scalar2=float(li * 14336), op0=ALU.mult, op1=ALU.add), R=[teb, idxb], W=[idxb])
            for j in range(NTILE):
                K.op("dve", lambda h, j=j: h.tensor_scalar(out=idxDf[:, j, :], in0=misc[:, 24:38], scalar1=te2[:, j:j + 1], scalar2=None, op0=ALU.add),
                     R=[miscb, idxb], W=[idxb])
            K.op("dve", lambda h: h.tensor_copy(out=idxD, in_=idxDf), R=[idxb], W=[idxb])

            xr = [A.alloc([128, 4, D], BF16) for _ in range(2)]
            xrb = [K.buf("xr%d" % i, True) for i in range(2)]
            K.op("pool", lambda h: h.memset(xr[0], 0.0), R=[], W=[xrb[0]])
            for j in range(NTILE):
                K.dma("pool", XS[j * 512:(j + 1) * 512, :].rearrange("(a p) d -> p a d", p=128), xr[0], R=[xrb[0]], W=[B_XS], dst=B_XS)
            slotf = A.alloc([128, NT, 2], F32)
            sloti = A.alloc([128, NT, 2], I32)
            gts = A.alloc([128, NT, 2], F32)
            slb = K.buf("slots")
            rte = [A.alloc([128, 24], F32) for _ in range(2)]
            rteb = [K.buf("rte%d" % i, True) for i in range(2)]
            xt = [A.alloc([128, D], F32) for _ in range(2)]
            xtb = [K.buf("p4x%d" % i, True) for i in range(2)]
            xb = [A.alloc([128, D], BF16) for _ in range(2)]
            xbb = [K.buf("p4xb%d" % i) for i in range(2)]
            j8 = A.alloc([128, 8], F32)
            j8b = K.buf("j8")
            for tt in range(NT):
                i = tt % 2
                K.dma("sp", rte[i], ROUTE[tt * 128:(tt + 1) * 128, :], R=[B_ROUTE], W=[rteb[i]], dst=rteb[i])
                K.dma("sp", xt[i], xr_src[tt * 128:(tt + 1) * 128, :], R=[xr_src_b], W=[xtb[i]], dst=xtb[i])
                K.act(xb[i], xt[i], AF.Copy, R=[xtb[i]], W=[xbb[i]])
                for k2 in range(2):
                    K.op("dve", lambda h, i=i, k2=k2, tt=tt: h.scalar_tensor_tensor(
                        out=j8, in0=rte[i][:, k2 * 8:(k2 + 1) * 8], scalar=1.0, in1=sbase, op0=ALU.mult, op1=ALU.mult,
                        accum_out=slotf[:, tt, k2:k2 + 1]), R=[rteb[i], tbb], W=[j8b, slb])
                K.op("dve", lambda h, i=i, tt=tt: h.tensor_tensor(out=slotf[:, tt, :], in0=slotf[:, tt, :], in1=rte[i][:, 16:18], op=ALU.add),
                     R=[rteb[i], slb], W=[slb])
                K.op("dve", lambda h, tt=tt: h.tensor_copy(out=sloti[:, tt, :], in_=slotf[:, tt, :]), R=[slb], W=[slb])
                K.op("dve", lambda h, i=i, tt=tt: h.tensor_copy(out=gts[:, tt, :], in_=rte[i][:, 18:20]), R=[rteb[i]], W=[slb])
                for k2 in range(2):
                    K.idma("pool", lambda h, i=i, tt=tt, k2=k2: h.indirect_dma_start(
                        out=XS[:, :], out_offset=bass.IndirectOffsetOnAxis(sloti[:, tt, k2:k2 + 1], 0), in_=xb[i], in_offset=None),
                        R=[xbb[i], slb], W=[B_XS], dst=B_XS)

            main_mark = A.off
            xTb = A.alloc([128, 8, 512], BF16)
            xTbb = K.buf("xTb")
            hT = A.alloc([128, 14, 512], BF16)
            hTb = [K.buf("hT%d" % i) for i in range(14)]
            wgu = [A.alloc([128, 8, 1792], BF16) for _ in range(3)]
            wgub = [K.buf("wgu%d" % i, "sw") for i in range(3)]
            wd = [A.alloc([128, 14, D], BF16) for _ in range(1)]
            wdb = [K.buf("wd%d" % i, "sw") for i in range(1)]
            sg = [A.alloc([128, 512], F32) for _ in range(2)]
            sgb = [K.buf("sg%d" % i) for i in range(2)]
            ysb = [A.alloc([128, 4, D], F32) for _ in range(1)]
            ysbb = [K.buf("ysb%d" % i) for i in range(1)]
            Wg_v = od_g.rearrange("l e k (q f) -> (l e k q) f", q=2)
            Wu_v = od_u.rearrange("l e k (q f) -> (l e k q) f", q=2)
            Wd_v = od_d.rearrange("l e (f2 t) d -> (l e f2) (t d)", t=2)
            gl = [(j, hf, gu) for j in range(NTILE) for hf in range(2) for gu in range(2)]
            dl = [(j, hf) for j in range(NTILE) for hf in range(2)]
            issued = {"g": 0, "d": 0}

            def ensure_g(n):
                while issued["g"] <= n and issued["g"] < len(gl):
                    m = issued["g"]
                    j, hf, gu = gl[m]
                    i = m % 3
                    Wv = Wg_v if gu == 0 else Wu_v
                    for kt in range(8):
                        K.idma("pool", lambda h, i=i, Wv=Wv, hf=hf, j=j, kt=kt: h.indirect_dma_start(
                            out=wgu[i][:, kt, :], out_offset=None, in_=Wv[:, :],
                            in_offset=bass.IndirectOffsetOnAxis(idxW[:, j, kt:kt + 1], 0), element_offset=hf * 1792),
                            R=[B_w, idxb], W=[wgub[i]], dst=wgub[i])
                    issued["g"] += 1

            def ensure_d(n):
                while issued["d"] <= n and issued["d"] < len(dl):
                    m = issued["d"]
                    j, hf = dl[m]
                    for r2 in range(7):
                        K.idma("pool", lambda h, r2=r2, j=j, hf=hf: h.indirect_dma_start(
                            out=wd[0][:, 2 * r2:2 * r2 + 2, :].rearrange("p a d -> p (a d)"), out_offset=None, in_=Wd_v[:, :],
                            in_offset=bass.IndirectOffsetOnAxis(idxD[:, j, hf * 7 + r2:hf * 7 + r2 + 1], 0)),
                            R=[B_w, idxb], W=[wdb[0]], dst=wdb[0])
                    issued["d"] += 1

            pcount = 0
            ycount = 0
            for j in range(NTILE):
                xi = j % 2
                K.dma("sp", xr[xi], XS[j * 512:(j + 1) * 512, :].rearrange("(a p) d -> p a d", p=128), R=[B_XS], W=[xrb[xi]], dst=xrb[xi])
                for a in range(4):
                    xv = xr[xi][:, a, :].rearrange("p (q k) -> p k q", k=8)
                    for kt in range(8):
                        K.tr(PSBv[:, kt, :], xv[:, kt, :], ident, R=[xrb[xi], identb], W=[psbb])
                    K.op("dve", lambda h, a=a: h.tensor_copy(out=xTb[:, :, a * 128:(a + 1) * 128], in_=PSBv), R=[psbb], W=[xTbb])
                yb_, ybb_ = ysb[0], ysbb[0]
                for hf in range(2):
                    n0 = (j * 2 + hf) * 2
                    ensure_g(n0 + 1)
                    ensure_d(j * 2 + hf)
                    ensure_g(n0 + 2)
                    wg_, wg_b = wgu[n0 % 3], wgub[n0 % 3]
                    wu_, wu_b = wgu[(n0 + 1) % 3], wgub[(n0 + 1) % 3]
                    wdt, wdtb = wd[0], wdb[0]
                    for c in range(14):
                        pg, pgb = PS[(pcount % 2) * 2], psb[(pcount % 2) * 2]
                        pu, pub = PS[(pcount % 2) * 2 + 1], psb[(pcount % 2) * 2 + 1]
                        s_, s_b = sg[pcount % 2], sgb[pcount % 2]
                        pcount += 1
                        for kt in range(8):
                            K.mm(pg[:, :], wg_[:, kt, :].rearrange("p (m c) -> p c m", c=14)[:, c, :], xTb[:, kt, :], kt == 0, kt == 7,
                                 R=[wg_b, xTbb], W=[pgb])
                        for kt in range(8):
                            K.mm(pu[:, :], wu_[:, kt, :].rearrange("p (m c) -> p c m", c=14)[:, c, :], xTb[:, kt, :], kt == 0, kt == 7,
                                 R=[wu_b, xTbb], W=[pub])
                        K.act(s_, pg[:, :], AF.Silu, R=[pgb], W=[s_b])
                        K.op("dve", lambda h, c=c, s_=s_, pu=pu: h.tensor_tensor(out=hT[:, c, :], in0=pu[:, :], in1=s_, op=ALU.mult),
                             R=[pub, s_b], W=[hTb[c]])
                    ensure_g(n0 + 3)
                    for a in range(4):
                        for dh in range(2):
                            py, pyb = PS[4 + ycount % 2], psb[4 + ycount % 2]
                            ycount += 1
                            for c in range(14):
                                K.mm(py[:, :], hT[:, c, a * 128:(a + 1) * 128], wdt[:, c, dh * 512:(dh + 1) * 512],
                                     c == 0, c == 13, R=[hTb[c], wdtb], W=[pyb])
                            if hf == 0:
                                K.op("dve", lambda h, py=py, a=a, dh=dh, yb_=yb_: h.tensor_copy(out=yb_[:, a, dh * 512:(dh + 1) * 512], in_=py[:, :]),
                                     R=[pyb], W=[ybb_])
                            else:
                                K.op("dve", lambda h, py=py, a=a, dh=dh, yb_=yb_: h.tensor_tensor(
                                    out=yb_[:, a, dh * 512:(dh + 1) * 512], in0=py[:, :], in1=yb_[:, a, dh * 512:(dh + 1) * 512], op=ALU.add),
                                    R=[pyb, ybb_], W=[ybb_])
                K.dma("sp", YS[j * 512:(j + 1) * 512, :].rearrange("(a p) d -> p a d", p=128), yb_, R=[ybb_], W=[B_YS], dst=B_YS)


            K.barrier()
            A.off = main_mark
            ya = [[A.alloc([128, D], F32) for _ in range(2)] for _ in range(2)]
            yab_ = [[K.buf("ya%d%d" % (i, k2), "sw") for k2 in range(2)] for i in range(2)]
            stg = [A.alloc([128, 8, 128], BF16) for _ in range(2)]
            stgb = [K.buf("p4s%d" % i) for i in range(2)]
            small = [A.alloc([128, 16], F32) for _ in range(2)]
            smallb = [K.buf("p4sm%d" % i) for i in range(2)]
            for tt in range(NT):
                i = tt % 2
                K.dma("sp", xt[i], xr_src[tt * 128:(tt + 1) * 128, :], R=[xr_src_b], W=[xtb[i]], dst=xtb[i])
                for k2 in range(2):
                    K.idma("pool", lambda h, i=i, tt=tt, k2=k2: h.indirect_dma_start(
                        out=ya[i][k2], out_offset=None, in_=YS[:, :], in_offset=bass.IndirectOffsetOnAxis(sloti[:, tt, k2:k2 + 1], 0)),
                        R=[B_YS, slb], W=[yab_[i][k2]], dst=yab_[i][k2])
                zt = xt[i]
                K.op("dve", lambda h, i=i, tt=tt: h.tensor_scalar(out=ya[i][0], in0=ya[i][0], scalar1=gts[:, tt, 0:1], scalar2=None, op0=ALU.mult),
                     R=[yab_[i][0], slb], W=[yab_[i][0]])
                K.op("dve", lambda h, i=i, tt=tt: h.scalar_tensor_tensor(out=ya[i][0], in0=ya[i][1], scalar=gts[:, tt, 1:2], in1=ya[i][0], op0=ALU.mult, op1=ALU.add),
                     R=[yab_[i][0], yab_[i][1], slb], W=[yab_[i][0]])
                K.op("dve", lambda h, zt=zt, i=i: h.scalar_tensor_tensor(out=zt, in0=zt, scalar=ALPHA, in1=ya[i][0], op0=ALU.mult, op1=ALU.add),
                     R=[xtb[i], yab_[i][0]], W=[xtb[i]])
                layer_norm(zt, xtb[i], 1, L, small[i], smallb[i], g_ap, b_ap, gbb)
                if last:
                    K.dma("sp", y_out[tt * 128:(tt + 1) * 128, :], zt, R=[xtb[i]], W=[B_Y], dst=B_Y)
                else:
                    K.dma("sp", xr_dst[tt * 128:(tt + 1) * 128, :], zt, R=[xtb[i]], W=[xr_dst_b], dst=xr_dst_b)
                    K.act(xb[i], zt, AF.Copy, R=[xtb[i]], W=[xbb[i]])
                    emit_xT(xb[i], xbb[i], tt, ident, identb, stg[i], stgb[i])
            K.end_phase()

        phase_p0()
        cur, curb = x_in, B_xin
        for n, L in enumerate(layers):
            last = (n == len(layers) - 1)
            phase_p1(L)
            if last and stop == "p1":
                break
            phase_p2(L)
            if last and stop == "p2":
                break
            phase_p3(L, cur, curb, XR[0], B_XR[0])
            if last and stop == "p3":
                break
            if SPARSE and L % 2 == 1:
                phase_p4s(L, XR[0], B_XR[0], XR[1], B_XR[1], last)
            else:
                phase_p4(L, XR[0], B_XR[0], XR[1], B_XR[1], last)
            cur, curb = XR[1], B_XR[1]
        sp = K.E["sp"]
        sp.prog.append(([(B_Y.sem, K.semcnt[B_Y.sem])], None, None, 0))

        with nc.Block() as block:
            @block.tensor
            def _(h):
                K.replay("pe", h)

            @block.scalar
            def _(h):
                K.replay("act", h)

            @block.vector
            def _(h):
                K.replay("dve", h)

            @block.gpsimd
            def _(h):
                K.replay("pool", h)

            @block.sync
            def _(h):
                K.replay("sp", h)
    return nc


def make_consts(S):
    NG = S // 512
    bf = ml_dtypes.bfloat16
    c = {}
    c["c_ident_bf"] = np.eye(128, dtype=np.float32).astype(bf)
    c["c_ident_f"] = np.eye(128, dtype=np.float32)
    c["c_tri"] = np.triu(np.ones((128, 128), np.float32)).astype(bf)
    koh = np.zeros((16, S), np.float32)
    for n in range(min(16, S // 256)):
        koh[n, n * 256:(n + 1) * 256] = 1.0
    c["c_koh"] = koh.astype(bf)
    past = np.zeros((NG, 128, 4, 16), np.float32)
    own = np.zeros((NG, 128, 4, 16), np.float32)
    for g in range(NG):
        for t4 in range(4):
            qblk = (g * 4 + t4) // 2
            past[g, :, t4, qblk:] = -1e30
            own[g, :, t4, qblk] = 1.0
    c["c_past"] = past
    c["c_own"] = own
    inv = np.zeros((4, 128, 16), np.float32)
    for g4, w in enumerate(POOL_W):
        for t in range(16):
            inv[g4, :, t] = 1.0 / min(t + 1, w)
    c["c_invcnt"] = inv
    c["c_ustrict"] = np.triu(np.ones((128, 128), np.float32), k=1).astype(bf)
    misc = np.zeros((128, 64), np.float32)
    misc[:, 0:24] = np.arange(24, dtype=np.float32)[None, :]
    for cc in range(14):
        misc[:, 24 + cc] = (cc // 7) * 896 + 7 * np.arange(128, dtype=np.float32) + (cc % 7)
    misc[:, 52] = np.arange(128, dtype=np.float32)
    misc[:, 53:61] = np.arange(128, dtype=np.float32)[:, None] * 8 + np.arange(8, dtype=np.float32)[None, :]
    c["c_misc"] = misc
    return c


def prep_shared(inp):
    f = lambda a: np.ascontiguousarray(np.asarray(a, dtype=np.float32))
    sh = {}
    for nm in ("ln1_g", "ln1_b", "ln2_g", "ln2_b"):
        a = f(inp[nm])
        sh[nm] = np.ascontiguousarray(np.broadcast_to(a[:, None, :], (a.shape[0], 128, a.shape[1])))
    sh["ev_w_in"] = f(inp["ev_w_in"])
    sh["ev_negb"] = f(inp["ev_b_forget"]).reshape(2, 8, 1)
    sh["ev_pool_w"] = f(inp["ev_pool_w"])
    sh["ev_pool_scale"] = f(inp["ev_pool_scale"]).reshape(2, 4, 128, 1)
    sh["ev_w_out"] = f(inp["ev_w_out"])
    sh["ev_ffn_gate"] = f(inp["ev_ffn_gate"])[:, None]
    sh["ev_ffn_up"] = f(inp["ev_ffn_up"])[:, None]
    sh["ev_ffn_down"] = f(inp["ev_ffn_down"])[:, None]
    sh["od_w_in"] = f(inp["od_w_in"])
    sh["od_w_out"] = f(inp["od_w_out"])
    r = f(inp["od_router"])
    rt = np.transpose(r, (0, 2, 1))
    sh["od_router"] = np.ascontiguousarray(np.broadcast_to(rt[:, :, None, :], (2, 8, 128, D)))
    sh["od_exp_gate"] = f(inp["od_exp_gate"])
    sh["od_exp_up"] = f(inp["od_exp_up"])
    sh["od_exp_down"] = f(inp["od_exp_down"])
    return sh


def run(inputs, S, layers, dbg=False, stop=None):
    x = np.asarray(inputs["x"], dtype=np.float32)
    B = x.shape[0]
    sh = prep_shared(inputs)
    sh.update(make_consts(S))
    nc = build_nc(S, layers, 4, dbg, stop)
    in_maps = []
    for b in range(B):
        m = dict(sh)
        m["x"] = np.ascontiguousarray(x[b, :S])
        in_maps.append(m)
    res = run_bass_kernel_spmd(nc, in_maps, core_ids=list(range(B)))
    if dbg:
        return res.results
    return np.stack([np.asarray(r["y"], dtype=np.float32) for r in res.results], axis=0)


def kernel(**inputs):
    return run(inputs, 4096, [0, 1, 2, 3])
```

```python
import numpy as np
import ml_dtypes
import concourse.bass as bass
import concourse.mybir as mybir
from concourse.bass_utils import run_bass_kernel_spmd

F32 = mybir.dt.float32
BF16 = mybir.dt.bfloat16
I32 = mybir.dt.int32
SPARSE = True
AF = mybir.ActivationFunctionType
ALU = mybir.AluOpType

D = 1024
DFF = 3584
NE = 8
ALPHA = float(8 ** 0.25)
EPS = 1e-5
POOL_W = (2, 4, 8, 16)
NEG = -30000.0
ARENA_BYTES = 206 * 1024


class Buf:
    __slots__ = ("name", "w", "r", "sem")

    def __init__(self, name, sem=None):
        self.name = name
        self.w = None
        self.r = {}
        self.sem = sem


class Eng:
    def __init__(self, name, sem):
        self.name = name
        self.sem = sem
        self.n = 0
        self.seen = {}
        self.prog = []


class Kern:
    def __init__(self, nc, sems):
        self.nc = nc
        self.sems = sems
        self.semcnt = [0] * len(sems)
        self.free = list(range(5, len(sems) - 12))
        self.free_sw = list(range(len(sems) - 12, len(sems)))
        self.phase_sems_sw = []
        self.E = {n: Eng(n, i) for i, n in enumerate(["pe", "act", "dve", "pool", "sp"])}
        self.phase_sems = []

    def buf(self, name, dma=False):
        s = None
        if dma == "sw":
            s = self.free_sw.pop()
            self.phase_sems_sw.append(s)
        elif dma:
            s = self.free.pop()
            self.phase_sems.append(s)
        return Buf(name, s)

    def end_phase(self):
        self.barrier()
        self.free.extend(self.phase_sems)
        self.phase_sems = []
        self.free_sw.extend(self.phase_sems_sw)
        self.phase_sems_sw = []

    def _waits(self, eng, R, W):
        deps = {}

        def add(s, v):
            if deps.get(s, 0) < v:
                deps[s] = v

        for b in R:
            if b.w is not None:
                add(*b.w)
        for b in W:
            if b.w is not None:
                add(*b.w)
            for s, v in b.r.items():
                add(s, v)
        waits = []
        for s, v in deps.items():
            if s == eng.sem and eng.name == "pe":
                continue
            if eng.seen.get(s, 0) < v:
                eng.seen[s] = v
                waits.append((s, v))
        return waits

    def op(self, en, fn, R=(), W=()):
        eng = self.E[en]
        waits = self._waits(eng, R, W)
        eng.n += 1
        tok = (eng.sem, eng.n)
        eng.prog.append((waits, fn, eng.sem, 1))
        for b in R:
            if b.r.get(tok[0], 0) < tok[1]:
                b.r[tok[0]] = tok[1]
        for b in W:
            b.w = tok
            b.r = {}

    def dma(self, qn, out, in_, R=(), W=(), dst=None):
        eng = self.E[qn]
        waits = self._waits(eng, R, W)
        s = dst.sem
        self.semcnt[s] += 16
        tok = (s, self.semcnt[s])
        eng.prog.append((waits, (lambda h, o=out, i=in_: h.dma_start(out=o, in_=i)), s, 16))
        for b in R:
            if b.r.get(s, 0) < tok[1]:
                b.r[s] = tok[1]
        for b in W:
            b.w = tok
            b.r = {}

    def idma(self, qn, fn, R=(), W=(), dst=None):
        eng = self.E[qn]
        waits = self._waits(eng, R, W)
        s = dst.sem
        self.semcnt[s] += 16
        tok = (s, self.semcnt[s])
        eng.prog.append((waits, fn, s, 16))
        for b in R:
            if b.r.get(s, 0) < tok[1]:
                b.r[s] = tok[1]
        for b in W:
            b.w = tok
            b.r = {}

    def barrier(self):
        for en, eng in self.E.items():
            waits = []
            for o in self.E.values():
                if o is eng or o.n == 0:
                    continue
                if eng.seen.get(o.sem, 0) < o.n:
                    eng.seen[o.sem] = o.n
                    waits.append((o.sem, o.n))
            for s in range(5, len(self.sems)):
                v = self.semcnt[s]
                if v and eng.seen.get(s, 0) < v:
                    eng.seen[s] = v
                    waits.append((s, v))
            if waits:
                eng.prog.append((waits, None, None, 0))

    def mm(self, out, lhsT, rhs, start, stop, R, W):
        self.op("pe", lambda h: h.matmul(out, lhsT, rhs, start=start, stop=stop), R, W)

    def tr(self, out, in_, ident, R, W):
        self.op("pe", lambda h: h.transpose(out, in_, ident), R, W)

    def act(self, out, in_, func, R, W, bias=None, scale=None):
        kw = {}
        if bias is not None:
            kw["bias"] = bias
        if scale is not None:
            kw["scale"] = scale
        self.op("act", lambda h: h.activation(out=out, in_=in_, func=func, **kw), R, W)

    def replay(self, en, h):
        for waits, fn, s, inc in self.E[en].prog:
            for ws, wv in waits:
                h.wait_ge(self.sems[ws], wv)
            if fn is not None:
                fn(h).then_inc(self.sems[s], inc)


class Arena:
    def __init__(self, t):
        self.t = t
        self.off = 0

    def reset(self):
        self.off = 0

    def alloc(self, shape, dt):
        esz = 2 if dt == BF16 else 4
        n = 1
        for s in shape[1:]:
            n *= s
        nbytes = (n * esz + 63) // 64 * 64
        a = self.off
        self.off += nbytes
        assert self.off <= ARENA_BYTES, ("arena overflow", self.off)
        v = self.t[0:shape[0], a // 2:(a + n * esz) // 2]
        if dt != BF16:
            v = v.bitcast(dt)
        if len(shape) == 3:
            v = v.rearrange("p (a b) -> p a b", a=shape[1])
        elif len(shape) == 4:
            v = v.rearrange("p (a b c) -> p a b c", a=shape[1], b=shape[2])
        return v


def build_nc(S, layers, n_layers_total, dbg=False, stop=None):
    NT = S // 128
    NG = S // 512
    NBLK = S // 1024
    nc = bass.Bass("TRN2", target_bir_lowering=False)

    def din(name, shape, dt=F32):
        return nc.dram_tensor(name, shape, dt, kind="ExternalInput").ap()

    def dscr(name, shape, dt):
        return nc.dram_tensor(name, shape, dt, kind=("ExternalOutput" if dbg else "Internal")).ap()

    x_in = din("x", [S, D])
    lng = [din("ln1_g", [4, 128, D]), din("ln2_g", [4, 128, D])]
    lnb = [din("ln1_b", [4, 128, D]), din("ln2_b", [4, 128, D])]
    ev_w_in = din("ev_w_in", [2, D, 2056])
    ev_negb = din("ev_negb", [2, 8, 1])
    ev_pool_w = din("ev_pool_w", [2, 4, 128, 128])
    ev_pool_scale = din("ev_pool_scale", [2, 4, 128, 1])
    ev_w_out = din("ev_w_out", [2, D, D])
    ev_g = din("ev_ffn_gate", [2, 1, D, DFF])
    ev_u = din("ev_ffn_up", [2, 1, D, DFF])
    ev_d = din("ev_ffn_down", [2, 1, DFF, D])
    od_w_in = din("od_w_in", [2, D, 3072])
    od_w_out = din("od_w_out", [2, D, D])
    od_router = din("od_router", [2, 8, 128, D])
    od_g = din("od_exp_gate", [2, NE, D, DFF])
    od_u = din("od_exp_up", [2, NE, D, DFF])
    od_d = din("od_exp_down", [2, NE, DFF, D])
    c_ident_bf = din("c_ident_bf", [128, 128], BF16)
    c_ident_f = din("c_ident_f", [128, 128])
    c_tri = din("c_tri", [128, 128], BF16)
    c_koh = din("c_koh", [16, S], BF16)
    c_past = din("c_past", [NG, 128, 4, 16])
    c_own = din("c_own", [NG, 128, 4, 16])
    c_invcnt = din("c_invcnt", [4, 128, 16])
    NTILE = 2 * S // 512 + 8
    NSLOT = NTILE * 512
    c_ustrict = din("c_ustrict", [128, 128], BF16)
    c_misc = din("c_misc", [128, 64])
    y_out = nc.dram_tensor("y", [S, D], F32, kind="ExternalOutput").ap()

    XR = [dscr("xr0", [S, D], F32), dscr("xr1", [S, D], F32)]
    XT = dscr("xT", [D, S], BF16)
    QT = dscr("qT", [D, S], BF16)
    KT = dscr("kT", [D, S], BF16)
    VV = dscr("vv", [S, D], BF16)
    CATT = dscr("catT", [D, S], BF16)
    FAUG = dscr("faug", [8, 3, S], BF16)
    NFC = dscr("nfc", [128, NT * 8], F32)
    MB = dscr("mb", [256, S], BF16)
    COMB = dscr("comb", [S, 8], F32)
    ROUTE = dscr("route", [S, 24], F32)
    CNT = dscr("cnt", [128, 8], F32)
    XS = dscr("xs", [NSLOT, D], BF16)
    YS = dscr("ys", [NSLOT, D], F32)

    from contextlib import ExitStack
    with ExitStack() as st:
        arena_t = st.enter_context(nc.sbuf_tensor("arena", [128, ARENA_BYTES // 2], BF16))
        PS = [st.enter_context(nc.psum_tensor("ps%d" % i, [128, 512], F32)) for i in range(7)]
        PSB = st.enter_context(nc.psum_tensor("psb", [128, 1024], BF16))
        NSEM = 60
        sems = [st.enter_context(nc.semaphore("s%d" % i)) for i in range(NSEM)]
        K = Kern(nc, sems)
        A = Arena(arena_t)
        psb = [K.buf("ps%d" % i) for i in range(7)]
        psbb = K.buf("psb")
        PSBv = PSB[:, :].rearrange("p (a b) -> p a b", a=8)

        B_xin = K.buf("x_in")
        B_w = K.buf("weights")
        B_XR = [K.buf("xr0", True), K.buf("xr1", True)]
        B_XT = K.buf("xT", True)
        B_QT = K.buf("qT", True)
        B_KT = K.buf("kT", True)
        B_VV = K.buf("vv", True)
        B_CATT = K.buf("catT", True)
        B_FAUG = K.buf("faug", True)
        B_NFC = K.buf("nfc", True)
        B_MB = K.buf("mb", True)
        B_COMB = K.buf("comb", True)
        B_ROUTE = K.buf("route", True)
        B_CNT = K.buf("cnt", True)
        B_XS = K.buf("xs", "sw")
        B_YS = K.buf("ys", True)
        B_Y = K.buf("y", True)
        K.phase_sems = []
        K.phase_sems_sw = []

        def consts_common():
            ident = A.alloc([128, 128], BF16)
            b = K.buf("ident", True)
            K.dma("sp", ident, c_ident_bf[:, :], R=[B_w], W=[b], dst=b)
            return ident, b

        def emit_xT(xb_ap, xb_buf, tt, ident, identb, stg, stgb):
            for kt in range(8):
                K.tr(PSBv[:, kt, :], xb_ap[:, kt * 128:(kt + 1) * 128], ident, R=[xb_buf, identb], W=[psbb])
            K.op("dve", lambda h: h.tensor_copy(out=stg, in_=PSBv), R=[psbb], W=[stgb])
            K.dma("sp", XT[:, tt * 128:(tt + 1) * 128].rearrange("(kt p) t -> p kt t", p=128), stg,
                  R=[stgb], W=[B_XT], dst=B_XT)

        def layer_norm(z, zb, which, L, small, smallb, g_ap, b_ap, gbb):
            st6 = small[:, 0:12].rearrange("p (a b) -> p a b", a=2)
            K.op("dve", lambda h: h.bn_stats(out=st6[:, 0, :], in_=z[:, 0:512]), R=[zb], W=[smallb])
            K.op("dve", lambda h: h.bn_stats(out=st6[:, 1, :], in_=z[:, 512:1024]), R=[zb], W=[smallb])
            K.op("dve", lambda h: h.bn_aggr(out=small[:, 12:14], in_=small[:, 0:12]), R=[smallb], W=[smallb])
            K.act(small[:, 14:15], small[:, 13:14], AF.Sqrt, R=[smallb], W=[smallb], bias=EPS, scale=1.0)
            K.op("dve", lambda h: h.reciprocal(out=small[:, 15:16], in_=small[:, 14:15]), R=[smallb], W=[smallb])
            K.op("dve", lambda h: h.tensor_scalar(out=z, in0=z, scalar1=small[:, 12:13], scalar2=small[:, 15:16],
                                                  op0=ALU.subtract, op1=ALU.mult), R=[zb, smallb], W=[zb])
            K.op("pool", lambda h: h.tensor_tensor(out=z, in0=z, in1=g_ap, op=ALU.mult), R=[zb, gbb], W=[zb])
            K.op("pool", lambda h: h.tensor_tensor(out=z, in0=z, in1=b_ap, op=ALU.add), R=[zb, gbb], W=[zb])

        def phase_p0():
            A.reset()
            ident, identb = consts_common()
            xt = [A.alloc([128, D], F32) for _ in range(2)]
            xtb = [K.buf("p0x%d" % i, True) for i in range(2)]
            xb = [A.alloc([128, D], BF16) for _ in range(2)]
            xbb = [K.buf("p0xb%d" % i) for i in range(2)]
            stg = [A.alloc([128, 8, 128], BF16) for _ in range(2)]
            stgb = [K.buf("p0s%d" % i) for i in range(2)]
            for tt in range(NT):
                i = tt % 2
                K.dma("sp", xt[i], x_in[tt * 128:(tt + 1) * 128, :], R=[B_xin], W=[xtb[i]], dst=xtb[i])
                K.act(xb[i], xt[i], AF.Copy, R=[xtb[i]], W=[xbb[i]])
                emit_xT(xb[i], xbb[i], tt, ident, identb, stg[i], stgb[i])
            K.end_phase()

        def phase_p1(L):
            even = (L % 2 == 0)
            li = L // 2
            A.reset()
            xTs = A.alloc([128, 8, S], BF16)
            xTsb = [K.buf("xTs%d" % g, True) for g in range(NG)]
            for g in range(NG):
                K.dma("sp", xTs[:, :, g * 512:(g + 1) * 512],
                      XT[:, g * 512:(g + 1) * 512].rearrange("(kt p) t -> p kt t", p=128),
                      R=[B_XT], W=[xTsb[g]], dst=xTsb[g])
            w_in = ev_w_in if even else od_w_in
            nqc = 4 if even else 8
            qoff, koff, voff = (0, 512, 1024) if even else (0, 1024, 2048)
            nvh = 1 if even else 2
            wslot = [A.alloc([128, 8, 512], BF16) for _ in range(2)]
            wslotb = [K.buf("wslot%d" % i, "sw") for i in range(2)]
            stg = [A.alloc([128, 512], BF16) for _ in range(3)]
            stgb = [K.buf("p1stg%d" % i) for i in range(3)]
            cnt = {"w": 0, "s": 0, "p": 0}

            def load_w(col0, ncols=512):
                i = cnt["w"] % 2
                cnt["w"] += 1
                K.dma("pool", wslot[i][:, :, 0:ncols],
                      w_in[li, :, col0:col0 + ncols].rearrange("(kt p) f -> p kt f", p=128),
                      R=[B_w], W=[wslotb[i]], dst=wslotb[i])
                return wslot[i], wslotb[i]

            def next_ps():
                i = cnt["p"] % 6
                cnt["p"] += 1
                return PS[i], psb[i]

            def next_stg():
                i = cnt["s"] % 3
                cnt["s"] += 1
                return stg[i], stgb[i]

            if not even:
                ident, identb = consts_common()
                kmT = A.alloc([128, 16], BF16)
                kmTb = K.buf("kmT")
                kmf = A.alloc([128, 16], F32)
                kmfb = K.buf("kmf")
                K.op("pool", lambda h: h.memset(kmf, 0.0), R=[], W=[kmfb])
                gm = A.alloc([128, 4, 2, 16], F32)
                gmb = K.buf("gm")
                m8 = A.alloc([128, 8, 8], F32)
                m8b = K.buf("m8")
                selb_t = A.alloc([128, 4, 2, 16], F32)
                selbb = K.buf("sel")
                mbt = A.alloc([128, 4, 2, 16], BF16)
                mbtb = K.buf("mbt")
                mbs = A.alloc([32, 512], BF16)
                mbsb = K.buf("mbs")
                cpast = A.alloc([128, NG, 4, 16], F32)
                cown = A.alloc([128, NG, 4, 16], F32)
                cpb = K.buf("cpast", True)
                for g in range(NG):
                    K.dma("sp", cpast[:, g], c_past[g], R=[B_w], W=[cpb], dst=cpb)
                    K.dma("sp", cown[:, g], c_own[g], R=[B_w], W=[cpb], dst=cpb)

            for which in ("k", "q"):
                off = koff if which == "k" else qoff
                dstT, dstB = (KT, B_KT) if which == "k" else (QT, B_QT)
                for cg in range(nqc // 4):
                    wt, wtb = load_w(off + cg * 512)
                    for c4 in range(4):
                        ct = cg * 4 + c4
                        for g in range(NG):
                            ps, pb = next_ps()
                            for kt in range(8):
                                K.mm(ps[:, :], wt[:, kt, c4 * 128:(c4 + 1) * 128], xTs[:, kt, g * 512:(g + 1) * 512],
                                     kt == 0, kt == 7, R=[wtb, xTsb[g]], W=[pb])
                            sg, sgb = next_stg()
                            if which == "k":
                                if even:
                                    K.act(sg, ps[:, :], AF.Copy, R=[pb], W=[sgb])
                                else:
                                    for bb in range(2):
                                        K.op("act", lambda h, sg=sg, ps=ps, bb=bb, g=g: h.activation(
                                            out=sg[:, bb * 256:(bb + 1) * 256], in_=ps[:, bb * 256:(bb + 1) * 256], func=AF.Copy,
                                            accum_out=kmf[:, 2 * g + bb:2 * g + bb + 1]), R=[pb], W=[sgb, kmfb])
                            else:
                                K.act(sg, ps[:, :], AF.Copy, R=[pb], W=[sgb], scale=0.125)
                            K.dma("sp", dstT[ct * 128:(ct + 1) * 128, g * 512:(g + 1) * 512], sg,
                                  R=[sgb], W=[dstB], dst=dstB)
                            if (not even) and which == "q":
                                gp, gpb = next_ps()
                                for t4 in range(4):
                                    K.mm(gp[:, t4 * 32:(t4 + 1) * 32], sg[:, t4 * 128:(t4 + 1) * 128], kmTs[ct][:, :], True, True,
                                         R=[sgb, kmTsb[ct]], W=[gpb])
                                gpv = gp[:, 0:128].rearrange("p (a b c) -> p a b c", a=4, b=2)
                                for hh in range(2):
                                    K.op("dve", lambda h, o=gm[:, :, hh, :], i=gpv[:, :, hh, :], c=cpast[:, g]:
                                         h.tensor_tensor(out=o, in0=i, in1=c, op=ALU.add), R=[gpb, cpb], W=[gmb])
                                for t4 in range(4):
                                    for hh in range(2):
                                        K.op("dve", lambda h, o=m8[:, t4 * 2 + hh, :], i=gm[:, t4, hh, :]: h.max(out=o, in_=i),
                                             R=[gmb], W=[m8b])
                                for t4 in range(4):
                                    for hh in range(2):
                                        K.op("dve", lambda h, o=selb_t[:, t4, hh, :], i=gm[:, t4, hh, :], s=m8[:, t4 * 2 + hh, 2:3]:
                                             h.tensor_scalar(out=o, in0=i, scalar1=s, scalar2=None, op0=ALU.is_ge),
                                             R=[gmb, m8b], W=[selbb])
                                for hh in range(2):
                                    K.op("dve", lambda h, o=selb_t[:, :, hh, :], c=cown[:, g]:
                                         h.tensor_tensor(out=o, in0=o, in1=c, op=ALU.max), R=[selbb, cpb], W=[selbb])
                                K.op("dve", lambda h: h.tensor_scalar(out=mbt, in0=selb_t, scalar1=-NEG, scalar2=NEG,
                                                                      op0=ALU.mult, op1=ALU.add), R=[selbb], W=[mbtb])
                                for t4 in range(4):
                                    K.tr(PSB[0:32, t4 * 128:(t4 + 1) * 128],
                                         mbt[:, t4, :, :].rearrange("p a b -> p (a b)"), ident, R=[mbtb, identb], W=[psbb])
                                K.op("dve", lambda h: h.tensor_copy(out=mbs, in_=PSB[0:32, 0:512]), R=[psbb], W=[mbsb])
                                K.dma("sp", MB[ct * 32:(ct + 1) * 32, g * 512:(g + 1) * 512], mbs, R=[mbsb], W=[B_MB], dst=B_MB)
                        if (not even) and which == "k":
                            if ct == 0:
                                kmTs = [A.alloc([128, 32], BF16) for _ in range(nqc)]
                                kmTsb = [K.buf("kmTs%d" % i) for i in range(nqc)]
                                for i_ in range(nqc):
                                    K.op("pool", lambda h, o=kmTs[i_]: h.memset(o, 0.0), R=[], W=[kmTsb[i_]])
                            for hh in range(2):
                                K.op("dve", lambda h, o=kmTs[ct][hh * 64:(hh + 1) * 64, hh * 16:(hh + 1) * 16], i_=kmf[hh * 64:(hh + 1) * 64, :]:
                                     h.tensor_scalar(out=o, in0=i_, scalar1=1.0 / 256, scalar2=None, op0=ALU.mult),
                                     R=[kmfb], W=[kmTsb[ct]])

            for vh in range(nvh):
                wt, wtb = load_w(voff + vh * 512)
                for tt in range(NT):
                    ps, pb = next_ps()
                    g = tt // 4
                    for kt in range(8):
                        K.mm(ps[:, :], xTs[:, kt, tt * 128:(tt + 1) * 128], wt[:, kt, :], kt == 0, kt == 7,
                             R=[wtb, xTsb[g]], W=[pb])
                    sg, sgb = next_stg()
                    K.act(sg, ps[:, :], AF.Copy, R=[pb], W=[sgb])
                    K.dma("sp", VV[tt * 128:(tt + 1) * 128, vh * 512:(vh + 1) * 512], sg, R=[sgb], W=[B_VV], dst=B_VV)

            if even:
                wt, wtb = load_w(1536, 8)
                negb = A.alloc([8, 1], F32)
                negbb = K.buf("negb", True)
                K.dma("sp", negb, ev_negb[li], R=[B_w], W=[negbb], dst=negbb)
                K.op("dve", lambda h: h.tensor_scalar(out=negb, in0=negb, scalar1=-1.0, scalar2=None, op0=ALU.mult),
                     R=[negbb], W=[negbb])
                f_mark = A.off
                cs = [A.alloc([8, S], F32) for _ in range(2)]
                csb = [K.buf("cs%d" % i) for i in range(2)]
                for g in range(NG):
                    ps, pb = next_ps()
                    for kt in range(8):
                        K.mm(ps[0:8, :], wt[:, kt, 0:8], xTs[:, kt, g * 512:(g + 1) * 512], kt == 0, kt == 7,
                             R=[wtb, xTsb[g]], W=[pb])
                    K.act(cs[1][:, g * 512:(g + 1) * 512], ps[0:8, :], AF.Exp, R=[pb, negbb], W=[csb[1]], bias=negb, scale=-1.0)
                K.act(cs[0], cs[1], AF.Ln, R=[csb[1]], W=[csb[0]], bias=1.0, scale=1.0)
                cur = 0
                sh = 1
                while sh < S:
                    o, i_ = cs[1 - cur], cs[cur]
                    K.op("dve", lambda h, o=o, i_=i_, sh=sh: h.tensor_tensor(out=o[:, sh:S], in0=i_[:, sh:S], in1=i_[:, 0:S - sh], op=ALU.add),
                         R=[csb[cur]], W=[csb[1 - cur]])
                    K.op("dve", lambda h, o=o, i_=i_, sh=sh: h.tensor_copy(out=o[:, 0:sh], in_=i_[:, 0:sh]),
                         R=[csb[cur]], W=[csb[1 - cur]])
                    cur = 1 - cur
                    sh *= 2
                C, Cb = cs[cur], csb[cur]
                r1, r1b = cs[1 - cur], csb[1 - cur]
                hb = [A.alloc([8, S], BF16) for _ in range(3)]
                hbb = [K.buf("hb%d" % i) for i in range(3)]
                h32 = A.alloc([8, S], F32)
                h32b = K.buf("h32")
                K.op("dve", lambda h: h.tensor_scalar(out=hb[0], in0=C, scalar1=-1.0, scalar2=None, op0=ALU.mult), R=[Cb], W=[hbb[0]])
                K.op("dve", lambda h: h.tensor_copy(out=h32, in_=hb[0]), R=[hbb[0]], W=[h32b])
                K.op("dve", lambda h: h.scalar_tensor_tensor(out=r1, in0=C, scalar=-1.0, in1=h32, op0=ALU.mult, op1=ALU.subtract),
                     R=[Cb, h32b], W=[r1b])
                K.op("dve", lambda h: h.tensor_copy(out=hb[1], in_=r1), R=[r1b], W=[hbb[1]])
                K.op("dve", lambda h: h.tensor_copy(out=h32, in_=hb[1]), R=[hbb[1]], W=[h32b])
                K.op("dve", lambda h: h.tensor_tensor(out=r1, in0=r1, in1=h32, op=ALU.subtract), R=[r1b, h32b], W=[r1b])
                K.op("dve", lambda h: h.tensor_copy(out=hb[2], in_=r1), R=[r1b], W=[hbb[2]])
                for j in range(3):
                    K.dma("sp", FAUG[:, j, :], hb[j], R=[hbb[j]], W=[B_FAUG], dst=B_FAUG)
                identf = A.alloc([8, 8], F32)
                identfb = K.buf("identf", True)
                K.dma("sp", identf, c_ident_f[0:8, 0:8], R=[B_w], W=[identfb], dst=identfb)
                ps, pb = next_ps()
                for kt in range(NT):
                    K.tr(ps[:, kt * 8:(kt + 1) * 8], C[:, kt * 128:(kt + 1) * 128], identf, R=[Cb, identfb], W=[pb])
                nfc = A.alloc([128, NT * 8], F32)
                nfcb = K.buf("nfc_s")
                K.op("dve", lambda h, ps=ps: h.tensor_copy(out=nfc, in_=ps[:, 0:NT * 8]), R=[pb], W=[nfcb])
                K.dma("sp", NFC[:, :], nfc, R=[nfcb], W=[B_NFC], dst=B_NFC)
                K.barrier()
                A.off = f_mark

                wt, wtb = load_w(1544)
                pw = A.alloc([128, 4, 128], BF16)
                pwb = K.buf("pw", "sw")
                for g4 in range(4):
                    K.dma("pool", pw[:, g4, :], ev_pool_w[li, g4], R=[B_w], W=[pwb], dst=pwb)
                psc = A.alloc([128, 4], F32)
                pscb = K.buf("psc", True)
                for g4 in range(4):
                    K.dma("sp", psc[:, g4:g4 + 1], ev_pool_scale[li, g4], R=[B_w], W=[pscb], dst=pscb)
                icn = A.alloc([128, 4, 16], F32)
                icnb = K.buf("icn", True)
                for g4 in range(4):
                    K.dma("sp", icn[:, g4, :], c_invcnt[g4], R=[B_w], W=[icnb], dst=icnb)
                uu = A.alloc([128, S], F32)
                uub = K.buf("uu")
                sa = [A.alloc([128, S], F32) for _ in range(2)]
                sab = [K.buf("sa%d" % i) for i in range(2)]
                mx = A.alloc([128, S], BF16)
                mxb = K.buf("mx")
                fix = A.alloc([128, 16], F32)
                fixb = K.buf("fix")
                for g4 in range(4):
                    w = POOL_W[g4]
                    for g in range(NG):
                        ps, pb = next_ps()
                        for kt in range(8):
                            K.mm(ps[:, :], wt[:, kt, g4 * 128:(g4 + 1) * 128], xTs[:, kt, g * 512:(g + 1) * 512],
                                 kt == 0, kt == 7, R=[wtb, xTsb[g]], W=[pb])
                        K.act(uu[:, g * 512:(g + 1) * 512], ps[:, :], AF.Copy, R=[pb], W=[uub])
                    src, srcb = uu, uub
                    sh = 1
                    k = 0
                    while sh < w:
                        o, ob = sa[k % 2], sab[k % 2]
                        K.op("dve", lambda h, o=o, i_=src, sh=sh: h.tensor_tensor(out=o[:, sh:S], in0=i_[:, sh:S], in1=i_[:, 0:S - sh], op=ALU.add),
                             R=[srcb], W=[ob])
                        K.op("pool", lambda h, o=o, i_=src, sh=sh: h.tensor_copy(out=o[:, 0:sh], in_=i_[:, 0:sh]), R=[srcb], W=[ob])
                        src, srcb = o, ob
                        sh *= 2
                        k += 1
                    K.op("dve", lambda h, s_=src, w=w: h.scalar_tensor_tensor(out=mx, in0=s_, scalar=1.0 / w, in1=uu, op0=ALU.mult, op1=ALU.subtract),
                         R=[srcb, uub], W=[mxb])
                    K.op("dve", lambda h, s_=src, w=w, g4=g4: h.tensor_tensor(out=fix[:, 0:w - 1], in0=s_[:, 0:w - 1], in1=icn[:, g4, 0:w - 1], op=ALU.mult),
                         R=[srcb, icnb], W=[fixb])
                    K.op("dve", lambda h, w=w: h.tensor_tensor(out=mx[:, 0:w - 1], in0=fix[:, 0:w - 1], in1=uu[:, 0:w - 1], op=ALU.subtract),
                         R=[fixb, uub, mxb], W=[mxb])
                    for g in range(NG):
                        ps, pb = next_ps()
                        K.mm(ps[:, :], pw[:, g4, :], mx[:, g * 512:(g + 1) * 512], True, True, R=[pwb, mxb], W=[pb])
                        sg, sgb = next_stg()
                        K.op("dve", lambda h, sg=sg, ps=ps, g4=g4: h.tensor_scalar(out=sg, in0=ps[:, :], scalar1=psc[:, g4:g4 + 1], scalar2=None, op0=ALU.mult),
                             R=[pb, pscb], W=[sgb])
                        K.dma("sp", CATT[512 + g4 * 128:512 + (g4 + 1) * 128, g * 512:(g + 1) * 512], sg, R=[sgb], W=[B_CATT], dst=B_CATT)
            K.end_phase()

        def phase_p2(L):
            even = (L % 2 == 0)
            H = 8 if even else 16
            aug = 3 if even else 16
            KA = 64 + aug
            A.reset()
            qa = [A.alloc([128, S], BF16) for _ in range(2)]
            ka = [A.alloc([128, S], BF16) for _ in range(2)]
            va = [A.alloc([128, NT, 128], BF16) for _ in range(2)]
            qab = [K.buf("qa%d" % i, True) for i in range(2)]
            kab = [K.buf("ka%d" % i, True) for i in range(2)]
            vab = [K.buf("va%d" % i, True) for i in range(2)]
            pt = [A.alloc([128, 512], BF16) for _ in range(4)]
            ptb = [K.buf("pt%d" % i) for i in range(4)]
            rr = [A.alloc([128, 512], F32) for _ in range(2)]
            rrb = [K.buf("rr%d" % i) for i in range(2)]
            ot = [A.alloc([64, 512], BF16) for _ in range(2)]
            otb = [K.buf("ot%d" % i) for i in range(2)]
            tri = A.alloc([128, 128], BF16)
            trib = K.buf("tri", True)
            K.dma("sp", tri, c_tri[:, :], R=[B_w], W=[trib], dst=trib)
            if even:
                nfc = A.alloc([128, NT * 8], F32)
                nfcb = K.buf("nfc2", True)
                K.dma("sp", nfc, NFC[:, :], R=[B_NFC], W=[nfcb], dst=nfcb)
            for i in range(2):
                K.op("pool", lambda h, i=i: h.memset(va[i][:, :, 64:128], 1.0), R=[], W=[vab[i]])
                if even:
                    K.op("pool", lambda h, i=i: h.memset(ka[i][64:67, :], 1.0), R=[], W=[kab[i]])
                else:
                    K.dma("sp", ka[i][64:80, :], c_koh[:, :], R=[B_w], W=[kab[i]], dst=kab[i])

            def load_head(hd):
                b = hd % 2
                K.dma("sp", qa[b][0:64, :], QT[hd * 64:(hd + 1) * 64, :], R=[B_QT], W=[qab[b]], dst=qab[b])
                if even:
                    K.dma("sp", qa[b][64:67, :], FAUG[hd], R=[B_FAUG], W=[qab[b]], dst=qab[b])
                else:
                    K.dma("sp", qa[b][64:80, :], MB[hd * 16:(hd + 1) * 16, :], R=[B_MB], W=[qab[b]], dst=qab[b])
                K.dma("sp", ka[b][0:64, :], KT[hd * 64:(hd + 1) * 64, :], R=[B_KT], W=[kab[b]], dst=kab[b])
                for j in range(S // 1024):
                    K.dma("sp", va[b][:, 8 * j:8 * j + 8, 0:64],
                          VV[j * 1024:(j + 1) * 1024, hd * 64:(hd + 1) * 64].rearrange("(kt p) d -> p kt d", p=128),
                          R=[B_VV], W=[vab[b]], dst=vab[b])

            its = []
            for hd in range(H):
                for g in range(NG):
                    for kt in range(4 * g + 4):
                        its.append((hd, g, kt))
            SPS = [(PS[i], psb[i]) for i in range(3)]
            OPS = [(PS[3 + i], psb[3 + i]) for i in range(2)]

            def emit_s(idx):
                hd, g, kt = its[idx]
                b = hd % 2
                j = kt - 4 * g
                c0 = 128 * j if j > 0 else 0
                n = 512 - c0
                sp_, spb = SPS[idx % 3]
                K.mm(sp_[:, 0:n], ka[b][0:KA, kt * 128:(kt + 1) * 128], qa[b][0:KA, g * 512 + c0:(g + 1) * 512],
                     True, True, R=[kab[b], qab[b]], W=[spb])
                p, pb_ = pt[idx % 4], ptb[idx % 4]
                if even:
                    K.act(p[:, 0:n], sp_[:, 0:n], AF.Exp, R=[spb, nfcb], W=[pb_], bias=nfc[:, kt * 8 + hd:kt * 8 + hd + 1], scale=1.0)
                else:
                    K.act(p[:, 0:n], sp_[:, 0:n], AF.Exp, R=[spb], W=[pb_])
                if j >= 0:
                    K.op("pool", lambda h, p=p: h.tensor_tensor(out=p[:, 0:128], in0=p[:, 0:128], in1=tri, op=ALU.mult),
                         R=[pb_, trib], W=[pb_])

            def emit_pv(idx):
                hd, g, kt = its[idx]
                b = hd % 2
                j = kt - 4 * g
                c0 = 128 * j if j > 0 else 0
                n = 512 - c0
                last = 4 * g + 3
                gi = hd * NG + g
                if g == 0 and kt == 0 and hd >= 1 and hd + 1 < H:
                    load_head(hd + 1)
                o, ob = OPS[gi % 2]
                p, pb_ = pt[idx % 4], ptb[idx % 4]
                K.mm(o[:, c0:512], va[b][:, kt, :], p[:, 0:n], kt == 0, kt == last, R=[vab[b], pb_], W=[ob])
                if kt == last:
                    r, rb = rr[gi % 2], rrb[gi % 2]
                    t_, tb = ot[gi % 2], otb[gi % 2]
                    K.op("dve", lambda h: h.reciprocal(out=r[64:128, :], in_=o[64:128, :]), R=[ob], W=[rb])
                    K.op("dve", lambda h: h.tensor_tensor(out=t_, in0=o[0:64, :], in1=r[64:128, :], op=ALU.mult), R=[ob, rb], W=[tb])
                    K.dma("sp", CATT[hd * 64:(hd + 1) * 64, g * 512:(g + 1) * 512], t_, R=[tb], W=[B_CATT], dst=B_CATT)

            n_it = len(its)
            load_head(0)
            if H > 1:
                load_head(1)
            emit_s(0)
            if n_it > 1:
                emit_s(1)
            for idx in range(n_it):
                emit_pv(idx)
                if idx + 2 < n_it:
                    emit_s(idx + 2)
            K.end_phase()

        def phase_p3(L, xr_src, xr_src_b, xr_dst, xr_dst_b):
            even = (L % 2 == 0)
            li = L // 2
            A.reset()
            ident, identb = consts_common()
            wo = A.alloc([128, 8, D], BF16)
            wob = K.buf("wo", "sw")
            w_out = ev_w_out if even else od_w_out
            for hf in range(2):
                K.dma("pool", wo[:, :, hf * 512:(hf + 1) * 512],
                      w_out[li, :, hf * 512:(hf + 1) * 512].rearrange("(kt p) f -> p kt f", p=128), R=[B_w], W=[wob], dst=wob)
            g_ap = A.alloc([128, D], F32)
            b_ap = A.alloc([128, D], F32)
            gbb = K.buf("gb", True)
            K.dma("sp", g_ap, lng[0][L], R=[B_w], W=[gbb], dst=gbb)
            K.dma("sp", b_ap, lnb[0][L], R=[B_w], W=[gbb], dst=gbb)
            ct = [A.alloc([128, 8, 512], BF16) for _ in range(2)]
            ctb = [K.buf("ct%d" % i, True) for i in range(2)]
            xt = [A.alloc([128, D], F32) for _ in range(2)]
            xtb = [K.buf("p3x%d" % i, True) for i in range(2)]
            z = [A.alloc([128, D], F32) for _ in range(2)]
            zb = [K.buf("p3z%d" % i) for i in range(2)]
            xb = [A.alloc([128, D], BF16) for _ in range(2)]
            xbb = [K.buf("p3xb%d" % i) for i in range(2)]
            stg = [A.alloc([128, 8, 128], BF16) for _ in range(2)]
            stgb = [K.buf("p3s%d" % i) for i in range(2)]
            small = [A.alloc([128, 16], F32) for _ in range(2)]
            smallb = [K.buf("p3sm%d" % i) for i in range(2)]
            if not even:
                wr = A.alloc([128, 8, D], F32)
                wrb = K.buf("wr", True)
                for e in range(8):
                    K.dma("sp", wr[:, e, :], od_router[li, e], R=[B_w], W=[wrb], dst=wrb)
                junk = A.alloc([128, D], F32)
                junkb = K.buf("junk")
                rt = [A.alloc([128, 96], F32) for _ in range(2)]
                rtb = [K.buf("rt%d" % i) for i in range(2)]
                for i_ in range(2):
                    K.op("pool", lambda h, i_=i_: h.memset(rt[i_], 0.0), R=[], W=[rtb[i_]])
                ohb = [A.alloc([128, 8], BF16) for _ in range(2)]
                ohbb = [K.buf("ohb%d" % i) for i in range(2)]
                ustr = A.alloc([128, 128], BF16)
                ustrb = K.buf("ustr", True)
                K.dma("sp", ustr, c_ustrict[:, :], R=[B_w], W=[ustrb], dst=ustrb)
                onesb = A.alloc([128, 128], BF16)
                K.op("pool", lambda h: h.memset(onesb, 1.0), R=[], W=[ustrb])
                tot = A.alloc([128, 8], F32)
                totb = K.buf("tot")
                K.op("pool", lambda h: h.memset(tot, 0.0), R=[], W=[totb])

            def load_ct(g):
                K.dma("sp", ct[g % 2], CATT[:, g * 512:(g + 1) * 512].rearrange("(kt p) t -> p kt t", p=128),
                      R=[B_CATT], W=[ctb[g % 2]], dst=ctb[g % 2])

            load_ct(0)
            for tt in range(NT):
                g = tt // 4
                i = tt % 2
                if tt % 4 == 0 and g + 1 < NG:
                    load_ct(g + 1)
                K.dma("sp", xt[i], xr_src[tt * 128:(tt + 1) * 128, :], R=[xr_src_b], W=[xtb[i]], dst=xtb[i])
                c = ct[g % 2]
                t4 = tt % 4
                for hf in range(2):
                    ps, pb = PS[(tt * 2 + hf) % 4], psb[(tt * 2 + hf) % 4]
                    for kt in range(8):
                        K.mm(ps[:, :], c[:, kt, t4 * 128:(t4 + 1) * 128], wo[:, kt, hf * 512:(hf + 1) * 512], kt == 0, kt == 7,
                             R=[ctb[g % 2], wob], W=[pb])
                    K.op("dve", lambda h, ps=ps, hf=hf, i=i: h.scalar_tensor_tensor(
                        out=z[i][:, hf * 512:(hf + 1) * 512], in0=xt[i][:, hf * 512:(hf + 1) * 512], scalar=ALPHA,
                        in1=ps[:, :], op0=ALU.mult, op1=ALU.add), R=[pb, xtb[i]], W=[zb[i]])
                layer_norm(z[i], zb[i], 0, L, small[i], smallb[i], g_ap, b_ap, gbb)
                K.dma("sp", xr_dst[tt * 128:(tt + 1) * 128, :], z[i], R=[zb[i]], W=[xr_dst_b], dst=xr_dst_b)
                K.act(xb[i], z[i], AF.Copy, R=[zb[i]], W=[xbb[i]])
                emit_xT(xb[i], xbb[i], tt, ident, identb, stg[i], stgb[i])
                if not even:
                    r = rt[i]
                    rb = rtb[i]
                    lg, m8_, dd, g1, g2, c1, c2 = r[:, 0:8], r[:, 8:16], r[:, 16:17], r[:, 66:67], r[:, 67:68], r[:, 24:32], r[:, 32:40]
                    oh0, oh1, rank, j8 = r[:, 48:56], r[:, 56:64], r[:, 72:80], r[:, 80:88]
                    for e in range(8):
                        K.op("dve", lambda h, e=e, i=i, lg=lg: h.scalar_tensor_tensor(
                            out=junk, in0=z[i], scalar=1.0, in1=wr[:, e, :], op0=ALU.mult, op1=ALU.mult, accum_out=lg[:, e:e + 1]),
                            R=[zb[i], wrb], W=[junkb, rb])
                    K.op("dve", lambda h, m8_=m8_, lg=lg: h.max(out=m8_, in_=lg), R=[rb], W=[rb])
                    K.op("dve", lambda h, dd=dd, m8_=m8_: h.tensor_tensor(out=dd, in0=m8_[:, 1:2], in1=m8_[:, 0:1], op=ALU.subtract), R=[rb], W=[rb])
                    K.act(dd, dd, AF.Exp, R=[rb], W=[rb])
                    K.op("dve", lambda h, g1=g1, dd=dd: h.tensor_scalar(out=g1, in0=dd, scalar1=1.0, scalar2=None, op0=ALU.add), R=[rb], W=[rb])
                    K.op("dve", lambda h, g1=g1: h.reciprocal(out=g1, in_=g1), R=[rb], W=[rb])
                    K.op("dve", lambda h, g1=g1, g2=g2, dd=dd: h.tensor_tensor(out=g2, in0=dd, in1=g1, op=ALU.mult), R=[rb], W=[rb])
                    K.op("dve", lambda h, c1=c1, lg=lg, m8_=m8_, g1=g1: h.tensor_scalar(out=c1, in0=lg, scalar1=m8_[:, 0:1], scalar2=g1, op0=ALU.is_equal, op1=ALU.mult), R=[rb], W=[rb])
                    K.op("dve", lambda h, c2=c2, lg=lg, m8_=m8_, g2=g2: h.tensor_scalar(out=c2, in0=lg, scalar1=m8_[:, 1:2], scalar2=g2, op0=ALU.is_equal, op1=ALU.mult), R=[rb], W=[rb])
                    K.op("dve", lambda h, c1=c1, c2=c2: h.tensor_tensor(out=c1, in0=c1, in1=c2, op=ALU.add), R=[rb], W=[rb])
                    K.dma("sp", COMB[tt * 128:(tt + 1) * 128, :], c1, R=[rb], W=[B_COMB], dst=B_COMB)
                    K.op("dve", lambda h, oh0=oh0, lg=lg, m8_=m8_: h.tensor_scalar(out=oh0, in0=lg, scalar1=m8_[:, 0:1], scalar2=None, op0=ALU.is_equal), R=[rb], W=[rb])
                    K.op("dve", lambda h, oh1=oh1, lg=lg, m8_=m8_: h.tensor_scalar(out=oh1, in0=lg, scalar1=m8_[:, 1:2], scalar2=None, op0=ALU.is_equal), R=[rb], W=[rb])
                    K.op("dve", lambda h, i=i, oh0=oh0, oh1=oh1: h.tensor_tensor(out=ohb[i], in0=oh0, in1=oh1, op=ALU.add), R=[rb], W=[ohbb[i]])
                    pr, prb = PS[4 + i], psb[4 + i]
                    K.mm(pr[:, 0:8], ustr, ohb[i], True, True, R=[ustrb, ohbb[i]], W=[prb])
                    K.mm(pr[:, 8:16], onesb, ohb[i], True, True, R=[ustrb, ohbb[i]], W=[prb])
                    K.op("dve", lambda h, rank=rank, pr=pr: h.tensor_tensor(out=rank, in0=pr[:, 0:8], in1=tot, op=ALU.add), R=[prb, totb], W=[rb])
                    K.op("dve", lambda h, pr=pr: h.tensor_tensor(out=tot, in0=pr[:, 8:16], in1=tot, op=ALU.add), R=[prb, totb], W=[totb])
                    K.op("dve", lambda h, oh0=oh0, rank=rank, j8=j8, o=r[:, 64:65]: h.scalar_tensor_tensor(
                        out=j8, in0=oh0, scalar=1.0, in1=rank, op0=ALU.mult, op1=ALU.mult, accum_out=o), R=[rb], W=[rb])
                    K.op("dve", lambda h, oh1=oh1, rank=rank, j8=j8, o=r[:, 65:66]: h.scalar_tensor_tensor(
                        out=j8, in0=oh1, scalar=1.0, in1=rank, op0=ALU.mult, op1=ALU.mult, accum_out=o), R=[rb], W=[rb])
                    K.dma("sp", ROUTE[tt * 128:(tt + 1) * 128, :], r[:, 48:72], R=[rb], W=[B_ROUTE], dst=B_ROUTE)
            if not even:
                K.dma("sp", CNT[:, :], tot, R=[totb], W=[B_CNT], dst=B_CNT)
            K.end_phase()

        def phase_p4(L, xr_src, xr_src_b, xr_dst, xr_dst_b, last):
            even = (L % 2 == 0)
            li = L // 2
            A.reset()
            ident, identb = consts_common()
            Wg, Wu, Wd = (ev_g, ev_u, ev_d) if even else (od_g, od_u, od_d)
            experts = [0] if even else list(range(NE))
            g_ap = A.alloc([128, D], F32)
            b_ap = A.alloc([128, D], F32)
            gbb = K.buf("gb4", True)
            K.dma("sp", g_ap, lng[1][L], R=[B_w], W=[gbb], dst=gbb)
            K.dma("sp", b_ap, lnb[1][L], R=[B_w], W=[gbb], dst=gbb)
            xTb = A.alloc([128, 8, 1024], BF16)
            xTbb = K.buf("xTb", True)
            yacc = A.alloc([128, 8, D], F32)
            yab = [K.buf("yacc%d" % i, True) for i in range(8)]
            hT = A.alloc([128, 14, 1024], BF16)
            hTb = [K.buf("hT%d" % i) for i in range(14)]
            wgu = [A.alloc([128, 2, 8, 256], BF16) for _ in range(2)]
            wgub = [K.buf("wgu%d" % i, "sw") for i in range(2)]
            wd = [A.alloc([128, 14, D], BF16) for _ in range(2)]
            wdb = [K.buf("wd%d" % i, "sw") for i in range(2)]
            sg = [A.alloc([128, 512], F32) for _ in range(2)]
            sgb = [K.buf("sg%d" % i) for i in range(2)]
            xb = [A.alloc([128, D], BF16) for _ in range(2)]
            xbb = [K.buf("p4xb%d" % i) for i in range(2)]
            stg = [A.alloc([128, 8, 128], BF16) for _ in range(2)]
            stgb = [K.buf("p4s%d" % i) for i in range(2)]
            small = [A.alloc([128, 16], F32) for _ in range(2)]
            smallb = [K.buf("p4sm%d" % i) for i in range(2)]
            comb = A.alloc([128, 8, 8], F32)
            combb = K.buf("comb_s", True)

            gl = []
            dl = []
            for blk in range(NBLK):
                for e in experts:
                    for hf in range(2):
                        dl.append((blk, e, hf))
                        for fg in range(7):
                            gl.append((blk, e, hf, fg))
            st_ = {"g": 0, "d": 0}

            def load_g(n):
                if n >= len(gl):
                    return
                blk, e, hf, fg = gl[n]
                f0 = hf * 1792 + fg * 256
                i = n % 2
                K.dma("pool", wgu[i][:, 0], Wg[li, e, :, f0:f0 + 256].rearrange("(kt p) f -> p kt f", p=128), R=[B_w], W=[wgub[i]], dst=wgub[i])
                K.dma("pool", wgu[i][:, 1], Wu[li, e, :, f0:f0 + 256].rearrange("(kt p) f -> p kt f", p=128), R=[B_w], W=[wgub[i]], dst=wgub[i])

            def load_d(n):
                if n >= len(dl):
                    return
                blk, e, hf = dl[n]
                i = n % 2
                K.dma("pool", wd[i], Wd[li, e, hf * 1792:(hf + 1) * 1792, :].rearrange("(fc p) d -> p fc d", p=128), R=[B_w], W=[wdb[i]], dst=wdb[i])

            load_g(0)
            load_d(0)
            gi = 0
            di = 0
            pcount = 0
            ycount = 0
            for blk in range(NBLK):
                t0 = blk * 1024
                K.dma("sp", xTb, XT[:, t0:t0 + 1024].rearrange("(kt p) t -> p kt t", p=128), R=[B_XT], W=[xTbb], dst=xTbb)
                for tt in range(8):
                    K.dma("sp", yacc[:, tt, :], xr_src[t0 + tt * 128:t0 + (tt + 1) * 128, :], R=[xr_src_b], W=[yab[tt]], dst=yab[tt])
                    K.op("pool", lambda h, tt=tt: h.tensor_scalar(out=yacc[:, tt, :], in0=yacc[:, tt, :], scalar1=ALPHA, scalar2=0.0, op0=ALU.mult, op1=ALU.add),
                         R=[yab[tt]], W=[yab[tt]])
                if not even:
                    K.dma("sp", comb, COMB[t0:t0 + 1024, :].rearrange("(tt p) e -> p tt e", p=128), R=[B_COMB], W=[combb], dst=combb)
                for e in experts:
                    for hf in range(2):
                        load_d(di + 1)
                        wdt, wdtb = wd[di % 2], wdb[di % 2]
                        di += 1
                        for fg in range(7):
                            load_g(gi + 1)
                            w_, w_b = wgu[gi % 2], wgub[gi % 2]
                            gi += 1
                            for fc2 in range(2):
                                fcl = fg * 2 + fc2
                                for tg in range(2):
                                    pg, pgb = PS[(pcount % 2) * 2], psb[(pcount % 2) * 2]
                                    pu, pub = PS[(pcount % 2) * 2 + 1], psb[(pcount % 2) * 2 + 1]
                                    s_, s_b = sg[pcount % 2], sgb[pcount % 2]
                                    pcount += 1
                                    for kt in range(8):
                                        K.mm(pg[:, :], w_[:, 0, kt, fc2 * 128:(fc2 + 1) * 128], xTb[:, kt, tg * 512:(tg + 1) * 512],
                                             kt == 0, kt == 7, R=[w_b, xTbb], W=[pgb])
                                    for kt in range(8):
                                        K.mm(pu[:, :], w_[:, 1, kt, fc2 * 128:(fc2 + 1) * 128], xTb[:, kt, tg * 512:(tg + 1) * 512],
                                             kt == 0, kt == 7, R=[w_b, xTbb], W=[pub])
                                    K.act(s_, pg[:, :], AF.Silu, R=[pgb], W=[s_b])
                                    K.op("dve", lambda h, fcl=fcl, tg=tg, s_=s_, pu=pu: h.tensor_tensor(
                                        out=hT[:, fcl, tg * 512:(tg + 1) * 512], in0=pu[:, :], in1=s_, op=ALU.mult),
                                        R=[pub, s_b], W=[hTb[fcl]])
                        for tt in range(8):
                            for dh in range(2):
                                py, pyb = PS[4 + ycount % 2], psb[4 + ycount % 2]
                                ycount += 1
                                for fcl in range(14):
                                    K.mm(py[:, :], hT[:, fcl, tt * 128:(tt + 1) * 128], wdt[:, fcl, dh * 512:(dh + 1) * 512],
                                         fcl == 0, fcl == 13, R=[hTb[fcl], wdtb], W=[pyb])
                                sc = 1.0 if even else comb[:, tt, e:e + 1]
                                rds = [pyb, yab[tt]] + ([] if even else [combb])
                                K.op("dve", lambda h, py=py, tt=tt, dh=dh, sc=sc: h.scalar_tensor_tensor(
                                    out=yacc[:, tt, dh * 512:(dh + 1) * 512], in0=py[:, :], scalar=sc,
                                    in1=yacc[:, tt, dh * 512:(dh + 1) * 512], op0=ALU.mult, op1=ALU.add), R=rds, W=[yab[tt]])
                for tt in range(8):
                    i = tt % 2
                    zt = yacc[:, tt, :]
                    layer_norm(zt, yab[tt], 1, L, small[i], smallb[i], g_ap, b_ap, gbb)
                    gt = blk * 8 + tt
                    if last:
                        K.dma("sp", y_out[gt * 128:(gt + 1) * 128, :], zt, R=[yab[tt]], W=[B_Y], dst=B_Y)
                    else:
                        K.dma("sp", xr_dst[gt * 128:(gt + 1) * 128, :], zt, R=[yab[tt]], W=[xr_dst_b], dst=xr_dst_b)
                        K.act(xb[i], zt, AF.Copy, R=[yab[tt]], W=[xbb[i]])
                        emit_xT(xb[i], xbb[i], gt, ident, identb, stg[i], stgb[i])
            K.end_phase()

        def phase_p4s(L, xr_src, xr_src_b, xr_dst, xr_dst_b, last):
            li = L // 2
            A.reset()
            ident, identb = consts_common()
            g_ap = A.alloc([128, D], F32)
            b_ap = A.alloc([128, D], F32)
            gbb = K.buf("gb4", True)
            K.dma("sp", g_ap, lng[1][L], R=[B_w], W=[gbb], dst=gbb)
            K.dma("sp", b_ap, lnb[1][L], R=[B_w], W=[gbb], dst=gbb)
            misc = A.alloc([128, 64], F32)
            miscb = K.buf("misc", True)
            K.dma("sp", misc, c_misc[:, :], R=[B_w], W=[miscb], dst=miscb)
            cnt = A.alloc([128, 8], F32)
            cntb = K.buf("cnt_s", True)
            K.dma("sp", cnt, CNT[:, :], R=[B_CNT], W=[cntb], dst=cntb)
            tb_ = A.alloc([128, 64], F32)
            tbb = K.buf("tb")
            nt, tbase, tend, sbase, tmp8 = tb_[:, 0:8], tb_[:, 8:16], tb_[:, 16:24], tb_[:, 24:32], tb_[:, 32:40]
            K.op("dve", lambda h: h.tensor_scalar(out=nt, in0=cnt, scalar1=0.0, scalar2=None, op0=ALU.is_gt), R=[cntb], W=[tbb])
            for m in range(1, 8):
                K.op("dve", lambda h, m=m: h.scalar_tensor_tensor(out=nt, in0=cnt, scalar=512.0 * m, in1=nt, op0=ALU.is_gt, op1=ALU.add),
                     R=[cntb, tbb], W=[tbb])
            K.op("dve", lambda h: h.memset(tbase[:, 0:1], 0.0), R=[], W=[tbb])
            for e in range(1, 8):
                K.op("dve", lambda h, e=e: h.tensor_tensor(out=tbase[:, e:e + 1], in0=tbase[:, e - 1:e], in1=nt[:, e - 1:e], op=ALU.add), R=[tbb], W=[tbb])
            K.op("dve", lambda h: h.tensor_tensor(out=tend, in0=tbase, in1=nt, op=ALU.add), R=[tbb], W=[tbb])
            K.op("dve", lambda h: h.tensor_scalar(out=sbase, in0=tbase, scalar1=512.0, scalar2=None, op0=ALU.mult), R=[tbb], W=[tbb])
            te = A.alloc([128, NTILE], F32)
            teb = K.buf("te")
            jrow = misc[:, 0:NTILE]
            K.op("dve", lambda h: h.tensor_scalar(out=te, in0=jrow, scalar1=tend[:, 0:1], scalar2=None, op0=ALU.is_ge), R=[miscb, tbb], W=[teb])
            for e in range(1, 8):
                K.op("dve", lambda h, e=e: h.scalar_tensor_tensor(out=te, in0=jrow, scalar=tend[:, e:e + 1], in1=te, op0=ALU.is_ge, op1=ALU.add),
                     R=[miscb, tbb, teb], W=[teb])
            K.op("dve", lambda h: h.tensor_scalar(out=te, in0=te, scalar1=7.0, scalar2=None, op0=ALU.min), R=[teb], W=[teb])
            idxWf = A.alloc([128, NTILE, 8], F32)
            idxW = A.alloc([128, NTILE, 8], I32)
            idxDf = A.alloc([128, NTILE, 14], F32)
            idxD = A.alloc([128, NTILE, 14], I32)
            idxb = K.buf("idx")
            te2 = A.alloc([128, NTILE], F32)
            K.op("dve", lambda h: h.tensor_scalar(out=te2, in0=te, scalar1=1024.0, scalar2=float(li * 8192), op0=ALU.mult, op1=ALU.add), R=[teb], W=[idxb])
            for j in range(NTILE):
                K.op("dve", lambda h, j=j: h.tensor_scalar(out=idxWf[:, j, :], in0=misc[:, 53:61], scalar1=te2[:, j:j + 1], scalar2=None, op0=ALU.add),
                     R=[miscb, idxb], W=[idxb])
            K.op("dve", lambda h: h.tensor_scalar(out=idxWf, in0=idxWf, scalar1=2.0, scalar2=None, op0=ALU.mult), R=[idxb], W=[idxb])
            K.op("dve", lambda h: h.tensor_copy(out=idxW, in_=idxWf), R=[idxb], W=[idxb])
            K.op("dve", lambda h: h.tensor_scalar(out=te2, in0=te, scalar1=1792.0, scalar2=float(li * 14336), op0=ALU.mult, op1=ALU.add), R=[teb, idxb], W=[idxb])
            for j in range(NTILE):
                K.op("dve", lambda h, j=j: h.tensor_scalar(out=idxDf[:, j, :], in0=misc[:, 24:38], scalar1=te2[:, j:j + 1], scalar2=None, op0=ALU.add),
                     R=[miscb, idxb], W=[idxb])
            K.op("dve", lambda h: h.tensor_copy(out=idxD, in_=idxDf), R=[idxb], W=[idxb])

            xr = [A.alloc([128, 4, D], BF16) for _ in range(1)]
            xrb = [K.buf("xr%d" % i, True) for i in range(1)]
            K.op("pool", lambda h: h.memset(xr[0], 0.0), R=[], W=[xrb[0]])
            for j in range(NTILE):
                K.dma("pool", XS[j * 512:(j + 1) * 512, :].rearrange("(a p) d -> p a d", p=128), xr[0], R=[xrb[0]], W=[B_XS], dst=B_XS)
            slotf = A.alloc([128, NT, 2], F32)
            sloti = A.alloc([128, NT, 2], I32)
            gts = A.alloc([128, NT, 2], F32)
            slb = K.buf("slots")
            pre_mark = A.off
            rte = [A.alloc([128, 24], F32) for _ in range(2)]
            rteb = [K.buf("rte%d" % i, True) for i in range(2)]
            xt = [A.alloc([128, D], F32) for _ in range(2)]
            xtb = [K.buf("p4x%d" % i, True) for i in range(2)]
            xb = [A.alloc([128, D], BF16) for _ in range(2)]
            xbb = [K.buf("p4xb%d" % i) for i in range(2)]
            j8 = A.alloc([128, 8], F32)
            j8b = K.buf("j8")
            for tt in range(NT):
                i = tt % 2
                K.dma("sp", rte[i], ROUTE[tt * 128:(tt + 1) * 128, :], R=[B_ROUTE], W=[rteb[i]], dst=rteb[i])
                K.dma("sp", xt[i], xr_src[tt * 128:(tt + 1) * 128, :], R=[xr_src_b], W=[xtb[i]], dst=xtb[i])
                K.act(xb[i], xt[i], AF.Copy, R=[xtb[i]], W=[xbb[i]])
                for k2 in range(2):
                    K.op("dve", lambda h, i=i, k2=k2, tt=tt: h.scalar_tensor_tensor(
                        out=j8, in0=rte[i][:, k2 * 8:(k2 + 1) * 8], scalar=1.0, in1=sbase, op0=ALU.mult, op1=ALU.mult,
                        accum_out=slotf[:, tt, k2:k2 + 1]), R=[rteb[i], tbb], W=[j8b, slb])
                K.op("dve", lambda h, i=i, tt=tt: h.tensor_tensor(out=slotf[:, tt, :], in0=slotf[:, tt, :], in1=rte[i][:, 16:18], op=ALU.add),
                     R=[rteb[i], slb], W=[slb])
                K.op("dve", lambda h, tt=tt: h.tensor_copy(out=sloti[:, tt, :], in_=slotf[:, tt, :]), R=[slb], W=[slb])
                K.op("dve", lambda h, i=i, tt=tt: h.tensor_copy(out=gts[:, tt, :], in_=rte[i][:, 18:20]), R=[rteb[i]], W=[slb])
                for k2 in range(2):
                    K.idma("pool", lambda h, i=i, tt=tt, k2=k2: h.indirect_dma_start(
                        out=XS[:, :], out_offset=bass.IndirectOffsetOnAxis(sloti[:, tt, k2:k2 + 1], 0), in_=xb[i], in_offset=None),
                        R=[xbb[i], slb], W=[B_XS], dst=B_XS)

            K.barrier()
            A.off = pre_mark
            main_mark = A.off
            xTb = A.alloc([128, 8, 512], BF16)
            xTbb = K.buf("xTb")
            hT = A.alloc([128, 14, 512], BF16)
            hTb = [K.buf("hT%d" % i) for i in range(14)]
            wgu = [A.alloc([128, 8, 1792], BF16) for _ in range(4)]
            wgub = [K.buf("wgu%d" % i, "sw") for i in range(4)]
            wd = [A.alloc([128, 14, D], BF16) for _ in range(1)]
            wdb = [K.buf("wd%d" % i, "sw") for i in range(1)]
            sg = [A.alloc([128, 512], F32) for _ in range(2)]
            sgb = [K.buf("sg%d" % i) for i in range(2)]
            ysb = [A.alloc([128, 4, D], F32) for _ in range(1)]
            ysbb = [K.buf("ysb%d" % i) for i in range(1)]
            Wg_v = od_g.rearrange("l e k (q f) -> (l e k q) f", q=2)
            Wu_v = od_u.rearrange("l e k (q f) -> (l e k q) f", q=2)
            Wd_v = od_d.rearrange("l e (f2 t) d -> (l e f2) (t d)", t=2)
            gl = [(j, hf, gu) for j in range(NTILE) for hf in range(2) for gu in range(2)]
            dl = [(j, hf) for j in range(NTILE) for hf in range(2)]
            issued = {"g": 0, "d": 0}

            def ensure_g(n):
                while issued["g"] <= n and issued["g"] < len(gl):
                    m = issued["g"]
                    j, hf, gu = gl[m]
                    i = m % 4
                    Wv = Wg_v if gu == 0 else Wu_v
                    for kt in range(8):
                        K.idma("pool", lambda h, i=i, Wv=Wv, hf=hf, j=j, kt=kt: h.indirect_dma_start(
                            out=wgu[i][:, kt, :], out_offset=None, in_=Wv[:, :],
                            in_offset=bass.IndirectOffsetOnAxis(idxW[:, j, kt:kt + 1], 0), element_offset=hf * 1792),
                            R=[B_w, idxb], W=[wgub[i]], dst=wgub[i])
                    issued["g"] += 1

            def ensure_d(n):
                while issued["d"] <= n and issued["d"] < len(dl):
                    m = issued["d"]
                    j, hf = dl[m]
                    for r2 in range(7):
                        K.idma("pool", lambda h, r2=r2, j=j, hf=hf: h.indirect_dma_start(
                            out=wd[0][:, 2 * r2:2 * r2 + 2, :].rearrange("p a d -> p (a d)"), out_offset=None, in_=Wd_v[:, :],
                            in_offset=bass.IndirectOffsetOnAxis(idxD[:, j, hf * 7 + r2:hf * 7 + r2 + 1], 0)),
                            R=[B_w, idxb], W=[wdb[0]], dst=wdb[0])
                    issued["d"] += 1

            pcount = 0
            ycount = 0
            for j in range(NTILE):
                xi = 0
                K.dma("sp", xr[xi], XS[j * 512:(j + 1) * 512, :].rearrange("(a p) d -> p a d", p=128), R=[B_XS], W=[xrb[xi]], dst=xrb[xi])
                for a in range(4):
                    xv = xr[xi][:, a, :].rearrange("p (q k) -> p k q", k=8)
                    for kt in range(8):
                        K.tr(PSBv[:, kt, :], xv[:, kt, :], ident, R=[xrb[xi], identb], W=[psbb])
                    K.op("dve", lambda h, a=a: h.tensor_copy(out=xTb[:, :, a * 128:(a + 1) * 128], in_=PSBv), R=[psbb], W=[xTbb])
                yb_, ybb_ = ysb[0], ysbb[0]
                for hf in range(2):
                    n0 = (j * 2 + hf) * 2
                    ensure_g(n0 + 1)
                    ensure_d(j * 2 + hf)
                    ensure_g(n0 + 3)
                    wg_, wg_b = wgu[n0 % 4], wgub[n0 % 4]
                    wu_, wu_b = wgu[(n0 + 1) % 4], wgub[(n0 + 1) % 4]
                    wdt, wdtb = wd[0], wdb[0]
                    for c in range(14):
                        pg, pgb = PS[(pcount % 2) * 2], psb[(pcount % 2) * 2]
                        pu, pub = PS[(pcount % 2) * 2 + 1], psb[(pcount % 2) * 2 + 1]
                        s_, s_b = sg[pcount % 2], sgb[pcount % 2]
                        pcount += 1
                        for kt in range(8):
                            K.mm(pg[:, :], wg_[:, kt, :].rearrange("p (m c) -> p c m", c=14)[:, c, :], xTb[:, kt, :], kt == 0, kt == 7,
                                 R=[wg_b, xTbb], W=[pgb])
                        for kt in range(8):
                            K.mm(pu[:, :], wu_[:, kt, :].rearrange("p (m c) -> p c m", c=14)[:, c, :], xTb[:, kt, :], kt == 0, kt == 7,
                                 R=[wu_b, xTbb], W=[pub])
                        K.act(s_, pg[:, :], AF.Silu, R=[pgb], W=[s_b])
                        K.op("dve", lambda h, c=c, s_=s_, pu=pu: h.tensor_tensor(out=hT[:, c, :], in0=pu[:, :], in1=s_, op=ALU.mult),
                             R=[pub, s_b], W=[hTb[c]])
                    ensure_g(n0 + 3)
                    for a in range(4):
                        for dh in range(2):
                            py, pyb = PS[4 + ycount % 2], psb[4 + ycount % 2]
                            ycount += 1
                            for c in range(14):
                                K.mm(py[:, :], hT[:, c, a * 128:(a + 1) * 128], wdt[:, c, dh * 512:(dh + 1) * 512],
                                     c == 0, c == 13, R=[hTb[c], wdtb], W=[pyb])
                            if hf == 0:
                                K.op("dve", lambda h, py=py, a=a, dh=dh, yb_=yb_: h.tensor_copy(out=yb_[:, a, dh * 512:(dh + 1) * 512], in_=py[:, :]),
                                     R=[pyb], W=[ybb_])
                            else:
                                K.op("dve", lambda h, py=py, a=a, dh=dh, yb_=yb_: h.tensor_tensor(
                                    out=yb_[:, a, dh * 512:(dh + 1) * 512], in0=py[:, :], in1=yb_[:, a, dh * 512:(dh + 1) * 512], op=ALU.add),
                                    R=[pyb, ybb_], W=[ybb_])
                K.dma("sp", YS[j * 512:(j + 1) * 512, :].rearrange("(a p) d -> p a d", p=128), yb_, R=[ybb_], W=[B_YS], dst=B_YS)


            K.barrier()
            A.off = main_mark
            ya = [[A.alloc([128, D], F32) for _ in range(2)] for _ in range(2)]
            yab_ = [[K.buf("ya%d%d" % (i, k2), "sw") for k2 in range(2)] for i in range(2)]
            stg = [A.alloc([128, 8, 128], BF16) for _ in range(2)]
            stgb = [K.buf("p4s%d" % i) for i in range(2)]
            small = [A.alloc([128, 16], F32) for _ in range(2)]
            smallb = [K.buf("p4sm%d" % i) for i in range(2)]
            zt_ = [A.alloc([128, D], F32) for _ in range(2)]
            ztb_ = [K.buf("p4y%d" % i, True) for i in range(2)]
            zb_ = [A.alloc([128, D], BF16) for _ in range(2)]
            zbb_ = [K.buf("p4yb%d" % i) for i in range(2)]
            for tt in range(NT):
                i = tt % 2
                K.dma("sp", zt_[i], xr_src[tt * 128:(tt + 1) * 128, :], R=[xr_src_b], W=[ztb_[i]], dst=ztb_[i])
                for k2 in range(2):
                    K.idma("pool", lambda h, i=i, tt=tt, k2=k2: h.indirect_dma_start(
                        out=ya[i][k2], out_offset=None, in_=YS[:, :], in_offset=bass.IndirectOffsetOnAxis(sloti[:, tt, k2:k2 + 1], 0)),
                        R=[B_YS, slb], W=[yab_[i][k2]], dst=yab_[i][k2])
                zt = zt_[i]
                K.op("dve", lambda h, i=i, tt=tt: h.tensor_scalar(out=ya[i][0], in0=ya[i][0], scalar1=gts[:, tt, 0:1], scalar2=None, op0=ALU.mult),
                     R=[yab_[i][0], slb], W=[yab_[i][0]])
                K.op("dve", lambda h, i=i, tt=tt: h.scalar_tensor_tensor(out=ya[i][0], in0=ya[i][1], scalar=gts[:, tt, 1:2], in1=ya[i][0], op0=ALU.mult, op1=ALU.add),
                     R=[yab_[i][0], yab_[i][1], slb], W=[yab_[i][0]])
                K.op("dve", lambda h, zt=zt, i=i: h.scalar_tensor_tensor(out=zt, in0=zt, scalar=ALPHA, in1=ya[i][0], op0=ALU.mult, op1=ALU.add),
                     R=[ztb_[i], yab_[i][0]], W=[ztb_[i]])
                layer_norm(zt, ztb_[i], 1, L, small[i], smallb[i], g_ap, b_ap, gbb)
                if last:
                    K.dma("sp", y_out[tt * 128:(tt + 1) * 128, :], zt, R=[ztb_[i]], W=[B_Y], dst=B_Y)
                else:
                    K.dma("sp", xr_dst[tt * 128:(tt + 1) * 128, :], zt, R=[ztb_[i]], W=[xr_dst_b], dst=xr_dst_b)
                    K.act(zb_[i], zt, AF.Copy, R=[ztb_[i]], W=[zbb_[i]])
                    emit_xT(zb_[i], zbb_[i], tt, ident, identb, stg[i], stgb[i])
            K.end_phase()

        phase_p0()
        cur, curb = x_in, B_xin
        for n, L in enumerate(layers):
            last = (n == len(layers) - 1)
            phase_p1(L)
            if last and stop == "p1":
                break
            phase_p2(L)
            if last and stop == "p2":
                break
            phase_p3(L, cur, curb, XR[0], B_XR[0])
            if last and stop == "p3":
                break
            if SPARSE and L % 2 == 1:
                phase_p4s(L, XR[0], B_XR[0], XR[1], B_XR[1], last)
            else:
                phase_p4(L, XR[0], B_XR[0], XR[1], B_XR[1], last)
            cur, curb = XR[1], B_XR[1]
        sp = K.E["sp"]
        sp.prog.append(([(B_Y.sem, K.semcnt[B_Y.sem])], None, None, 0))

        with nc.Block() as block:
            @block.tensor
            def _(h):
                K.replay("pe", h)

            @block.scalar
            def _(h):
                K.replay("act", h)

            @block.vector
            def _(h):
                K.replay("dve", h)

            @block.gpsimd
            def _(h):
                K.replay("pool", h)

            @block.sync
            def _(h):
                K.replay("sp", h)
    return nc


def make_consts(S):
    NG = S // 512
    bf = ml_dtypes.bfloat16
    c = {}
    c["c_ident_bf"] = np.eye(128, dtype=np.float32).astype(bf)
    c["c_ident_f"] = np.eye(128, dtype=np.float32)
    c["c_tri"] = np.triu(np.ones((128, 128), np.float32)).astype(bf)
    koh = np.zeros((16, S), np.float32)
    for n in range(min(16, S // 256)):
        koh[n, n * 256:(n + 1) * 256] = 1.0
    c["c_koh"] = koh.astype(bf)
    past = np.zeros((NG, 128, 4, 16), np.float32)
    own = np.zeros((NG, 128, 4, 16), np.float32)
    for g in range(NG):
        for t4 in range(4):
            qblk = (g * 4 + t4) // 2
            past[g, :, t4, qblk:] = -1e30
            own[g, :, t4, qblk] = 1.0
    c["c_past"] = past
    c["c_own"] = own
    inv = np.zeros((4, 128, 16), np.float32)
    for g4, w in enumerate(POOL_W):
        for t in range(16):
            inv[g4, :, t] = 1.0 / min(t + 1, w)
    c["c_invcnt"] = inv
    c["c_ustrict"] = np.triu(np.ones((128, 128), np.float32), k=1).astype(bf)
    misc = np.zeros((128, 64), np.float32)
    misc[:, 0:24] = np.arange(24, dtype=np.float32)[None, :]
    for cc in range(14):
        misc[:, 24 + cc] = (cc // 7) * 896 + 7 * np.arange(128, dtype=np.float32) + (cc % 7)
    misc[:, 52] = np.arange(128, dtype=np.float32)
    misc[:, 53:61] = np.arange(128, dtype=np.float32)[:, None] * 8 + np.arange(8, dtype=np.float32)[None, :]
    c["c_misc"] = misc
    return c


def prep_shared(inp):
    f = lambda a: np.ascontiguousarray(np.asarray(a, dtype=np.float32))
    sh = {}
    for nm in ("ln1_g", "ln1_b", "ln2_g", "ln2_b"):
        a = f(inp[nm])
        sh[nm] = np.ascontiguousarray(np.broadcast_to(a[:, None, :], (a.shape[0], 128, a.shape[1])))
    sh["ev_w_in"] = f(inp["ev_w_in"])
    sh["ev_negb"] = f(inp["ev_b_forget"]).reshape(2, 8, 1)
    sh["ev_pool_w"] = f(inp["ev_pool_w"])
    sh["ev_pool_scale"] = f(inp["ev_pool_scale"]).reshape(2, 4, 128, 1)
    sh["ev_w_out"] = f(inp["ev_w_out"])
    sh["ev_ffn_gate"] = f(inp["ev_ffn_gate"])[:, None]
    sh["ev_ffn_up"] = f(inp["ev_ffn_up"])[:, None]
    sh["ev_ffn_down"] = f(inp["ev_ffn_down"])[:, None]
    sh["od_w_in"] = f(inp["od_w_in"])
    sh["od_w_out"] = f(inp["od_w_out"])
    r = f(inp["od_router"])
    rt = np.transpose(r, (0, 2, 1))
    sh["od_router"] = np.ascontiguousarray(np.broadcast_to(rt[:, :, None, :], (2, 8, 128, D)))
    sh["od_exp_gate"] = f(inp["od_exp_gate"])
    sh["od_exp_up"] = f(inp["od_exp_up"])
    sh["od_exp_down"] = f(inp["od_exp_down"])
    return sh


def run(inputs, S, layers, dbg=False, stop=None):
    x = np.asarray(inputs["x"], dtype=np.float32)
    B = x.shape[0]
    sh = prep_shared(inputs)
    sh.update(make_consts(S))
    nc = build_nc(S, layers, 4, dbg, stop)
    in_maps = []
    for b in range(B):
        m = dict(sh)
        m["x"] = np.ascontiguousarray(x[b, :S])
        in_maps.append(m)
    res = run_bass_kernel_spmd(nc, in_maps, core_ids=list(range(B)))
    if dbg:
        return res.results
    return np.stack([np.asarray(r["y"], dtype=np.float32) for r in res.results], axis=0)


def kernel(**inputs):
    return run(inputs, 4096, [0, 1, 2, 3])
```
